# Optimizing a Trainium2 kernel written in Bass

```python
import math
import jax, jax.numpy as jnp
from jax import lax
import numpy as np

D_MODEL = 4096
BATCH = 2
SEQ = 4096
DEPTH = 1

MIX_POOL = D_MODEL // 2
N_POOL_GROUPS = 4
POOL_WINDOWS = (2, 4, 8, 16)
POOL_GW = MIX_POOL // N_POOL_GROUPS
HEAD_DIM = 128
N_Q_HEADS = (D_MODEL - MIX_POOL) // HEAD_DIM
N_KV_HEADS = 4
Q_PER_KV = N_Q_HEADS // N_KV_HEADS
N_IDX_HEADS = 8
IDX_DIM = 64
TOPK_MAX = 256
Q_BLOCK = 128
ROPE_THETA = 10000.0
N_MEM = 256
N_MEM_HEADS = 4
MEM_HEAD_DIM = D_MODEL // N_MEM_HEADS
N_GROUPS = 8
EXPERTS_PER_GROUP = 8
N_EXPERTS = N_GROUPS * EXPERTS_PER_GROUP
TOP_K_IN_GROUP = 2
EXPERT_FF = 512
MOE_BLOCK = 128
LN_EPS = 1e-5
ALPHA = (2.0 * DEPTH) ** 0.25
BETA = (8.0 * DEPTH) ** -0.25
IN_SIZES = (MIX_POOL, N_Q_HEADS * HEAD_DIM, N_KV_HEADS * HEAD_DIM, N_KV_HEADS * HEAD_DIM,
            N_IDX_HEADS * IDX_DIM, IDX_DIM, N_IDX_HEADS)

kernel_name = "hybrid_pool_dsa_hiermoe_deepnorm"


def layer_norm(x, g, b):
    xf = x.astype(jnp.float32)
    mu = jnp.mean(xf, axis=-1, keepdims=True)
    var = jnp.mean(jnp.square(xf - mu), axis=-1, keepdims=True)
    y = (xf - mu) * lax.rsqrt(var + LN_EPS) * g.astype(jnp.float32) + b.astype(jnp.float32)
    return y.astype(x.dtype)


def rope_tables(positions, dim):
    inv = 1.0 / (ROPE_THETA ** (jnp.arange(0, dim, 2, dtype=jnp.float32) / dim))
    ang = positions.astype(jnp.float32)[..., None] * inv
    return jnp.cos(ang), jnp.sin(ang)


def apply_rope(x, cos, sin):
    xf = x.astype(jnp.float32)
    half = x.shape[-1] // 2
    x1, x2 = xf[..., :half], xf[..., half:]
    c, s = cos[:, :, None, :], sin[:, :, None, :]
    return jnp.concatenate([x1 * c - x2 * s, x2 * c + x1 * s], axis=-1).astype(x.dtype)


def pool_mixer(v, pool_w, pool_scale):
    B, S, _ = v.shape
    vg = v.reshape(B, S, N_POOL_GROUPS, POOL_GW)
    csum = jnp.cumsum(vg.astype(jnp.float32), axis=1)
    csum = jnp.pad(csum, ((0, 0), (1, 0), (0, 0), (0, 0)))
    t = jnp.arange(S)
    outs = []
    for g, w in enumerate(POOL_WINDOWS):
        start = jnp.maximum(t + 1 - w, 0)
        win_sum = csum[:, 1:, g] - csum[:, start, g]
        cnt = (t + 1 - start).astype(jnp.float32)[None, :, None]
        outs.append(win_sum / cnt - vg[:, :, g].astype(jnp.float32))
    pooled = jnp.stack(outs, axis=2).astype(v.dtype)
    mixed = jnp.einsum('bsgc,gcd->bsgd', pooled, pool_w)
    return mixed.reshape(B, S, MIX_POOL) * pool_scale


def dsa_attention(q, k, v, qi, ki, wi):
    B, S = q.shape[0], q.shape[1]
    n_sel = min(TOPK_MAX, S // 4)
    nb = S // Q_BLOCK
    key_pos = jnp.arange(S)

    def to_blocks(a):
        return a.reshape((B, nb, Q_BLOCK) + a.shape[2:]).swapaxes(0, 1)

    def block_fn(args):
        blk, qb, qib, wib = args
        tpos = blk * Q_BLOCK + jnp.arange(Q_BLOCK)
        rel = jax.nn.relu(jnp.einsum('bqhd,bsd->bqhs', qib, ki).astype(jnp.float32))
        score = jnp.einsum('bqhs,bqh->bqs', rel, wib.astype(jnp.float32))
        causal = key_pos[None, :] <= tpos[:, None]
        score = jnp.where(causal[None], score, -jnp.inf)
        _, idx = lax.top_k(score, n_sel)
        valid = idx <= tpos[None, :, None]
        kg = jax.vmap(lambda kb, ib: kb[ib])(k, idx)
        vg = jax.vmap(lambda vb, ib: vb[ib])(v, idx)
        qg = qb.reshape(B, Q_BLOCK, N_KV_HEADS, Q_PER_KV, HEAD_DIM)
        logits = jnp.einsum('bqgrd,bqkgd->bqgrk', qg, kg).astype(jnp.float32) * (HEAD_DIM ** -0.5)
        logits = jnp.where(valid[:, :, None, None, :], logits, -jnp.inf)
        p = jax.nn.softmax(logits, axis=-1).astype(v.dtype)
        o = jnp.einsum('bqgrk,bqkgd->bqgrd', p, vg)
        return o.reshape(B, Q_BLOCK, N_Q_HEADS * HEAD_DIM)

    out = lax.map(block_fn, (jnp.arange(nb), to_blocks(q), to_blocks(qi), to_blocks(wi)))
    return out.swapaxes(0, 1).reshape(B, S, N_Q_HEADS * HEAD_DIM)


def mem_cross_attention(x, mem, w_mq, w_mk, w_mv, w_mo):
    B, S, _ = x.shape
    M = mem.shape[1]
    q = (x @ w_mq).reshape(B, S, N_MEM_HEADS, MEM_HEAD_DIM)
    k = (mem @ w_mk).reshape(B, M, N_MEM_HEADS, MEM_HEAD_DIM)
    v = (mem @ w_mv).reshape(B, M, N_MEM_HEADS, MEM_HEAD_DIM)
    logits = jnp.einsum('bshd,bmhd->bhsm', q, k).astype(jnp.float32) * (MEM_HEAD_DIM ** -0.5)
    p = jax.nn.softmax(logits, axis=-1).astype(v.dtype)
    o = jnp.einsum('bhsm,bmhd->bshd', p, v).reshape(B, S, D_MODEL)
    return o @ w_mo


def hier_moe(x, w_gr, b_gr, w_er, b_er, w_gate, w_up, w_down):
    B, S, D = x.shape
    N = B * S
    xt = x.reshape(N, D)
    g_logits = (xt @ w_gr).astype(jnp.float32) + b_gr.astype(jnp.float32)
    g_prob = jax.nn.softmax(g_logits, axis=-1)
    g_idx = jnp.argmax(g_logits, axis=-1)
    g_gate = jnp.take_along_axis(g_prob, g_idx[:, None], axis=-1)
    e_logits = ((xt @ w_er).astype(jnp.float32) + b_er.astype(jnp.float32)).reshape(N, N_GROUPS, EXPERTS_PER_GROUP)
    e_logits = jnp.take_along_axis(e_logits, g_idx[:, None, None], axis=1)[:, 0]
    top_vals, top_loc = lax.top_k(e_logits, TOP_K_IN_GROUP)
    gate = jax.nn.softmax(top_vals, axis=-1) * g_gate
    expert_id = g_idx[:, None] * EXPERTS_PER_GROUP + top_loc

    A = N * TOP_K_IN_GROUP
    eid = expert_id.reshape(A)
    tok = jnp.arange(A, dtype=jnp.int32) // TOP_K_IN_GROUP
    wgt = gate.reshape(A)
    order = jnp.argsort(eid)
    e_sorted = eid[order]
    counts = jnp.bincount(eid, length=N_EXPERTS)
    padded = ((counts + MOE_BLOCK - 1) // MOE_BLOCK) * MOE_BLOCK
    start = jnp.cumsum(counts) - counts
    pend = jnp.cumsum(padded)
    pstart = pend - padded
    dest = pstart[e_sorted] + (jnp.arange(A) - start[e_sorted])
    P = A + N_EXPERTS * MOE_BLOCK
    row_tok = jnp.full((P,), N, dtype=jnp.int32).at[dest].set(tok[order])
    row_w = jnp.zeros((P,), jnp.float32).at[dest].set(wgt[order])
    nblk = P // MOE_BLOCK
    blk_e = jnp.minimum(jnp.searchsorted(pend, jnp.arange(nblk) * MOE_BLOCK, side='right'), N_EXPERTS - 1)
    xpad = jnp.concatenate([xt, jnp.zeros((1, D), xt.dtype)], axis=0)

    def expert_block(args):
        rows, e = args
        xb = xpad[rows]
        h = jax.nn.silu(xb @ w_gate[e]) * (xb @ w_up[e])
        return h @ w_down[e]

    yb = lax.map(expert_block, (row_tok.reshape(nblk, MOE_BLOCK), blk_e))
    contrib = yb.reshape(P, D).astype(jnp.float32) * row_w[:, None]
    y = jax.ops.segment_sum(contrib, row_tok, num_segments=N + 1)[:N]
    return y.astype(x.dtype).reshape(B, S, D)


def setup_inputs(seed: int = 0) -> dict:
    key = jax.random.key(seed)
    ks = jax.random.split(key, 23)
    f32 = jnp.float32
    L = DEPTH
    d_in = sum(IN_SIZES)

    def nrm(k, shape, scale):
        return jax.random.normal(k, shape, f32) * scale

    return {
        'x': nrm(ks[0], (BATCH, SEQ, D_MODEL), 1.0),
        'mem': nrm(ks[1], (BATCH, N_MEM, D_MODEL), 1.0),
        'positions': jnp.broadcast_to(jnp.arange(SEQ, dtype=jnp.int32), (BATCH, SEQ)),
        'w_in': nrm(ks[2], (L, D_MODEL, d_in), D_MODEL ** -0.5),
        'pool_w': nrm(ks[3], (L, N_POOL_GROUPS, POOL_GW, POOL_GW), POOL_GW ** -0.5),
        'pool_scale': 1.0 + nrm(ks[4], (L, MIX_POOL), 0.02),
        'w_o': nrm(ks[5], (L, D_MODEL, D_MODEL), BETA * D_MODEL ** -0.5),
        'ln1_g': 1.0 + nrm(ks[6], (L, D_MODEL), 0.02),
        'ln1_b': nrm(ks[7], (L, D_MODEL), 0.02),
        'w_mq': nrm(ks[8], (L, D_MODEL, D_MODEL), D_MODEL ** -0.5),
        'w_mk': nrm(ks[9], (L, D_MODEL, D_MODEL), D_MODEL ** -0.5),
        'w_mv': nrm(ks[10], (L, D_MODEL, D_MODEL), D_MODEL ** -0.5),
        'w_mo': nrm(ks[11], (L, D_MODEL, D_MODEL), BETA * D_MODEL ** -0.5),
        'ln2_g': 1.0 + nrm(ks[12], (L, D_MODEL), 0.02),
        'ln2_b': nrm(ks[13], (L, D_MODEL), 0.02),
        'w_group_router': nrm(ks[14], (L, D_MODEL, N_GROUPS), D_MODEL ** -0.5),
        'b_group_router': nrm(ks[15], (L, N_GROUPS), 0.01),
        'w_expert_router': nrm(ks[16], (L, D_MODEL, N_EXPERTS), D_MODEL ** -0.5),
        'b_expert_router': nrm(ks[17], (L, N_EXPERTS), 0.01),
        'w_gate': nrm(ks[18], (L, N_EXPERTS, D_MODEL, EXPERT_FF), D_MODEL ** -0.5),
        'w_up': nrm(ks[19], (L, N_EXPERTS, D_MODEL, EXPERT_FF), D_MODEL ** -0.5),
        'w_down': nrm(ks[20], (L, N_EXPERTS, EXPERT_FF, D_MODEL), BETA * EXPERT_FF ** -0.5),
        'ln3_g': 1.0 + nrm(ks[21], (L, D_MODEL), 0.02),
        'ln3_b': nrm(ks[22], (L, D_MODEL), 0.02),
    }


def reference(x, mem, positions, w_in, pool_w, pool_scale, w_o, ln1_g, ln1_b,
              w_mq, w_mk, w_mv, w_mo, ln2_g, ln2_b,
              w_group_router, b_group_router, w_expert_router, b_expert_router,
              w_gate, w_up, w_down, ln3_g, ln3_b):
    B, S, _ = x.shape
    cos_h, sin_h = rope_tables(positions, HEAD_DIM)
    cos_i, sin_i = rope_tables(positions, IDX_DIM)
    split_pts = []
    acc = 0
    for sz in IN_SIZES[:-1]:
        acc += sz
        split_pts.append(acc)
    idx_w_scale = (N_IDX_HEADS ** -0.5) * (IDX_DIM ** -0.5)

    for l in range(DEPTH):
        h = x @ w_in[l]
        v_pool, q, k, v, qi, ki, wi = jnp.split(h, split_pts, axis=-1)
        q = apply_rope(q.reshape(B, S, N_Q_HEADS, HEAD_DIM), cos_h, sin_h)
        k = apply_rope(k.reshape(B, S, N_KV_HEADS, HEAD_DIM), cos_h, sin_h)
        v = v.reshape(B, S, N_KV_HEADS, HEAD_DIM)
        qi = apply_rope(qi.reshape(B, S, N_IDX_HEADS, IDX_DIM), cos_i, sin_i)
        ki = apply_rope(ki.reshape(B, S, 1, IDX_DIM), cos_i, sin_i)[:, :, 0]
        wi = wi * idx_w_scale
        a_pool = pool_mixer(v_pool, pool_w[l], pool_scale[l])
        a_attn = dsa_attention(q, k, v, qi, ki, wi)
        mix = jnp.concatenate([a_pool, a_attn], axis=-1) @ w_o[l]
        x = layer_norm(ALPHA * x + mix, ln1_g[l], ln1_b[l])
        c = mem_cross_attention(x, mem, w_mq[l], w_mk[l], w_mv[l], w_mo[l])
        x = layer_norm(ALPHA * x + c, ln2_g[l], ln2_b[l])
        f = hier_moe(x, w_group_router[l], b_group_router[l], w_expert_router[l], b_expert_router[l],
                     w_gate[l], w_up[l], w_down[l])
        x = layer_norm(ALPHA * x + f, ln3_g[l], ln3_b[l])
    return x
```

```python
import os
import math
from contextlib import ExitStack
import numpy as np
import ml_dtypes
import concourse.bass as bass
import concourse.mybir as mybir
from concourse.bass_utils import run_bass_kernel_spmd

F32 = mybir.dt.float32
BF16 = mybir.dt.bfloat16
I32 = mybir.dt.int32
AF = mybir.ActivationFunctionType
ALU = mybir.AluOpType

D = 4096
S = 4096
T = 1024
NT = 8
DIN = 5704
ALPHA = 2.0 ** 0.25
LN_EPS = 1e-5
NEXP = 64
CAP = 128
TWO_PI = 2.0 * math.pi
C1 = 6.28125
C2 = TWO_PI - C1
PI_SAFE = 3.1415925
NEG_BIG = -3.0e30
NEG_MID = -2.0e30
ENGS = ["tensor", "vector", "scalar", "gpsimd", "sync"]


class Sem:
    def __init__(self, h, name):
        self.h = h
        self.name = name
        self.count = 0


class Prog:
    def __init__(self, nc, es):
        self.nc = nc
        self.es = es
        self.nsem = 0
        self.pool = []
        self.pool_idx = 0
        self.eng_sem = {e: self._alloc_sem("pg_" + e) for e in ENGS}
        self.waited = {e: {} for e in ENGS}
        self.uid = 0

    def _alloc_sem(self, name):
        self.nsem += 1
        nm = "%s_%d" % (name, self.nsem)
        return Sem(self.es.enter_context(self.nc.semaphore(nm)), nm)

    def new_sem(self, name="s"):
        if self.pool_idx == len(self.pool):
            self.pool.append(self._alloc_sem("d"))
        sem = self.pool[self.pool_idx]
        self.pool_idx += 1
        return sem

    def name(self, base):
        self.uid += 1
        return "%s_%d" % (base, self.uid)


class Phase:
    def __init__(self, prog):
        self.p = prog
        self.nc = prog.nc
        self.ops = {e: [] for e in ENGS}
        self.dma_toks = {}
        self.es = None

    def sb(self, name, shape, dt):
        return self.es.enter_context(self.nc.sbuf_tensor(self.p.name(name), list(shape), dt))

    def ps(self, name, shape=(128, 512), dt=F32):
        return self.es.enter_context(self.nc.psum_tensor(self.p.name(name), list(shape), dt))

    def op(self, eng, fn, waits=(), sig=True):
        ws = tuple(w for w in waits if w is not None)
        if sig:
            s = self.p.eng_sem[eng]
            s.count += 1
            tok = (s, s.count)
            self.ops[eng].append((ws, fn, (s, 1)))
            return tok
        self.ops[eng].append((ws, fn, None))
        return None

    def pe(self, fn, waits=(), sig=True):
        return self.op("tensor", fn, waits, sig)

    def dve(self, fn, waits=(), sig=True):
        return self.op("vector", fn, waits, sig)

    def act(self, fn, waits=(), sig=True):
        return self.op("scalar", fn, waits, sig)

    def pool(self, fn, waits=(), sig=True):
        return self.op("gpsimd", fn, waits, sig)

    def dma(self, eng, out, in_, sem, waits=()):
        ws = tuple(w for w in waits if w is not None)
        sem.count += 16
        tok = (sem, sem.count)
        self.ops[eng].append((ws, (lambda e, o=out, i=in_: e.dma_start(out=o, in_=i)), (sem, 16)))
        self.dma_toks[sem.name] = tok
        return tok

    def idma(self, out, in_, sem, waits=(), out_off=None, in_off=None):
        ws = tuple(w for w in waits if w is not None)
        sem.count += 16
        tok = (sem, sem.count)

        def fn(e, o=out, i=in_, oo=out_off, io=in_off):
            return e.indirect_dma_start(
                out=o, out_offset=(bass.IndirectOffsetOnAxis(ap=oo, axis=0) if oo is not None else None),
                in_=i, in_offset=(bass.IndirectOffsetOnAxis(ap=io, axis=0) if io is not None else None))
        self.ops["gpsimd"].append((ws, fn, (sem, 16)))
        self.dma_toks[sem.name] = tok
        return tok

    def emit(self, block):
        fin = tuple(self.dma_toks.values())
        if fin:
            self.ops["sync"].append((fin, None, None))
        for eng in ENGS:
            ops = self.ops[eng]
            if not ops:
                continue
            waited = self.p.waited[eng]

            def body(e, ops=ops, waited=waited):
                for ws, fn, inc in ops:
                    for (sem, val) in ws:
                        if waited.get(sem.name, 0) < val:
                            e.wait_ge(sem.h, val)
                            waited[sem.name] = val
                    if fn is None:
                        continue
                    inst = fn(e)
                    if inc is not None:
                        inst.then_inc(inc[0].h, inc[1])
            getattr(block, eng)(body)


def run_phase(prog, fn):
    nc = prog.nc
    with ExitStack() as es:
        ph = Phase(prog)
        prog.pool_idx = 0
        ph.es = es
        block = es.enter_context(nc.Block())
        fn(ph)
        ph.emit(block)


class Ring:
    def __init__(self, ph, name, nslots, shape, dt, queue="gpsimd"):
        self.ph = ph
        self.n = nslots
        self.tiles = [ph.sb(name + str(i), shape, dt) for i in range(nslots)]
        self.sems = [ph.p.new_sem(name) for _ in range(nslots)]
        self.free = [None] * nslots
        self.k = 0
        self.queue = queue

    def load(self, parts):
        s = self.k % self.n
        self.k += 1
        tok = None
        for dst_fn, src in parts:
            tok = self.ph.dma(self.queue, dst_fn(self.tiles[s]), src, self.sems[s], waits=[self.free[s]])
        return s, self.tiles[s], tok

    def release(self, s, tok):
        self.free[s] = tok


class Stream:
    def __init__(self, ring, pieces):
        self.ring = ring
        self.pieces = pieces
        self.loaded = []

    def get(self, i):
        want = min(len(self.pieces), i + self.ring.n)
        while len(self.loaded) < want:
            self.loaded.append(self.ring.load(self.pieces[len(self.loaded)]))
        return self.loaded[i]

    def done(self, i, tok):
        self.ring.release(self.loaded[i][0], tok)


def rope_tables(ph, K, posi, n, tabs, tmp, waits):
    posf, arg, ki, kf, r = tmp
    t = ph.dve(lambda e: e.tensor_copy(out=posf[:, :n], in_=posi[:, :n]), waits)
    last_act = None
    for ti, tab in enumerate(tabs):
        fam = ti // 2
        is_cos = (ti % 2 == 0)
        inv = K.invs[:, 2 * fam:2 * fam + 1]
        sign = K.invs[:, 2 * fam + 1:2 * fam + 2]
        shift = (math.pi / 2.0) if is_cos else 0.0
        t = ph.dve(lambda e, inv=inv, shift=shift: e.tensor_scalar(
            out=arg[:, :n], in0=posf[:, :n], scalar1=inv, scalar2=shift, op0=ALU.mult, op1=ALU.add), [t, last_act])
        t = ph.dve(lambda e: e.tensor_scalar(
            out=ki[:, :n], in0=arg[:, :n], scalar1=1.0 / TWO_PI, scalar2=None, op0=ALU.mult), [t])
        t = ph.dve(lambda e: e.tensor_copy(out=kf[:, :n], in_=ki[:, :n]), [t])
        t = ph.dve(lambda e: e.scalar_tensor_tensor(
            out=r[:, :n], in0=kf[:, :n], scalar=-C1, in1=arg[:, :n], op0=ALU.mult, op1=ALU.add), [t, last_act])
        t = ph.dve(lambda e: e.scalar_tensor_tensor(
            out=r[:, :n], in0=kf[:, :n], scalar=-C2, in1=r[:, :n], op0=ALU.mult, op1=ALU.add), [t])
        t = ph.dve(lambda e: e.tensor_scalar(
            out=r[:, :n], in0=r[:, :n], scalar1=-PI_SAFE, scalar2=PI_SAFE, op0=ALU.max, op1=ALU.min), [t])
        if is_cos:
            last_act = ph.act(lambda e, tab=tab: e.activation(out=tab[:, :n], in_=r[:, :n], func=AF.Sin), [t])
        else:
            last_act = ph.act(lambda e, tab=tab, sign=sign: e.activation(
                out=tab[:, :n], in_=r[:, :n], func=AF.Sin, scale=sign), [t])
        t = last_act
    return last_act


def rope_fm(ph, src_ps, n, u, sw_ps, t1, t2, cosT, sinT, perm, out_ap, waits, view=None):
    if view is None:
        view = lambda ap: ap
    a = ph.act(lambda e: e.activation(out=u[:, :n], in_=src_ps, func=AF.Copy), waits)
    b = ph.pe(lambda e: e.matmul(sw_ps[:, :n], perm, u[:, :n], start=True, stop=True), [a])
    c = ph.dve(lambda e: e.tensor_tensor(out=t1[:, :n], in0=u[:, :n], in1=cosT, op=ALU.mult), [a])
    d = ph.dve(lambda e: e.tensor_tensor(out=t2[:, :n], in0=sw_ps[:, :n], in1=sinT, op=ALU.mult), [b, c])
    f = ph.dve(lambda e: e.tensor_tensor(out=out_ap, in0=view(t1[:, :n]), in1=view(t2[:, :n]), op=ALU.add), [d])
    return f, a


class Consts:
    pass


def phase_consts(ph, K, Dm):
    sem = ph.p.new_sem("c")
    for nm in ["ident_bf", "ident_f", "permH", "permI", "invs", "ones_bf", "tri", "iota64"]:
        ph.dma("sync", getattr(K, nm)[:], Dm[nm], sem)
    K.tok = ph.dma("sync", K.bias_r[:], Dm["b_r"].to_broadcast([128, 72]), sem)


def phase_A(ph, K, Dm):
    CH = 256
    NCH = S // CH
    w_in_v = Dm["w_in"].rearrange("(kc p) n -> p kc n", p=128)
    xT_v = Dm["xT_ctx"].rearrange("(kc p) t -> p kc t", p=128)
    Wk = ph.sb("Wk", [128, 32, 512], BF16)
    Wv = ph.sb("Wv", [128, 32, 512], BF16)
    Wki = ph.sb("Wki", [128, 32, 128], BF16)
    semw = ph.p.new_sem("wA")
    tw = None
    for kg in range(4):
        ks = slice(kg * 8, kg * 8 + 8)
        ph.dma("gpsimd", Wk[:, ks, :], w_in_v[:, ks, 4096:4608], semw)
        ph.dma("gpsimd", Wv[:, ks, :], w_in_v[:, ks, 4608:5120], semw)
        ph.dma("gpsimd", Wki[:, ks, 0:64], w_in_v[:, ks, 5632:5696], semw)
        tw = ph.dma("gpsimd", Wki[:, ks, 64:128], w_in_v[:, ks, 5632:5696], semw)
    xring = Ring(ph, "xc", 2, [128, 32, CH], BF16)
    pieces = []
    for c in range(NCH):
        pieces.append([((lambda t, kg=kg: t[:, kg * 8:kg * 8 + 8, :]), xT_v[:, kg * 8:kg * 8 + 8, c * CH:(c + 1) * CH])
                       for kg in range(4)])
    xs = Stream(xring, pieces)
    posi = [ph.sb("posi", [128, CH], I32) for _ in range(2)]
    sem_pos = [ph.p.new_sem("pos") for _ in range(2)]
    tmp = (ph.sb("posf", [128, CH], F32), ph.sb("arg", [128, CH], F32), ph.sb("ki", [128, CH], I32),
           ph.sb("kf", [128, CH], F32), ph.sb("r", [128, CH], F32))
    tabs = [tuple(ph.sb("tab", [128, CH], F32) for _ in range(4)) for _ in range(2)]
    psk = [ph.ps("psk") for _ in range(4)]
    psv = [ph.ps("psv") for _ in range(2)]
    pski = ph.ps("pski")
    pssw = ph.ps("pssw")
    u = ph.sb("u", [128, CH], BF16)
    t1 = ph.sb("t1", [128, CH], F32)
    t2 = ph.sb("t2", [128, CH], F32)
    kout = [ph.sb("kout", [128, 4, CH], BF16) for _ in range(2)]
    vout = [ph.sb("vout", [128, 2, 512], BF16) for _ in range(2)]
    kiout = [ph.sb("kiout", [128, CH], BF16) for _ in range(2)]
    sem_o = [ph.p.new_sem("oA") for _ in range(2)]
    out_tok = [None, None]
    tab_free = [None, None]
    psk_free = [None] * 4
    psv_free = [None] * 2
    pski_free = None
    s_v_v = Dm["s_v"].rearrange("(n p) c -> p n c", p=128)
    for c in range(NCH):
        par = c % 2
        s, xt, xtok = xs.get(c)
        tp = ph.dma("sync", posi[par][:], Dm["pos_ctx"][0:1, c * CH:(c + 1) * CH].to_broadcast([128, CH]),
                    sem_pos[par], waits=[tab_free[par]])
        ttab = rope_tables(ph, K, posi[par], CH, tabs[par], tmp, [tp, tab_free[par]])
        cosH, sinH, cosI, sinI = tabs[par]
        last_rope = None
        for g in range(4):
            tk = None
            for kc in range(32):
                tk = ph.pe(lambda e, g=g, kc=kc, xt=xt: e.matmul(
                    psk[g][:, :CH], Wk[:, kc, g * 128:(g + 1) * 128], xt[:, kc, :], start=(kc == 0), stop=(kc == 31)),
                    [tw, xtok, psk_free[g]] if kc == 0 else (), sig=(kc == 31))
            f, a = rope_fm(ph, psk[g][:, :CH], CH, u, pssw, t1, t2, cosH[:, :], sinH[:, :], K.permH[:],
                           kout[par][:, g, :], [tk, ttab, out_tok[par], last_rope])
            psk_free[g] = a
            last_rope = f
        tk = None
        for kc in range(32):
            tk = ph.pe(lambda e, kc=kc, xt=xt: e.matmul(
                pski[:, :CH], Wki[:, kc, :], xt[:, kc, :], start=(kc == 0), stop=(kc == 31)),
                [pski_free] if kc == 0 else (), sig=(kc == 31))
        f, a = rope_fm(ph, pski[:, :CH], CH, u, pssw, t1, t2, cosI[:, :], sinI[:, :], K.permI[:],
                       kiout[par][:, :], [tk, last_rope])
        pski_free = a
        last_rope = f
        tab_free[par] = f
        tv_ev = []
        for tt in range(2):
            tk = None
            for kc in range(32):
                tk = ph.pe(lambda e, tt=tt, kc=kc, xt=xt: e.matmul(
                    psv[tt][:, :], xt[:, kc, tt * 128:(tt + 1) * 128], Wv[:, kc, :], start=(kc == 0), stop=(kc == 31)),
                    [psv_free[tt]] if kc == 0 else (), sig=(kc == 31))
            ev = ph.act(lambda e, tt=tt, par=par: e.activation(out=vout[par][:, tt, :], in_=psv[tt][:, :], func=AF.Copy),
                        [tk, out_tok[par]])
            psv_free[tt] = ev
            tv_ev.append(ev)
            if tt == 1:
                xs.done(c, tk)
        ph.dma("sync", Dm["s_kT"][:, :, c * CH:(c + 1) * CH], kout[par][:, :, :], sem_o[par], waits=[last_rope])
        ph.dma("sync", Dm["s_ki2"][:, c * CH:(c + 1) * CH], kiout[par][:, :], sem_o[par], waits=[last_rope])
        out_tok[par] = ph.dma("sync", s_v_v[:, 2 * c:2 * c + 2, :], vout[par][:, :, :], sem_o[par], waits=tv_ev)


def phase_B(ph, K, Dm):
    TO = T + 128
    w_in_v = Dm["w_in"].rearrange("(kc p) n -> p kc n", p=128)
    xT_v = Dm["xT_own"].rearrange("(kc p) t -> p kc t", p=128)
    xo = ph.sb("xo", [128, 32, TO], BF16)
    semx = ph.p.new_sem("xo")
    tx = None
    for kg in range(8):
        tx = ph.dma("gpsimd", xo[:, kg * 4:kg * 4 + 4, :], xT_v[:, kg * 4:kg * 4 + 4, :], semx)
    posi = [ph.sb("posi", [128, 512], I32) for _ in range(2)]
    tmp = (ph.sb("posf", [128, 512], F32), ph.sb("arg", [128, 512], F32), ph.sb("ki", [128, 512], I32),
           ph.sb("kf", [128, 512], F32), ph.sb("r", [128, 512], F32))
    tabs = tuple(ph.sb("tab", [128, T], F32) for _ in range(4))
    ttab = None
    for hf in range(2):
        tp = ph.dma("sync", posi[hf][:], Dm["pos_own"][0:1, hf * 512:hf * 512 + 512].to_broadcast([128, 512]),
                    ph.p.new_sem("posB"))
        ttab = rope_tables(ph, K, posi[hf], 512, tuple(tb[:, hf * 512:hf * 512 + 512] for tb in tabs), tmp, [tp, ttab])
    cosH, sinH, cosI, sinI = tabs
    vp = ph.sb("vp", [128, 9, 2048], BF16)
    ring = Ring(ph, "wB", 4, [128, 8, 512], BF16)
    blocks = [("vp", nb, nb * 512, 512) for nb in range(4)] + [("vph", nb, nb * 512, 512) for nb in range(4)] + \
             [("q", nb, 2048 + nb * 512, 512) for nb in range(4)] + [("qi", 0, 5120, 512), ("wi", 0, 5696, 8)]
    pieces = []
    for (kind, nb, c0, nc_) in blocks:
        for kg in range(4):
            pieces.append([((lambda t, nc_=nc_: t[:, :, :nc_]), w_in_v[:, kg * 8:kg * 8 + 8, c0:c0 + nc_])])
    ws = Stream(ring, pieces)
    psb = [ph.ps("psB") for _ in range(8)]
    bank_free = [None] * 8
    u = ph.sb("u", [128, 512], BF16)
    t1 = ph.sb("t1", [128, 512], F32)
    t2 = ph.sb("t2", [128, 512], F32)
    qst = [ph.sb("qst", [128, 8, 512], BF16) for _ in range(2)]
    qist = [ph.sb("qist", [128, T], BF16) for _ in range(2)]
    sem_q = [ph.p.new_sem("qo") for _ in range(2)]
    q_free = [None, None]
    nq = 0
    last_rope = None
    vp_done = []
    for bi, (kind, nb, c0, nc_) in enumerate(blocks):
        last_tok = [None] * 8
        for kg in range(4):
            pi = bi * 4 + kg
            s, wt, wtok = ws.get(pi)
            tk = None
            for k8 in range(8):
                kc = kg * 8 + k8
                first = (kc == 0)
                lastk = (kc == 31)
                if kind in ("vp", "wi"):
                    for t in range(8):
                        tk = ph.pe(lambda e, t=t, kc=kc, k8=k8, wt=wt, nc_=nc_: e.matmul(
                            psb[t][:, :nc_], xo[:, kc, 128 + t * 128:256 + t * 128], wt[:, k8, :nc_],
                            start=(kc == 0), stop=(kc == 31)),
                            [wtok, tx, bank_free[t]] if first else ([wtok] if k8 == 0 and t == 0 else ()),
                            sig=(lastk or (k8 == 7 and t == 7)))
                        if lastk:
                            last_tok[t] = tk
                elif kind == "vph":
                    tk = ph.pe(lambda e, kc=kc, k8=k8, wt=wt: e.matmul(
                        psb[0][:, :], xo[:, kc, 0:128], wt[:, k8, :], start=(kc == 0), stop=(kc == 31)),
                        [wtok, tx, bank_free[0]] if first else ([wtok] if k8 == 0 else ()), sig=(lastk or k8 == 7))
                    if lastk:
                        last_tok[0] = tk
                else:
                    for ch in range(4):
                        for hf in range(2):
                            b = ch * 2 + hf
                            tk = ph.pe(lambda e, ch=ch, hf=hf, b=b, kc=kc, k8=k8, wt=wt: e.matmul(
                                psb[b][:, :], wt[:, k8, ch * 128:(ch + 1) * 128], xo[:, kc, 128 + hf * 512:640 + hf * 512],
                                start=(kc == 0), stop=(kc == 31)),
                                [wtok, tx, bank_free[b]] if first else ([wtok] if k8 == 0 and b == 0 else ()),
                                sig=(lastk or (k8 == 7 and b == 7)))
                            if lastk:
                                last_tok[b] = tk
            ws.done(pi, tk)
        if kind == "vp":
            for t in range(8):
                dst = vp[:, 1 + t, c0:c0 + 512]
                if t % 2 == 0:
                    ev = ph.act(lambda e, t=t, dst=dst: e.activation(out=dst, in_=psb[t][:, :], func=AF.Copy), [last_tok[t]])
                else:
                    ev = ph.dve(lambda e, t=t, dst=dst: e.tensor_copy(out=dst, in_=psb[t][:, :]), [last_tok[t]])
                bank_free[t] = ev
                vp_done.append(ev)
        elif kind == "vph":
            ev = ph.act(lambda e, c0=c0: e.activation(out=vp[:, 0, c0:c0 + 512], in_=psb[0][:, :], func=AF.Copy), [last_tok[0]])
            bank_free[0] = ev
            vp_done.append(ev)
        elif kind == "wi":
            sc = (8 ** -0.5) * (64 ** -0.5)
            for t in range(8):
                b_ = ph.dve(lambda e, t=t: e.tensor_scalar(out=K.wsgn[:, t, :], in0=psb[t][:, 0:8], scalar1=0.0, scalar2=2.0,
                                                          op0=ALU.is_gt, op1=ALU.mult), [last_tok[t]])
                c_ = ph.dve(lambda e, t=t: e.tensor_scalar(out=K.wsgn[:, t, :], in0=K.wsgn[:, t, :], scalar1=-1.0, scalar2=None,
                                                          op0=ALU.add), [b_])
                c_ = ph.dve(lambda e, t=t: e.scalar_tensor_tensor(out=K.wabs[:, t, :], in0=psb[t][:, 0:8], scalar=sc,
                                                                 in1=K.wsgn[:, t, :], op0=ALU.mult, op1=ALU.mult), [c_])
                bank_free[t] = c_
        else:
            par = nq % 2
            nq += 1
            for ch in range(4):
                for hf in range(2):
                    b = ch * 2 + hf
                    if kind == "q":
                        out_ap = qst[par][:, hf * 4:hf * 4 + 4, ch * 128:(ch + 1) * 128]
                        f, a = rope_fm(ph, psb[b][:, :], 512, u, psb[b], t1, t2, cosH[:, hf * 512:hf * 512 + 512],
                                       sinH[:, hf * 512:hf * 512 + 512], K.permH[:], out_ap,
                                       [last_tok[b], ttab, q_free[par], last_rope],
                                       view=lambda ap: ap.rearrange("p (a b) -> p a b", a=4))
                    else:
                        out_ap = qist[par][:, hf * 512:hf * 512 + 512]
                        f, a = rope_fm(ph, psb[b][:, :], 512, u, psb[b], t1, t2, cosI[:, hf * 512:hf * 512 + 512],
                                       sinI[:, hf * 512:hf * 512 + 512], K.permI[:], out_ap,
                                       [last_tok[b], ttab, q_free[par], last_rope])
                    bank_free[b] = f
                    last_rope = f
                if kind == "qi" :
                    if True:
                        q_free[par] = ph.dma("sync", Dm["s_qi2"][:, ch, :], qist[par][:, :], sem_q[par], waits=[last_rope])
                        par = nq % 2
                        nq += 1
            if kind == "q":
                q_free[par] = ph.dma("sync", Dm["s_qT"][:, :, nb * 512:(nb + 1) * 512], qst[par][:, :, :], sem_q[par],
                                     waits=[last_rope])
    ph.dma("sync", Dm["s_vp"], vp[:], semx, waits=vp_done)


def phase_B2(ph, K, Dm):
    semx = ph.p.new_sem("b2")
    band = ph.sb("band", [128, 4, 4, 128], BF16)
    pw = ph.sb("pw", [128, 4, 4, 512], BF16)
    pscale = ph.sb("pscale", [128, 16], F32)
    vp = ph.sb("vp", [128, 9, 2048], BF16)
    ph.dma("sync", band[:], Dm["band"], semx)
    ph.dma("sync", pscale[:], Dm["pool_scale_c"], semx)
    ph.dma("sync", vp[:], Dm["s_vp"], semx)
    tpw = None
    for g in range(4):
        tpw = ph.dma("gpsimd", pw[:, g, :, :], Dm["pool_w"][g].rearrange("(cc p) d -> p cc d", p=128), semx)
    vp_done = [tpw]
    psb = [ph.ps("psB2") for _ in range(4)]
    bank_free = [None] * 4
    pl = [ph.sb("pl", [128, 512], BF16) for _ in range(4)]
    atst = [ph.sb("atst", [128, 4, 512], BF16) for _ in range(2)]
    sem_at = [ph.p.new_sem("at") for _ in range(2)]
    at_free = [None, None]
    pl_free = [None] * 4
    it = 0
    for g in range(4):
        for hf in range(2):
            par = it % 2
            it += 1
            pl_tok = []
            for cc in range(4):
                bank = cc % 2
                tk = None
                for i in range(4):
                    tile = hf * 4 + i
                    kA, kB = (2, 3) if tile == 0 else (0, 1)
                    col = g * 512 + cc * 128
                    ph.pe(lambda e, bank=bank, i=i, tile=tile, col=col, kA=kA, g=g: e.matmul(
                        psb[bank][:, i * 128:(i + 1) * 128], vp[:, 1 + tile, col:col + 128], band[:, g, kA, :],
                        start=True, stop=False), (vp_done + [bank_free[bank], tpw]) if i == 0 else (), sig=False)
                    tk = ph.pe(lambda e, bank=bank, i=i, tile=tile, col=col, kB=kB, g=g: e.matmul(
                        psb[bank][:, i * 128:(i + 1) * 128], vp[:, tile, col:col + 128], band[:, g, kB, :],
                        start=False, stop=True), (), sig=(i == 3))
                ev = ph.act(lambda e, bank=bank, cc=cc: e.activation(out=pl[cc][:, :], in_=psb[bank][:, :], func=AF.Copy),
                            [tk, pl_free[cc]])
                bank_free[bank] = ev
                pl_tok.append(ev)
            last_m = None
            for dc in range(4):
                bank = 2 + dc % 2
                tk = None
                for cc in range(4):
                    tk = ph.pe(lambda e, bank=bank, cc=cc, dc=dc, g=g: e.matmul(
                        psb[bank][:, :], pw[:, g, cc, dc * 128:(dc + 1) * 128], pl[cc][:, :], start=(cc == 0), stop=(cc == 3)),
                        (pl_tok + [bank_free[bank]]) if cc == 0 else (), sig=(cc == 3))
                ev = ph.act(lambda e, bank=bank, dc=dc, g=g, par=par: e.activation(
                    out=atst[par][:, dc, :], in_=psb[bank][:, :], func=AF.Identity, scale=pscale[:, g * 4 + dc:g * 4 + dc + 1]),
                    [tk, at_free[par]])
                bank_free[bank] = ev
                last_m = tk
            for cc in range(4):
                pl_free[cc] = last_m
            at_free[par] = ph.dma("sync", Dm["s_AT"][:, g * 4:g * 4 + 4, hf * 512:hf * 512 + 512], atst[par][:, :, :],
                                  sem_at[par], waits=[ev])


PHASES = []
NCORES = int(os.environ.get("K_NCORES", "2"))
NQ = 8 // NCORES
MOE_FUSED = os.environ.get("K_MOE", "1") == "1"


def build(stop=99, dbg=()):
    nc = bass.Bass("TRN2", target_bir_lowering=False)
    Dm = {}

    def din(name, shape, dt=F32):
        Dm[name] = nc.dram_tensor(name, list(shape), dt, kind="ExternalInput").ap()

    def dscr(name, shape, dt):
        kind = "ExternalOutput" if name in dbg else "Internal"
        Dm[name] = nc.dram_tensor(name, list(shape), dt, kind=kind).ap()

    din("xT_ctx", [D, S]); din("memT", [D, 256]); din("pos_ctx", [1, S], I32)
    for j in range(NQ):
        din("xT_own_%d" % j, [D, T + 128]); din("x_own_%d" % j, [T, D]); din("pos_own_%d" % j, [1, T], I32)
        din("cbias_%d" % j, [T, S]); din("band_%d" % j, [128, 4, 4, 128], BF16)
    din("ident_bf", [128, 128], BF16); din("ident_f", [128, 128]); din("permH", [128, 128], BF16)
    din("permI", [128, 128], BF16); din("invs", [128, 4]); din("ones_bf", [128, 128], BF16)
    din("tri", [128, 128], BF16); din("iota64", [128, 64])
    din("w_in", [D, DIN]); din("pool_w", [4, 512, 512]); din("pool_scale_c", [128, 16]); din("w_o", [D, D])
    for nm in ["ln1_g", "ln1_b", "ln2_g", "ln2_b", "ln3_g", "ln3_b"]:
        din(nm, [1, D])
    din("w_mq", [D, D]); din("w_mk", [D, D]); din("w_mv", [D, D]); din("w_mo", [D, D])
    din("w_r", [D, 72]); din("b_r", [1, 72])
    if MOE_FUSED:
        din("w_gate", [NEXP, D, 512]); din("w_up", [NEXP, D, 512]); din("w_down", [NEXP, 512, D])
    dscr("s_kT", [128, 4, S], BF16); dscr("s_v", [S, 512], BF16); dscr("s_ki2", [128, S], BF16)
    dscr("s_qT", [128, 8, 2048], BF16); dscr("s_qi2", [128, 4, T], BF16); dscr("s_AT", [128, 32, T], BF16); dscr("s_vp", [128, 9, 2048], BF16)
    dscr("s_r", [T, D], F32); dscr("s_x1", [T, D], F32); dscr("s_x1T", [128, 32, T], BF16)
    dscr("s_qmT", [128, 32, T], BF16); dscr("s_kmT", [128, 32, 256], BF16); dscr("s_vm", [128, 2, D], BF16); dscr("s_oT", [128, 32, T], BF16); dscr("s_x2T", [128, 32, T], BF16); dscr("s_x2", [T, D], F32); dscr("s_x2b", [T, D], BF16)
    dscr("s_xe", [NEXP * CAP, D], BF16); dscr("s_ye", [NEXP * CAP, D], F32)
    dscr("s_dbg", [128, 512], F32); dscr("s_dbgi", [128, 16], I32)
    out_full = nc.dram_tensor("out", [NQ * T, D], F32, kind="ExternalOutput").ap()

    with ExitStack() as es:
        prog = Prog(nc, es)
        K = Consts()

        def psb(name, shape, dt):
            return es.enter_context(nc.sbuf_tensor(name, list(shape), dt))
        K.ident_bf = psb("ident_bf_s", [128, 128], BF16); K.ident_f = psb("ident_f_s", [128, 128], F32)
        K.permH = psb("permH_s", [128, 128], BF16); K.permI = psb("permI_s", [128, 128], BF16)
        K.invs = psb("invs_s", [128, 4], F32); K.ones_bf = psb("ones_s", [128, 128], BF16)
        K.tri = psb("tri_s", [128, 128], BF16); K.iota64 = psb("iota_s", [128, 64], F32)
        K.bias_r = psb("bias_r_s", [128, 72], F32)
        K.wabs = psb("wabs_s", [128, 8, 8], F32); K.wsgn = psb("wsgn_s", [128, 8, 8], F32)
        K.Aall = psb("Aall_s", [128, 8, 64], BF16)
        K.A12 = psb("A12_s", [128, 8, 2, 64], F32)
        K.gates = psb("gates_s", [128, 8, 2], F32)
        K.dest = psb("dest_s", [128, 8, 2], I32)
        run_phase(prog, lambda ph: phase_consts(ph, K, Dm))
        run_phase(prog, lambda ph: phase_A(ph, K, Dm))
        plist = [phase_B, phase_B2] + PHASES
        for j in range(NQ):
            for nm in ("xT_own", "x_own", "pos_own", "cbias", "band"):
                Dm[nm] = Dm["%s_%d" % (nm, j)]
            Dm["out"] = out_full[j * T:(j + 1) * T, :]
            for i, fn in enumerate(plist):
                if i > stop:
                    break
                run_phase(prog, lambda ph, fn=fn: fn(ph, K, Dm))
        for nm in ("xT_own", "x_own", "pos_own", "cbias", "band", "out"):
            Dm.pop(nm, None)
        if dbg:
            def dbgf(ph):
                sem = ph.p.new_sem("dbg")
                ph.dma("sync", Dm["s_dbg"][:, 0:64], K.wabs[:].rearrange("p a b -> p (a b)"), sem)
                ph.dma("sync", Dm["s_dbg"][:, 64:128], K.wsgn[:].rearrange("p a b -> p (a b)"), sem)
                ph.dma("sync", Dm["s_dbg"][:, 128:144], K.gates[:].rearrange("p a b -> p (a b)"), sem)
                ph.dma("sync", Dm["s_dbgi"][:, :], K.dest[:].rearrange("p a b -> p (a b)"), sem)
            run_phase(prog, dbgf)
    nc._in_names = [k for k in Dm if not k.startswith("s_") and k != "out"]
    return nc


def _bf(a):
    return np.ascontiguousarray(a).astype(ml_dtypes.bfloat16)


def host_consts():
    c = {}
    c["ident_bf"] = _bf(np.eye(128, dtype=np.float32))
    c["ident_f"] = np.eye(128, dtype=np.float32)
    m = np.arange(128)
    pH = np.zeros((128, 128), np.float32); pH[(m + 64) % 128, m] = 1.0
    pI = np.zeros((128, 128), np.float32); pI[64 * (m // 64) + ((m % 64) + 32) % 64, m] = 1.0
    c["permH"] = _bf(pH); c["permI"] = _bf(pI)
    invH = (1.0 / (np.float32(10000.0) ** (np.arange(0, 128, 2, dtype=np.float32) / np.float32(128)))).astype(np.float32)
    invI = (1.0 / (np.float32(10000.0) ** (np.arange(0, 64, 2, dtype=np.float32) / np.float32(64)))).astype(np.float32)
    invs = np.zeros((128, 4), np.float32)
    invs[:, 0] = invH[m % 64]; invs[:, 1] = np.where(m < 64, -1.0, 1.0)
    invs[:, 2] = invI[m % 32]; invs[:, 3] = np.where((m % 64) < 32, -1.0, 1.0)
    c["invs"] = invs
    c["ones_bf"] = _bf(np.ones((128, 128), np.float32))
    c["tri"] = _bf((m[:, None] < m[None, :]).astype(np.float32))
    c["iota64"] = np.tile(np.arange(64, dtype=np.float32)[None, :], (128, 1))
    return c


def host_band(first_of_seq):
    band = np.zeros((128, 4, 4, 128), np.float32)
    tp = np.arange(128)[:, None]; t = np.arange(128)[None, :]
    for g, w in enumerate((2, 4, 8, 16)):
        inwin = (tp <= t) & (tp >= t - w + 1)
        PA = inwin * (1.0 / w) - (tp == t)
        PB = ((tp - 128) >= (t - w + 1)) * (1.0 / w)
        cnt = np.minimum(t + 1, w).astype(np.float32)
        PA0 = inwin / cnt - (tp == t)
        band[:, g, 0] = PA; band[:, g, 1] = PB
        band[:, g, 2] = PA0 if first_of_seq else PA
        band[:, g, 3] = 0.0 if first_of_seq else PB
    return _bf(band)


_NC_CACHE = {}


def kernel(**inp):
    stop = int(os.environ.get("K_STOP", "99"))
    dbg = tuple(x for x in os.environ.get("K_DBG", "").split(",") if x)
    key = (stop, dbg)
    if key not in _NC_CACHE:
        _NC_CACHE[key] = build(stop, dbg)
    nc = _NC_CACHE[key]
    x = np.asarray(inp["x"], np.float32); mem = np.asarray(inp["mem"], np.float32)
    pos = np.asarray(inp["positions"]).astype(np.int32)
    shared = host_consts()
    g = lambda k: np.ascontiguousarray(np.asarray(inp[k], np.float32)[0])
    shared["w_in"] = g("w_in"); shared["pool_w"] = g("pool_w")
    shared["pool_scale_c"] = np.ascontiguousarray(g("pool_scale").reshape(16, 128).T)
    shared["w_o"] = g("w_o")
    for nm in ["ln1_g", "ln1_b", "ln2_g", "ln2_b", "ln3_g", "ln3_b"]:
        shared[nm] = g(nm).reshape(1, D)
    for nm in ["w_mq", "w_mk", "w_mv", "w_mo", "w_gate", "w_up", "w_down"]:
        shared[nm] = g(nm)
    shared["w_r"] = np.ascontiguousarray(np.concatenate([g("w_group_router"), g("w_expert_router")], axis=1))
    shared["b_r"] = np.concatenate([g("b_group_router"), g("b_expert_router")]).reshape(1, 72)
    xT = [np.ascontiguousarray(x[b].T) for b in range(2)]
    memT = [np.ascontiguousarray(mem[b].T) for b in range(2)]
    bands = {True: host_band(True), False: host_band(False)}
    in_maps = []
    kidx = np.arange(S)[None, :]
    cpb = NCORES // 2 if NCORES >= 2 else 1
    for c in range(NCORES):
        b = c // cpb
        m = dict(shared)
        m["xT_ctx"] = xT[b]
        m["memT"] = memT[b]
        m["pos_ctx"] = np.ascontiguousarray(pos[b].reshape(1, S))
        for jl in range(NQ):
            j = (c % cpb) * NQ + jl
            own = np.zeros((D, T + 128), np.float32)
            own[:, 128:] = xT[b][:, j * T:(j + 1) * T]
            if j > 0:
                own[:, :128] = xT[b][:, j * T - 128:j * T]
            m["xT_own_%d" % jl] = own
            m["x_own_%d" % jl] = np.ascontiguousarray(x[b, j * T:(j + 1) * T])
            m["pos_own_%d" % jl] = np.ascontiguousarray(pos[b, j * T:(j + 1) * T].reshape(1, T))
            qidx = (j * T + np.arange(T))[:, None]
            m["cbias_%d" % jl] = np.where(kidx <= qidx, np.float32(0.0), np.float32(NEG_BIG)).astype(np.float32)
            m["band_%d" % jl] = bands[j == 0]
        in_maps.append({k: m[k] for k in nc._in_names})
    res = run_bass_kernel_spmd(nc, in_maps, core_ids=list(range(NCORES)))
    kernel.last = res
    out = np.zeros((2, S, D), np.float32)
    for c in range(NCORES):
        b = c // cpb
        j0 = (c % cpb) * NQ
        out[b, j0 * T:(j0 + NQ) * T] = np.asarray(res.results[c]["out"])
    return out

def phase_C(ph, K, Dm):
    sem_l = ph.p.new_sem("lc")
    kT = ph.sb("kT", [128, 4, S], BF16)
    va = ph.sb("va", [128, 32, 4, 130], BF16)
    ki2 = ph.sb("ki2", [128, S], BF16)
    qT = ph.sb("qT", [128, 8, 2048], BF16)
    qi2 = ph.sb("qi2", [128, 4, T], BF16)
    for g in range(4):
        ph.dma("sync", kT[:, g, :], Dm["s_kT"][:, g, :], sem_l)
        ph.dma("sync", va[:, :, g, 0:128], Dm["s_v"][:, g * 128:(g + 1) * 128].rearrange("(n p) d -> p n d", p=128), sem_l)
    ph.dma("sync", ki2[:], Dm["s_ki2"], sem_l)
    ph.dma("sync", qT[:], Dm["s_qT"], sem_l)
    tl = ph.dma("sync", qi2[:], Dm["s_qi2"], sem_l)
    tones = ph.dve(lambda e: e.memset(va[:, :, :, 128:130], 1.0))
    sc = ph.sb("sc", [128, S], F32)
    work = ph.sb("work", [128, S], F32)
    rl = [ph.sb("rl", [128, 512], F32) for _ in range(2)]
    mask = ph.sb("mask", [128, S], BF16)
    mT = ph.sb("mT", [128, 32, 128], BF16)
    pT = [ph.sb("pT", [128, 512], BF16) for _ in range(3)]
    osb = ph.sb("osb", [128, 2048], BF16)
    atT = ph.sb("atT", [128, 16, 128], BF16)
    m8 = ph.sb("m8", [128, 8], F32)
    rec = ph.sb("rec", [128, 4], F32)
    psS = [ph.ps("psS") for _ in range(2)]
    psT = ph.ps("psT", (128, 1024), BF16)
    psO = [ph.ps("psO") for _ in range(4)]
    sem_b = ph.p.new_sem("cb")
    sem_at = ph.p.new_sem("cat")
    psS_free = [None, None]
    psT_free = None
    psO_free = [None] * 4
    rl_free = [None, None]
    pT_free = [None] * 3
    sc_free = None
    mT_free = None
    osb_free = None
    atT_free = None
    nS = 0
    nP = 0
    scale = 128 ** -0.5
    for i in range(NT):
        tb = ph.dma("sync", sc[:], Dm["cbias"][i * 128:(i + 1) * 128, :], sem_b, waits=[sc_free])
        nR = 0
        last_acc = None
        for kb in range(8):
            acc = tb
            for h in range(8):
                par = nS % 2
                nS += 1
                lo = (h % 2) * 64
                tk = ph.pe(lambda e, par=par, lo=lo, h=h, kb=kb, i=i: e.matmul(
                    psS[par][:, :], qi2[lo:lo + 64, h // 2, i * 128:(i + 1) * 128], ki2[lo:lo + 64, kb * 512:(kb + 1) * 512],
                    start=True, stop=True), [tl, psS_free[par]])
                rp = nR % 2
                nR += 1
                a = ph.act(lambda e, par=par, rp=rp, h=h, i=i: e.activation(
                    out=rl[rp][:, :], in_=psS[par][:, :], func=AF.Relu, scale=K.wabs[:, i, h:h + 1]), [tk, rl_free[rp]])
                psS_free[par] = a
                acc = ph.dve(lambda e, rp=rp, h=h, i=i, kb=kb: e.scalar_tensor_tensor(
                    out=sc[:, kb * 512:(kb + 1) * 512], in0=rl[rp][:, :], scalar=K.wsgn[:, i, h:h + 1],
                    in1=sc[:, kb * 512:(kb + 1) * 512], op0=ALU.mult, op1=ALU.add), [a, acc])
                rl_free[rp] = acc
            last_acc = acc
        t = ph.dve(lambda e: e.max(out=m8[:, :], in_=sc[:, :]), [last_acc, mT_free])
        t = ph.dve(lambda e: e.match_replace(out=work[:, :], in_to_replace=m8[:, :], in_values=sc[:, :], imm_value=NEG_MID), [t])
        for it in range(31):
            t = ph.dve(lambda e: e.max(out=m8[:, :], in_=work[:, :]), [t])
            if it < 30:
                t = ph.dve(lambda e: e.match_replace(out=work[:, :], in_to_replace=m8[:, :], in_values=work[:, :],
                                                     imm_value=NEG_MID), [t])
        tm = ph.dve(lambda e: e.tensor_scalar(out=mask[:, :], in0=sc[:, :], scalar1=m8[:, 7:8], scalar2=None, op0=ALU.is_ge), [t])
        sc_free = tm
        evs = []
        for k4 in range(8):
            tk = None
            for q in range(4):
                tk = ph.pe(lambda e, k4=k4, q=q: e.transpose(psT[:, q * 128:(q + 1) * 128], mask[:, (k4 * 4 + q) * 128:(k4 * 4 + q + 1) * 128],
                                                          K.ident_bf[:]), [tm, psT_free] if q == 0 else (), sig=(q == 3))
            ev = ph.act(lambda e, k4=k4: e.activation(out=mT[:, k4 * 4:k4 * 4 + 4, :],
                                                     in_=psT[:, 0:512].rearrange("p (a b) -> p a b", a=4), func=AF.Copy),
                        [tk, mT_free])
            psT_free = ev
            evs.append(ev)
        tmT = evs[-1]
        last_pool = None
        for g in range(4):
            for kt in range(32):
                par = nS % 2
                nS += 1
                tk = ph.pe(lambda e, par=par, g=g, kt=kt, i=i: e.matmul(
                    psS[par][:, :], kT[:, g, kt * 128:(kt + 1) * 128], qT[:, i, g * 512:(g + 1) * 512], start=True, stop=True),
                    [tl, psS_free[par]])
                p3 = nP % 3
                nP += 1
                a = ph.act(lambda e, par=par, p3=p3: e.activation(out=pT[p3][:, :], in_=psS[par][:, :], func=AF.Exp, scale=scale),
                           [tk, pT_free[p3]])
                psS_free[par] = a
                m = ph.pool(lambda e, p3=p3, kt=kt: e.tensor_tensor(
                    out=pT[p3][:, :].rearrange("p (a b) -> p a b", a=4), in0=pT[p3][:, :].rearrange("p (a b) -> p a b", a=4),
                    in1=mT[:, kt, :].unsqueeze(1).to_broadcast([128, 4, 128]), op=ALU.mult), [a, tmT])
                last_pool = m
                for hh in range(4):
                    tk = ph.pe(lambda e, hh=hh, p3=p3, kt=kt, g=g: e.matmul(
                        psO[hh][:, 0:130], pT[p3][:, hh * 128:(hh + 1) * 128], va[:, kt, g, :], start=(kt == 0), stop=(kt == 31)),
                        [m, tones, psO_free[hh]] if (hh == 0 or kt == 0) else (), sig=(hh == 3))
                pT_free[p3] = tk
            tr = ph.dve(lambda e: e.tensor_copy(out=rec[:, :], in_=rec[:, :]), [tk], sig=False) if False else None
            for hh in range(4):
                r1 = ph.dve(lambda e, hh=hh: e.reciprocal(out=rec[:, hh:hh + 1], in_=psO[hh][:, 128:129]), [tk, osb_free])
                r2 = ph.dve(lambda e, hh=hh, g=g: e.tensor_scalar(
                    out=osb[:, (4 * g + hh) * 128:(4 * g + hh + 1) * 128], in0=psO[hh][:, 0:128], scalar1=rec[:, hh:hh + 1],
                    scalar2=None, op0=ALU.mult), [r1])
                psO_free[hh] = r2
            last_o = r2
        mT_free = last_pool
        for hq in range(4):
            tk = None
            for q in range(4):
                h = hq * 4 + q
                tk = ph.pe(lambda e, q=q, h=h: e.transpose(psT[:, q * 128:(q + 1) * 128], osb[:, h * 128:(h + 1) * 128], K.ident_bf[:]),
                           [last_o, psT_free] if q == 0 else (), sig=(q == 3))
            ev = ph.act(lambda e, hq=hq: e.activation(out=atT[:, hq * 4:hq * 4 + 4, :],
                                                     in_=psT[:, 0:512].rearrange("p (a b) -> p a b", a=4), func=AF.Copy),
                        [tk, atT_free])
            psT_free = ev
        osb_free = tk
        atT_free = ph.dma("sync", Dm["s_AT"][:, 16:32, i * 128:(i + 1) * 128], atT[:, :, :], sem_at, waits=[ev])


def gemm_resid(ph, K, Dm, at_name, w_name, xres_name):
    sem_l = ph.p.new_sem("gl")
    AT = ph.sb("AT", [128, 32, T], BF16)
    tA = None
    for kg in range(4):
        tA = ph.dma("sync", AT[:, kg * 8:kg * 8 + 8, :], Dm[at_name][:, kg * 8:kg * 8 + 8, :], sem_l)
    w_v = Dm[w_name].rearrange("(kc p) n -> p kc n", p=128)
    x_v = Dm[xres_name].rearrange("(t p) c -> p t c", p=128)
    r_v = Dm["s_r"].rearrange("(t p) c -> p t c", p=128)
    ring = Ring(ph, "wG", 4, [128, 8, 512], BF16)
    pieces = [[((lambda t: t[:, :, :]), w_v[:, kg * 8:kg * 8 + 8, nb * 512:(nb + 1) * 512])] for nb in range(8) for kg in range(4)]
    ws = Stream(ring, pieces)
    xring = Ring(ph, "xb", 2, [128, 8, 512], F32, queue="sync")
    xs = Stream(xring, [[((lambda t: t[:, :, :]), x_v[:, :, nb * 512:(nb + 1) * 512])] for nb in range(8)])
    rb = [ph.sb("rb", [128, 8, 512], F32) for _ in range(2)]
    sem_r = [ph.p.new_sem("ro") for _ in range(2)]
    rb_free = [None, None]
    psb = [ph.ps("psG") for _ in range(8)]
    bank_free = [None] * 8
    for nb in range(8):
        last_tok = [None] * 8
        for kg in range(4):
            pi = nb * 4 + kg
            s, wt, wtok = ws.get(pi)
            tk = None
            for k8 in range(8):
                kc = kg * 8 + k8
                for t in range(8):
                    tk = ph.pe(lambda e, t=t, kc=kc, k8=k8, wt=wt: e.matmul(
                        psb[t][:, :], AT[:, kc, t * 128:(t + 1) * 128], wt[:, k8, :], start=(kc == 0), stop=(kc == 31)),
                        [wtok, tA, bank_free[t]] if kc == 0 else ([wtok] if k8 == 0 and t == 0 else ()),
                        sig=(kc == 31 or (k8 == 7 and t == 7)))
                    if kc == 31:
                        last_tok[t] = tk
            ws.done(pi, tk)
        par = nb % 2
        sx, xt, xtok = xs.get(nb)
        ev = None
        for t in range(8):
            ev = ph.dve(lambda e, t=t, xt=xt, par=par: e.scalar_tensor_tensor(
                out=rb[par][:, t, :], in0=xt[:, t, :], scalar=ALPHA, in1=psb[t][:, :], op0=ALU.mult, op1=ALU.add),
                [last_tok[t], xtok, rb_free[par]])
            bank_free[t] = ev
        xs.done(nb, ev)
        rb_free[par] = ph.dma("sync", r_v[:, :, nb * 512:(nb + 1) * 512], rb[par][:, :, :], sem_r[par], waits=[ev])


def ln_phase(ph, K, Dm, g_name, b_name, dst_x, dst_T=None, dst_b16=None):
    sem_l = ph.p.new_sem("ll")
    gb = ph.sb("gb", [128, D], F32)
    bb = ph.sb("bb", [128, D], F32)
    ph.dma("sync", gb[:], Dm[g_name].to_broadcast([128, D]), sem_l)
    tgb = ph.dma("sync", bb[:], Dm[b_name].to_broadcast([128, D]), sem_l)
    rring = Ring(ph, "rt", 2, [128, D], F32, queue="sync")
    rs = Stream(rring, [[((lambda t: t[:, :]), Dm["s_r"][t * 128:(t + 1) * 128, :])] for t in range(NT)])
    st = ph.sb("st", [128, 8, 6], F32)
    mv = ph.sb("mv", [128, 2], F32)
    sd = ph.sb("sd", [128, 4], F32)
    xn = ph.sb("xn", [128, D], F32)
    xg = ph.sb("xg", [128, D], F32)
    yt = [ph.sb("yt", [128, D], F32) for _ in range(2)]
    sem_y = [ph.p.new_sem("yo") for _ in range(2)]
    y_free = [[], []]
    sem_T2 = ph.p.new_sem("yT2")
    yT = ph.sb("yT", [128, 32, 128], BF16) if dst_T is not None else None
    yb = ph.sb("ybf", [128, D], BF16) if dst_b16 is not None else None
    sem_T = ph.p.new_sem("yT")
    yT_free = None
    yb_free = None
    psF = [ph.ps("psF") for _ in range(2)]
    psF_free = [None, None]
    xn_free = None
    xg_free = None
    for t in range(NT):
        s, rt, rtok = rs.get(t)
        par = t % 2
        d = None
        for c in range(8):
            d = ph.dve(lambda e, c=c, rt=rt: e.bn_stats(out=st[:, c, :], in_=rt[:, c * 512:(c + 1) * 512]), [rtok, d])
        d = ph.dve(lambda e: e.bn_aggr(out=mv[:, :], in_=st[:, :, :].rearrange("p a b -> p (a b)")), [d])
        d = ph.dve(lambda e: e.tensor_scalar(out=sd[:, 0:1], in0=mv[:, 1:2], scalar1=LN_EPS, scalar2=None, op0=ALU.add), [d])
        a = ph.act(lambda e: e.activation(out=sd[:, 1:2], in_=sd[:, 0:1], func=AF.Sqrt), [d])
        d = ph.dve(lambda e: e.reciprocal(out=sd[:, 2:3], in_=sd[:, 1:2]), [a])
        d = ph.dve(lambda e: e.tensor_scalar(out=sd[:, 3:4], in0=mv[:, 0:1], scalar1=sd[:, 2:3], scalar2=-1.0,
                                             op0=ALU.mult, op1=ALU.mult), [d])
        a = ph.act(lambda e, rt=rt: e.activation(out=xn[:, :], in_=rt[:, :], func=AF.Identity, scale=sd[:, 2:3], bias=sd[:, 3:4]),
                   [d, xn_free])
        rs.done(t, a)
        p = ph.pool(lambda e: e.tensor_tensor(out=xg[:, :], in0=xn[:, :], in1=gb[:, :], op=ALU.mult), [a, tgb, xg_free])
        xn_free = p
        y = ph.dve(lambda e, par=par: e.tensor_tensor(out=yt[par][:, :], in0=xg[:, :], in1=bb[:, :], op=ALU.add),
                   [p, tgb] + y_free[par])
        xg_free = y
        users = [ph.dma("sync", Dm[dst_x][t * 128:(t + 1) * 128, :], yt[par][:, :], sem_y[par], waits=[y])]
        if dst_b16 is not None:
            cb = ph.act(lambda e, par=par: e.activation(out=yb[:, :], in_=yt[par][:, :], func=AF.Copy), [y, yb_free])
            yb_free = ph.dma("sync", Dm[dst_b16][t * 128:(t + 1) * 128, :], yb[:, :], sem_T, waits=[cb])
            users.append(cb)
        if dst_T is not None:
            ev = None
            for c4 in range(8):
                bp = c4 % 2
                tk = None
                for q in range(4):
                    c = c4 * 4 + q
                    tk = ph.pe(lambda e, bp=bp, q=q, c=c, par=par: e.transpose(
                        psF[bp][:, q * 128:(q + 1) * 128], yt[par][:, c * 128:(c + 1) * 128], K.ident_f[:]),
                        [y, psF_free[bp]] if q == 0 else (), sig=(q == 3))
                if c4 % 2 == 0:
                    ev = ph.act(lambda e, bp=bp, c4=c4: e.activation(
                        out=yT[:, c4 * 4:c4 * 4 + 4, :], in_=psF[bp][:, :].rearrange("p (a b) -> p a b", a=4), func=AF.Copy),
                        [tk, yT_free])
                else:
                    ev = ph.dve(lambda e, bp=bp, c4=c4: e.tensor_copy(
                        out=yT[:, c4 * 4:c4 * 4 + 4, :], in_=psF[bp][:, :].rearrange("p (a b) -> p a b", a=4)), [tk, yT_free, ev])
                psF_free[bp] = ev
                last_tr = tk
            users.append(last_tr)
            yT_free = ph.dma("sync", Dm[dst_T][:, :, t * 128:(t + 1) * 128], yT[:, :, :], sem_T2,
                             waits=[ev, psF_free[0], psF_free[1]])
        y_free[par] = users


def phase_D1(ph, K, Dm):
    gemm_resid(ph, K, Dm, "s_AT", "w_o", "x_own")


def phase_D2(ph, K, Dm):
    ln_phase(ph, K, Dm, "ln1_g", "ln1_b", "s_x1", dst_T="s_x1T")


def phase_E1(ph, K, Dm):
    sem_l = ph.p.new_sem("e1")
    mt_sb = ph.sb("memT", [128, 32, 256], BF16)
    tm = None
    mem_v = Dm["memT"].rearrange("(kc p) t -> p kc t", p=128)
    for kg in range(4):
        tm = ph.dma("gpsimd", mt_sb[:, kg * 8:kg * 8 + 8, :], mem_v[:, kg * 8:kg * 8 + 8, :], sem_l)
    kmT = ph.sb("kmT", [128, 32, 256], BF16)
    vm = ph.sb("vm", [128, 2, D], BF16)
    ring = Ring(ph, "wE1", 4, [128, 8, 512], BF16)
    pieces = []
    for wn in ("w_mk", "w_mv"):
        w_v = Dm[wn].rearrange("(kc p) n -> p kc n", p=128)
        for nb in range(8):
            for kg in range(4):
                pieces.append([((lambda t: t[:, :, :]), w_v[:, kg * 8:kg * 8 + 8, nb * 512:(nb + 1) * 512])])
    ws = Stream(ring, pieces)
    psb = [ph.ps("psE1") for _ in range(8)]
    bank_free = [None] * 8
    evs = []
    for wi_, wn in enumerate(("w_mk", "w_mv")):
        for nb in range(8):
            off = (nb % 2) * 4 if wi_ == 0 else (nb % 4) * 2
            nbk = 4 if wi_ == 0 else 2
            last_tok = [None] * 8
            for kg in range(4):
                pi = (wi_ * 8 + nb) * 4 + kg
                s, wt, wtok = ws.get(pi)
                tk = None
                for k8 in range(8):
                    kc = kg * 8 + k8
                    for b in range(nbk):
                        if wi_ == 0:
                            fn = (lambda e, b=b, kc=kc, k8=k8, wt=wt, off=off: e.matmul(
                                psb[off + b][:, 0:256], wt[:, k8, b * 128:(b + 1) * 128], mt_sb[:, kc, :],
                                start=(kc == 0), stop=(kc == 31)))
                        else:
                            fn = (lambda e, b=b, kc=kc, k8=k8, wt=wt, off=off: e.matmul(
                                psb[off + b][:, :], mt_sb[:, kc, b * 128:(b + 1) * 128], wt[:, k8, :],
                                start=(kc == 0), stop=(kc == 31)))
                        tk = ph.pe(fn, [wtok, tm, bank_free[off + b]] if kc == 0 else ([wtok] if k8 == 0 and b == 0 else ()),
                                   sig=(kc == 31 or (k8 == 7 and b == nbk - 1)))
                        if kc == 31:
                            last_tok[b] = tk
                ws.done(pi, tk)
            for b in range(nbk):
                if wi_ == 0:
                    dst = kmT[:, nb * 4 + b, :]
                    src = psb[off + b][:, 0:256]
                else:
                    dst = vm[:, b, nb * 512:(nb + 1) * 512]
                    src = psb[off + b][:, :]
                if b % 2 == 0:
                    ev = ph.act(lambda e, dst=dst, src=src: e.activation(out=dst, in_=src, func=AF.Copy), [last_tok[b]])
                else:
                    ev = ph.dve(lambda e, dst=dst, src=src: e.tensor_copy(out=dst, in_=src), [last_tok[b]])
                bank_free[off + b] = ev
                evs.append(ev)
    ph.dma("sync", Dm["s_kmT"], kmT[:], sem_l, waits=evs)
    ph.dma("sync", Dm["s_vm"], vm[:], sem_l, waits=evs)


def phase_E2(ph, K, Dm):
    sem_l = ph.p.new_sem("e2")
    xT = ph.sb("x1T", [128, 32, T], BF16)
    tx = None
    for kg in range(4):
        tx = ph.dma("sync", xT[:, kg * 8:kg * 8 + 8, :], Dm["s_x1T"][:, kg * 8:kg * 8 + 8, :], sem_l)
    w_v = Dm["w_mq"].rearrange("(kc p) n -> p kc n", p=128)
    ring = Ring(ph, "wE2", 4, [128, 8, 512], BF16)
    ws = Stream(ring, [[((lambda t: t[:, :, :]), w_v[:, kg * 8:kg * 8 + 8, nb * 512:(nb + 1) * 512])]
                       for nb in range(8) for kg in range(4)])
    psb = [ph.ps("psE2") for _ in range(8)]
    bank_free = [None] * 8
    qst = [ph.sb("qmst", [128, 4, T], BF16) for _ in range(2)]
    sem_q = [ph.p.new_sem("qmo") for _ in range(2)]
    q_free = [None, None]
    for nb in range(8):
        last_tok = [None] * 8
        for kg in range(4):
            pi = nb * 4 + kg
            s, wt, wtok = ws.get(pi)
            tk = None
            for k8 in range(8):
                kc = kg * 8 + k8
                for b in range(8):
                    ch, hf = b // 2, b % 2
                    tk = ph.pe(lambda e, b=b, ch=ch, hf=hf, kc=kc, k8=k8, wt=wt: e.matmul(
                        psb[b][:, :], wt[:, k8, ch * 128:(ch + 1) * 128], xT[:, kc, hf * 512:(hf + 1) * 512],
                        start=(kc == 0), stop=(kc == 31)),
                        [wtok, tx, bank_free[b]] if kc == 0 else ([wtok] if k8 == 0 and b == 0 else ()),
                        sig=(kc == 31 or (k8 == 7 and b == 7)))
                    if kc == 31:
                        last_tok[b] = tk
            ws.done(pi, tk)
        par = nb % 2
        evs = []
        for b in range(8):
            ch, hf = b // 2, b % 2
            dst = qst[par][:, ch, hf * 512:(hf + 1) * 512]
            if b % 2 == 0:
                ev = ph.act(lambda e, b=b, dst=dst: e.activation(out=dst, in_=psb[b][:, :], func=AF.Copy), [last_tok[b], q_free[par]])
            else:
                ev = ph.dve(lambda e, b=b, dst=dst: e.tensor_copy(out=dst, in_=psb[b][:, :]), [last_tok[b], q_free[par]])
            bank_free[b] = ev
            evs.append(ev)
        q_free[par] = ph.dma("sync", Dm["s_qmT"][:, nb * 4:nb * 4 + 4, :], qst[par][:, :, :], sem_q[par], waits=evs)


def phase_E3(ph, K, Dm):
    sem_l = ph.p.new_sem("e3")
    qmT = ph.sb("qmT", [128, 32, T], BF16)
    kmT = ph.sb("kmT", [128, 32, 256], BF16)
    vm = ph.sb("vm", [128, 2, D], BF16)
    for kg in range(4):
        ph.dma("sync", qmT[:, kg * 8:kg * 8 + 8, :], Dm["s_qmT"][:, kg * 8:kg * 8 + 8, :], sem_l)
    ph.dma("sync", kmT[:], Dm["s_kmT"], sem_l)
    tl = ph.dma("sync", vm[:], Dm["s_vm"], sem_l)
    pT = [[ph.sb("pTm", [128, 512], BF16) for _ in range(2)] for _ in range(2)]
    rs = ph.sb("rs", [128, 512], F32)
    ost = [ph.sb("ost", [128, 8, 512], BF16) for _ in range(2)]
    sem_o = [ph.p.new_sem("oo") for _ in range(2)]
    o_free = [None, None]
    psS = [ph.ps("psS3") for _ in range(2)]
    psZ = ph.ps("psZ")
    psO = [ph.ps("psO3") for _ in range(2)]
    psS_free = [None, None]
    psZ_free = None
    psO_free = [None, None]
    pT_free = [None, None]
    rs_free = None
    it = 0
    scale = 1024 ** -0.5
    for hm in range(4):
        for hf in range(2):
            par = it % 2
            it += 1
            exps = []
            for mt in range(2):
                tk = None
                for kc in range(8):
                    tk = ph.pe(lambda e, mt=mt, kc=kc, hm=hm, hf=hf: e.matmul(
                        psS[mt][:, :], kmT[:, hm * 8 + kc, mt * 128:(mt + 1) * 128], qmT[:, hm * 8 + kc, hf * 512:(hf + 1) * 512],
                        start=(kc == 0), stop=(kc == 7)), [tl, psS_free[mt]] if kc == 0 else (), sig=(kc == 7))
                a = ph.act(lambda e, mt=mt, par=par: e.activation(out=pT[par][mt][:, :], in_=psS[mt][:, :], func=AF.Exp, scale=scale),
                           [tk, pT_free[par]])
                psS_free[mt] = a
                exps.append(a)
            tk = None
            for mt in range(2):
                tk = ph.pe(lambda e, mt=mt, par=par: e.matmul(psZ[:, :], K.ones_bf[:], pT[par][mt][:, :], start=(mt == 0), stop=(mt == 1)),
                           (exps + [psZ_free]) if mt == 0 else (), sig=(mt == 1))
            r = ph.dve(lambda e: e.reciprocal(out=rs[:, :], in_=psZ[:, :]), [tk, rs_free])
            psZ_free = r
            ev = None
            for dc in range(8):
                bp = dc % 2
                tk = None
                for mt in range(2):
                    col = hm * 1024 + dc * 128
                    tk = ph.pe(lambda e, mt=mt, bp=bp, col=col, par=par: e.matmul(
                        psO[bp][:, :], vm[:, mt, col:col + 128], pT[par][mt][:, :], start=(mt == 0), stop=(mt == 1)),
                        (exps + [psO_free[bp]]) if mt == 0 else (), sig=(mt == 1))
                ev = ph.dve(lambda e, bp=bp, dc=dc, par=par: e.tensor_tensor(out=ost[par][:, dc, :], in0=psO[bp][:, :], in1=rs[:, :], op=ALU.mult),
                            [tk, r, o_free[par]])
                psO_free[bp] = ev
                last_pv = tk
            pT_free[par] = last_pv
            rs_free = ev
            o_free[par] = ph.dma("sync", Dm["s_oT"][:, hm * 8:hm * 8 + 8, hf * 512:(hf + 1) * 512], ost[par][:, :, :], sem_o[par],
                                 waits=[ev])


def phase_E4(ph, K, Dm):
    gemm_resid(ph, K, Dm, "s_oT", "w_mo", "s_x1")


def phase_E5(ph, K, Dm):
    ln_phase(ph, K, Dm, "ln2_g", "ln2_b", "s_x2", dst_T="s_x2T", dst_b16="s_x2b")


AXX = mybir.AxisListType.X


def phase_F1(ph, K, Dm):
    sem_l = ph.p.new_sem("f1")
    xT = ph.sb("x2T", [128, 32, T], BF16)
    tx = None
    for kg in range(4):
        tx = ph.dma("sync", xT[:, kg * 8:kg * 8 + 8, :], Dm["s_x2T"][:, kg * 8:kg * 8 + 8, :], sem_l)
    wr = ph.sb("wr", [128, 32, 72], BF16)
    twr = ph.dma("gpsimd", wr[:], Dm["w_r"].rearrange("(kc p) n -> p kc n", p=128), ph.p.new_sem("wr"))
    psb = [ph.ps("psF1") for _ in range(8)]
    lg = ph.sb("lg", [128, 72], F32)
    gm = ph.sb("gm", [128, 8], F32)
    ohg = ph.sb("ohg", [128, 8], F32)
    eg = ph.sb("eg", [128, 8], F32)
    sm = ph.sb("sm", [128, 8], F32)
    tmp3 = ph.sb("tmp3", [128, 8, 8], F32)
    esel = ph.sb("esel", [128, 8], F32)
    em = ph.sb("em", [128, 8], F32)
    oh1 = ph.sb("oh1", [128, 8], F32)
    oh2 = ph.sb("oh2", [128, 8], F32)
    d = None
    for t in range(NT):
        tk = None
        for kc in range(32):
            tk = ph.pe(lambda e, t=t, kc=kc: e.matmul(psb[t][:, 0:72], xT[:, kc, t * 128:(t + 1) * 128], wr[:, kc, :],
                                                      start=(kc == 0), stop=(kc == 31)), [tx, twr] if kc == 0 else (), sig=(kc == 31))
        d = ph.dve(lambda e, t=t: e.tensor_tensor(out=lg[:, :], in0=psb[t][:, 0:72], in1=K.bias_r[:, :], op=ALU.add), [tk, d])
        d = ph.dve(lambda e: e.max(out=gm[:, :], in_=lg[:, 0:8]), [d])
        d = ph.dve(lambda e: e.tensor_scalar(out=ohg[:, :], in0=lg[:, 0:8], scalar1=gm[:, 0:1], scalar2=None, op0=ALU.is_equal), [d])
        d = ph.dve(lambda e: e.tensor_scalar(out=sm[:, 0:1], in0=gm[:, 0:1], scalar1=-1.0, scalar2=None, op0=ALU.mult), [d])
        a = ph.act(lambda e: e.activation(out=eg[:, :], in_=lg[:, 0:8], func=AF.Exp, bias=sm[:, 0:1]), [d])
        d = ph.dve(lambda e: e.tensor_reduce(out=sm[:, 1:2], in_=eg[:, :], axis=AXX, op=ALU.add), [a])
        d = ph.dve(lambda e: e.reciprocal(out=sm[:, 2:3], in_=sm[:, 1:2]), [d])
        d = ph.dve(lambda e: e.tensor_tensor(out=tmp3[:, :, :], in0=lg[:, 8:72].rearrange("p (g j) -> p g j", g=8),
                                             in1=ohg[:, :].unsqueeze(2).to_broadcast([128, 8, 8]), op=ALU.mult), [d])
        d = ph.dve(lambda e: e.tensor_reduce(out=esel[:, :], in_=tmp3[:, :, :].rearrange("p g j -> p j g"), axis=AXX, op=ALU.add), [d])
        d = ph.dve(lambda e: e.max(out=em[:, :], in_=esel[:, :]), [d])
        d = ph.dve(lambda e: e.tensor_scalar(out=oh1[:, :], in0=esel[:, :], scalar1=em[:, 0:1], scalar2=None, op0=ALU.is_equal), [d])
        d = ph.dve(lambda e: e.tensor_scalar(out=oh2[:, :], in0=esel[:, :], scalar1=em[:, 1:2], scalar2=None, op0=ALU.is_equal), [d])
        d = ph.dve(lambda e: e.tensor_tensor(out=sm[:, 3:4], in0=em[:, 1:2], in1=em[:, 0:1], op=ALU.subtract), [d])
        a = ph.act(lambda e: e.activation(out=sm[:, 4:5], in_=sm[:, 3:4], func=AF.Exp), [d])
        d = ph.dve(lambda e: e.tensor_scalar(out=sm[:, 5:6], in0=sm[:, 4:5], scalar1=1.0, scalar2=None, op0=ALU.add), [a])
        d = ph.dve(lambda e: e.reciprocal(out=sm[:, 6:7], in_=sm[:, 5:6]), [d])
        d = ph.dve(lambda e, t=t: e.tensor_tensor(out=K.gates[:, t, 0:1], in0=sm[:, 6:7], in1=sm[:, 2:3], op=ALU.mult), [d])
        d = ph.dve(lambda e, t=t: e.tensor_tensor(out=K.gates[:, t, 1:2], in0=K.gates[:, t, 0:1], in1=sm[:, 4:5], op=ALU.mult), [d])
        for a_i, oh in enumerate((oh1, oh2)):
            d = ph.dve(lambda e, t=t, a_i=a_i, oh=oh: e.tensor_tensor(
                out=K.A12[:, t, a_i, :].rearrange("p (g j) -> p g j", g=8), in0=ohg[:, :].unsqueeze(2).to_broadcast([128, 8, 8]),
                in1=oh[:, :].unsqueeze(1).to_broadcast([128, 8, 8]), op=ALU.mult), [d])
        d = ph.dve(lambda e, t=t: e.tensor_tensor(out=K.Aall[:, t, :], in0=K.A12[:, t, 0, :], in1=K.A12[:, t, 1, :], op=ALU.add), [d])
    cnt = ph.sb("cnt", [128, 64], F32)
    tmpc = ph.sb("tmpc", [128, 64], F32)
    pe_ = ph.sb("pe_", [128, 4], F32)
    psC = psb[0]
    zt = ph.sb("zt", [128, D], BF16)
    tz = ph.pool(lambda e: e.memset(zt[:, :], 0.0))
    sem_z = ph.p.new_sem("zf")
    xe_v = Dm["s_xe"].rearrange("(e p) d -> p e d", p=128)
    tzf = None
    for e8 in range(8):
        tzf = ph.dma("sync", xe_v[:, e8 * 8:e8 * 8 + 8, :], zt[:, :].unsqueeze(1).to_broadcast([128, 8, D]), sem_z, waits=[tz])
    x2b = [ph.sb("x2b", [128, D], BF16) for _ in range(2)]
    sem_x = [ph.p.new_sem("x2b") for _ in range(2)]
    sem_s = [ph.p.new_sem("sct") for _ in range(2)]
    x_free = [None, None]
    for t in range(NT):
        tk = None
        for tp in range(t + 1):
            lhs = K.tri if tp == t else K.ones_bf
            tk = ph.pe(lambda e, tp=tp, lhs=lhs, t=t: e.matmul(psC[:, 0:64], lhs[:], K.Aall[:, tp, :], start=(tp == 0), stop=(tp == t)),
                       [d] if tp == 0 else (), sig=(tp == t))
        d = ph.dve(lambda e: e.tensor_copy(out=cnt[:, :], in_=psC[:, 0:64]), [tk, d])
        for a_i in range(2):
            d = ph.dve(lambda e, t=t, a_i=a_i: e.tensor_tensor(out=tmpc[:, :], in0=K.A12[:, t, a_i, :], in1=cnt[:, :], op=ALU.mult), [d])
            d = ph.dve(lambda e, a_i=a_i: e.tensor_reduce(out=pe_[:, 2 * a_i:2 * a_i + 1], in_=tmpc[:, :], axis=AXX, op=ALU.add), [d])
            d = ph.dve(lambda e, t=t, a_i=a_i: e.tensor_tensor(out=tmpc[:, :], in0=K.A12[:, t, a_i, :], in1=K.iota64[:, :], op=ALU.mult), [d])
            d = ph.dve(lambda e, a_i=a_i: e.tensor_reduce(out=pe_[:, 2 * a_i + 1:2 * a_i + 2], in_=tmpc[:, :], axis=AXX, op=ALU.add), [d])
            d = ph.dve(lambda e, a_i=a_i: e.scalar_tensor_tensor(out=pe_[:, 2 * a_i:2 * a_i + 1], in0=pe_[:, 2 * a_i + 1:2 * a_i + 2],
                                                                scalar=float(CAP), in1=pe_[:, 2 * a_i:2 * a_i + 1],
                                                                op0=ALU.mult, op1=ALU.add), [d])
            d = ph.dve(lambda e, a_i=a_i: e.tensor_scalar(out=pe_[:, 2 * a_i:2 * a_i + 1], in0=pe_[:, 2 * a_i:2 * a_i + 1], scalar1=0.0,
                                                         scalar2=float(NEXP * CAP - 1), op0=ALU.max, op1=ALU.min), [d])
            d = ph.dve(lambda e, t=t, a_i=a_i: e.tensor_copy(out=K.dest[:, t, a_i:a_i + 1], in_=pe_[:, 2 * a_i:2 * a_i + 1]), [d])
        par = t % 2
        tl = ph.dma("sync", x2b[par][:, :], Dm["s_x2b"][t * 128:(t + 1) * 128, :], sem_x[par], waits=[x_free[par]])
        for a_i in range(2):
            x_free[par] = ph.idma(out=Dm["s_xe"], in_=x2b[par][:, :], sem=sem_s[par], waits=[tl, d, tzf],
                                  out_off=K.dest[:, t, a_i:a_i + 1])


def phase_F2(ph, K, Dm):
    xring = Ring(ph, "xe", 2, [128, D], BF16, queue="sync")
    xs = Stream(xring, [[((lambda t: t[:, :]), Dm["s_xe"][e * CAP:(e + 1) * CAP, :])] for e in range(NEXP)])
    ring = Ring(ph, "wF", 8, [128, 4, 512], BF16)
    pieces = []
    for e in range(NEXP):
        wg = Dm["w_gate"][e].rearrange("(kc p) f -> p kc f", p=128)
        wu = Dm["w_up"][e].rearrange("(kc p) f -> p kc f", p=128)
        wd = Dm["w_down"][e].rearrange("(fc p) n -> p fc n", p=128)
        for kg in range(8):
            pieces.append([((lambda t: t[:, :, :]), wg[:, kg * 4:kg * 4 + 4, :])])
            pieces.append([((lambda t: t[:, :, :]), wu[:, kg * 4:kg * 4 + 4, :])])
        for nb in range(8):
            pieces.append([((lambda t: t[:, :, :]), wd[:, :, nb * 512:(nb + 1) * 512])])
    ws = Stream(ring, pieces)
    xeT = [ph.sb("xeT", [128, 32, 128], BF16) for _ in range(2)]
    sg = ph.sb("sg", [128, 512], F32)
    hh = ph.sb("hh", [128, 512], BF16)
    hT = ph.sb("hT", [128, 4, 128], BF16)
    ye = [ph.sb("ye", [128, D], F32) for _ in range(2)]
    sem_y = [ph.p.new_sem("yeo") for _ in range(2)]
    ye_free = [None, None]
    psT = [ph.ps("psTe", (128, 1024), BF16) for _ in range(2)]
    psG = ph.ps("psGe")
    psU = ph.ps("psUe")
    psD = [ph.ps("psDe") for _ in range(2)]
    psT_free = [None, None]
    psG_free = None
    psU_free = None
    psD_free = [None, None]
    xeT_free = [None, None]
    sg_free = None
    hh_free = None
    hT_free = None
    nT = 0
    pi = 0
    for e in range(NEXP):
        par = e % 2
        s, xe, xtok = xs.get(e)
        evs = []
        last_tr = None
        for c4 in range(8):
            bp = nT % 2
            nT += 1
            tk = None
            for q in range(4):
                c = c4 * 4 + q
                tk = ph.pe(lambda e_, bp=bp, q=q, c=c, xe=xe: e_.transpose(psT[bp][:, q * 128:(q + 1) * 128], xe[:, c * 128:(c + 1) * 128],
                                                                        K.ident_bf[:]),
                           [xtok, psT_free[bp]] if q == 0 else (), sig=(q == 3))
            src = psT[bp][:, 0:512].rearrange("p (a b) -> p a b", a=4)
            dst = xeT[par][:, c4 * 4:c4 * 4 + 4, :]
            if c4 % 2 == 0:
                ev = ph.act(lambda e_, src=src, dst=dst: e_.activation(out=dst, in_=src, func=AF.Copy), [tk, xeT_free[par]])
            else:
                ev = ph.dve(lambda e_, src=src, dst=dst: e_.tensor_copy(out=dst, in_=src), [tk, xeT_free[par]])
            psT_free[bp] = ev
            evs.append(ev)
            last_tr = tk
        xs.done(e, last_tr)
        tg = tu = None
        for kg in range(8):
            for which in range(2):
                s_, wt, wtok = ws.get(pi)
                ps_ = psG if which == 0 else psU
                fr = psG_free if which == 0 else psU_free
                tk = None
                for k4 in range(4):
                    kc = kg * 4 + k4
                    tk = ph.pe(lambda e_, ps_=ps_, kc=kc, k4=k4, wt=wt, par=par: e_.matmul(
                        ps_[:, :], xeT[par][:, kc, :], wt[:, k4, :], start=(kc == 0), stop=(kc == 31)),
                        ([wtok, fr] + evs) if kc == 0 else ([wtok] if k4 == 0 else ()), sig=(k4 == 3))
                ws.done(pi, tk)
                pi += 1
                if which == 0:
                    tg = tk
                else:
                    tu = tk
        xeT_free[par] = tu
        a = ph.act(lambda e_: e_.activation(out=sg[:, :], in_=psG[:, :], func=AF.Silu), [tg, sg_free])
        psG_free = a
        m = ph.dve(lambda e_: e_.tensor_tensor(out=hh[:, :], in0=sg[:, :], in1=psU[:, :], op=ALU.mult), [a, tu, hh_free])
        psU_free = m
        sg_free = m
        bp = nT % 2
        nT += 1
        tk = None
        for q in range(4):
            tk = ph.pe(lambda e_, bp=bp, q=q: e_.transpose(psT[bp][:, q * 128:(q + 1) * 128], hh[:, q * 128:(q + 1) * 128], K.ident_bf[:]),
                       [m, psT_free[bp]] if q == 0 else (), sig=(q == 3))
        hh_free = tk
        ev = ph.act(lambda e_, bp=bp: e_.activation(out=hT[:, :, :], in_=psT[bp][:, 0:512].rearrange("p (a b) -> p a b", a=4), func=AF.Copy),
                    [tk, hT_free])
        psT_free[bp] = ev
        evd = None
        ev6 = None
        last_dn = None
        for nb in range(8):
            s_, wt, wtok = ws.get(pi)
            bd = nb % 2
            tk = None
            for fc in range(4):
                tk = ph.pe(lambda e_, bd=bd, fc=fc, wt=wt: e_.matmul(psD[bd][:, :], hT[:, fc, :], wt[:, fc, :], start=(fc == 0), stop=(fc == 3)),
                           [wtok, ev, psD_free[bd]] if fc == 0 else (), sig=(fc == 3))
            ws.done(pi, tk)
            pi += 1
            dst = ye[par][:, nb * 512:(nb + 1) * 512]
            if nb % 2 == 0:
                evd = ph.act(lambda e_, bd=bd, dst=dst: e_.activation(out=dst, in_=psD[bd][:, :], func=AF.Copy), [tk, ye_free[par]])
            else:
                evd = ph.dve(lambda e_, bd=bd, dst=dst: e_.tensor_copy(out=dst, in_=psD[bd][:, :]), [tk, ye_free[par]])
            psD_free[bd] = evd
            if nb == 6:
                ev6 = evd
            last_dn = tk
        hT_free = last_dn
        ye_free[par] = ph.dma("sync", Dm["s_ye"][e * CAP:(e + 1) * CAP, :], ye[par][:, :], sem_y[par], waits=[evd, ev6])


def phase_F3(ph, K, Dm):
    x2 = [ph.sb("x2t", [128, D], F32) for _ in range(2)]
    r1 = [ph.sb("r1", [128, D], F32) for _ in range(2)]
    r2 = [ph.sb("r2", [128, D], F32) for _ in range(2)]
    acc = [ph.sb("acc", [128, D], F32) for _ in range(2)]
    sem_x = [ph.p.new_sem("f3x") for _ in range(2)]
    sem_1 = [ph.p.new_sem("f31") for _ in range(2)]
    sem_2 = [ph.p.new_sem("f32") for _ in range(2)]
    sem_o = [ph.p.new_sem("f3o") for _ in range(2)]
    in_free = [None, None]
    acc_free = [None, None]
    for t in range(NT):
        par = t % 2
        tx = ph.dma("sync", x2[par][:, :], Dm["s_x2"][t * 128:(t + 1) * 128, :], sem_x[par], waits=[in_free[par]])
        t1 = ph.idma(out=r1[par][:, :], in_=Dm["s_ye"], sem=sem_1[par], waits=[in_free[par]], in_off=K.dest[:, t, 0:1])
        t2 = ph.idma(out=r2[par][:, :], in_=Dm["s_ye"], sem=sem_2[par], waits=[in_free[par]], in_off=K.dest[:, t, 1:2])
        a = ph.act(lambda e, par=par: e.activation(out=acc[par][:, :], in_=x2[par][:, :], func=AF.Copy, scale=ALPHA), [tx, acc_free[par]])
        d = ph.dve(lambda e, par=par, t=t: e.scalar_tensor_tensor(out=acc[par][:, :], in0=r1[par][:, :], scalar=K.gates[:, t, 0:1],
                                                                in1=acc[par][:, :], op0=ALU.mult, op1=ALU.add), [a, t1])
        d = ph.dve(lambda e, par=par, t=t: e.scalar_tensor_tensor(out=acc[par][:, :], in0=r2[par][:, :], scalar=K.gates[:, t, 1:2],
                                                                in1=acc[par][:, :], op0=ALU.mult, op1=ALU.add), [d, t2])
        in_free[par] = d
        acc_free[par] = ph.dma("sync", Dm["s_r"][t * 128:(t + 1) * 128, :], acc[par][:, :], sem_o[par], waits=[d])


def phase_F4(ph, K, Dm):
    ln_phase(ph, K, Dm, "ln3_g", "ln3_b", "out")


PHASES[:] = [phase_C, phase_D1, phase_D2, phase_E1, phase_E2, phase_E3, phase_E4, phase_E5,
             phase_F1, phase_F2, phase_F3, phase_F4]
```

```python
import os
import math
from contextlib import ExitStack
import numpy as np
import ml_dtypes
import concourse.bass as bass
import concourse.mybir as mybir
from concourse.bass_utils import run_bass_kernel_spmd

F32 = mybir.dt.float32
BF16 = mybir.dt.bfloat16
I32 = mybir.dt.int32
AF = mybir.ActivationFunctionType
ALU = mybir.AluOpType

D = 4096
S = 4096
T = 1024
NT = 8
DIN = 5704
ALPHA = 2.0 ** 0.25
LN_EPS = 1e-5
NEXP = 64
CAP = 128
TWO_PI = 2.0 * math.pi
C1 = 6.28125
C2 = TWO_PI - C1
PI_SAFE = 3.1415925
NEG_BIG = -3.0e30
NEG_MID = -2.0e30
ENGS = ["tensor", "vector", "scalar", "gpsimd", "sync"]


class Sem:
    def __init__(self, h, name):
        self.h = h
        self.name = name
        self.count = 0


class Prog:
    def __init__(self, nc, es):
        self.nc = nc
        self.es = es
        self.nsem = 0
        self.pool = []
        self.pool_idx = 0
        self.eng_sem = {e: self._alloc_sem("pg_" + e) for e in ENGS}
        self.waited = {e: {} for e in ENGS}
        self.uid = 0

    def _alloc_sem(self, name):
        self.nsem += 1
        nm = "%s_%d" % (name, self.nsem)
        return Sem(self.es.enter_context(self.nc.semaphore(nm)), nm)

    def new_sem(self, name="s"):
        if self.pool_idx == len(self.pool):
            self.pool.append(self._alloc_sem("d"))
        sem = self.pool[self.pool_idx]
        self.pool_idx += 1
        return sem

    def name(self, base):
        self.uid += 1
        return "%s_%d" % (base, self.uid)


class Phase:
    def __init__(self, prog):
        self.p = prog
        self.nc = prog.nc
        self.ops = {e: [] for e in ENGS}
        self.dma_toks = {}
        self.es = None

    def sb(self, name, shape, dt):
        return self.es.enter_context(self.nc.sbuf_tensor(self.p.name(name), list(shape), dt))

    def ps(self, name, shape=(128, 512), dt=F32):
        return self.es.enter_context(self.nc.psum_tensor(self.p.name(name), list(shape), dt))

    def op(self, eng, fn, waits=(), sig=True):
        ws = tuple(w for w in waits if w is not None)
        if sig:
            s = self.p.eng_sem[eng]
            s.count += 1
            tok = (s, s.count)
            self.ops[eng].append((ws, fn, (s, 1)))
            return tok
        self.ops[eng].append((ws, fn, None))
        return None

    def pe(self, fn, waits=(), sig=True):
        return self.op("tensor", fn, waits, sig)

    def dve(self, fn, waits=(), sig=True):
        return self.op("vector", fn, waits, sig)

    def act(self, fn, waits=(), sig=True):
        return self.op("scalar", fn, waits, sig)

    def pool(self, fn, waits=(), sig=True):
        return self.op("gpsimd", fn, waits, sig)

    def dma(self, eng, out, in_, sem, waits=()):
        ws = tuple(w for w in waits if w is not None)
        sem.count += 16
        tok = (sem, sem.count)
        self.ops[eng].append((ws, (lambda e, o=out, i=in_: e.dma_start(out=o, in_=i)), (sem, 16)))
        self.dma_toks[sem.name] = tok
        return tok

    def idma(self, out, in_, sem, waits=(), out_off=None, in_off=None):
        ws = tuple(w for w in waits if w is not None)
        sem.count += 16
        tok = (sem, sem.count)

        def fn(e, o=out, i=in_, oo=out_off, io=in_off):
            return e.indirect_dma_start(
                out=o, out_offset=(bass.IndirectOffsetOnAxis(ap=oo, axis=0) if oo is not None else None),
                in_=i, in_offset=(bass.IndirectOffsetOnAxis(ap=io, axis=0) if io is not None else None))
        self.ops["gpsimd"].append((ws, fn, (sem, 16)))
        self.dma_toks[sem.name] = tok
        return tok

    def emit(self, block):
        fin = tuple(self.dma_toks.values())
        if fin:
            self.ops["sync"].append((fin, None, None))
        for eng in ENGS:
            ops = self.ops[eng]
            if not ops:
                continue
            waited = self.p.waited[eng]

            def body(e, ops=ops, waited=waited):
                for ws, fn, inc in ops:
                    for (sem, val) in ws:
                        if waited.get(sem.name, 0) < val:
                            e.wait_ge(sem.h, val)
                            waited[sem.name] = val
                    if fn is None:
                        continue
                    inst = fn(e)
                    if inc is not None:
                        inst.then_inc(inc[0].h, inc[1])
            getattr(block, eng)(body)


def run_phase(prog, fn):
    nc = prog.nc
    with ExitStack() as es:
        ph = Phase(prog)
        prog.pool_idx = 0
        ph.es = es
        block = es.enter_context(nc.Block())
        fn(ph)
        ph.emit(block)


class Ring:
    def __init__(self, ph, name, nslots, shape, dt, queue="gpsimd"):
        self.ph = ph
        self.n = nslots
        self.tiles = [ph.sb(name + str(i), shape, dt) for i in range(nslots)]
        self.sems = [ph.p.new_sem(name) for _ in range(nslots)]
        self.free = [None] * nslots
        self.k = 0
        self.queue = queue

    def load(self, parts):
        s = self.k % self.n
        self.k += 1
        tok = None
        for dst_fn, src in parts:
            tok = self.ph.dma(self.queue, dst_fn(self.tiles[s]), src, self.sems[s], waits=[self.free[s]])
        return s, self.tiles[s], tok

    def release(self, s, tok):
        self.free[s] = tok


class Stream:
    def __init__(self, ring, pieces):
        self.ring = ring
        self.pieces = pieces
        self.loaded = []

    def get(self, i):
        want = min(len(self.pieces), i + self.ring.n)
        while len(self.loaded) < want:
            self.loaded.append(self.ring.load(self.pieces[len(self.loaded)]))
        return self.loaded[i]

    def done(self, i, tok):
        self.ring.release(self.loaded[i][0], tok)


def rope_tables(ph, K, posi, n, tabs, tmp, waits):
    posf, arg, ki, kf, r = tmp
    t = ph.dve(lambda e: e.tensor_copy(out=posf[:, :n], in_=posi[:, :n]), waits)
    last_act = None
    for ti, tab in enumerate(tabs):
        fam = ti // 2
        is_cos = (ti % 2 == 0)
        inv = K.invs[:, 2 * fam:2 * fam + 1]
        sign = K.invs[:, 2 * fam + 1:2 * fam + 2]
        shift = (math.pi / 2.0) if is_cos else 0.0
        t = ph.dve(lambda e, inv=inv, shift=shift: e.tensor_scalar(
            out=arg[:, :n], in0=posf[:, :n], scalar1=inv, scalar2=shift, op0=ALU.mult, op1=ALU.add), [t, last_act])
        t = ph.dve(lambda e: e.tensor_scalar(
            out=ki[:, :n], in0=arg[:, :n], scalar1=1.0 / TWO_PI, scalar2=None, op0=ALU.mult), [t])
        t = ph.dve(lambda e: e.tensor_copy(out=kf[:, :n], in_=ki[:, :n]), [t])
        t = ph.dve(lambda e: e.scalar_tensor_tensor(
            out=r[:, :n], in0=kf[:, :n], scalar=-C1, in1=arg[:, :n], op0=ALU.mult, op1=ALU.add), [t, last_act])
        t = ph.dve(lambda e: e.scalar_tensor_tensor(
            out=r[:, :n], in0=kf[:, :n], scalar=-C2, in1=r[:, :n], op0=ALU.mult, op1=ALU.add), [t])
        t = ph.dve(lambda e: e.tensor_scalar(
            out=r[:, :n], in0=r[:, :n], scalar1=-PI_SAFE, scalar2=PI_SAFE, op0=ALU.max, op1=ALU.min), [t])
        if is_cos:
            last_act = ph.act(lambda e, tab=tab: e.activation(out=tab[:, :n], in_=r[:, :n], func=AF.Sin), [t])
        else:
            last_act = ph.act(lambda e, tab=tab, sign=sign: e.activation(
                out=tab[:, :n], in_=r[:, :n], func=AF.Sin, scale=sign), [t])
        t = last_act
    return last_act


def rope_fm(ph, src_ps, n, u, sw_ps, t1, t2, cosT, sinT, perm, out_ap, waits, view=None):
    if view is None:
        view = lambda ap: ap
    a = ph.act(lambda e: e.activation(out=u[:, :n], in_=src_ps, func=AF.Copy), waits)
    b = ph.pe(lambda e: e.matmul(sw_ps[:, :n], perm, u[:, :n], start=True, stop=True), [a])
    c = ph.dve(lambda e: e.tensor_tensor(out=t1[:, :n], in0=u[:, :n], in1=cosT, op=ALU.mult), [a])
    d = ph.dve(lambda e: e.tensor_tensor(out=t2[:, :n], in0=sw_ps[:, :n], in1=sinT, op=ALU.mult), [b, c])
    f = ph.dve(lambda e: e.tensor_tensor(out=out_ap, in0=view(t1[:, :n]), in1=view(t2[:, :n]), op=ALU.add), [d])
    return f, a


class Consts:
    pass


def phase_consts(ph, K, Dm):
    sem = ph.p.new_sem("c")
    for nm in ["ident_bf", "ident_f", "permH", "permI", "invs", "ones_bf", "tri", "iota64"]:
        ph.dma("sync", getattr(K, nm)[:], Dm[nm], sem)
    K.tok = ph.dma("sync", K.bias_r[:], Dm["b_r"].to_broadcast([128, 72]), sem)


def phase_A(ph, K, Dm):
    CH = 256
    NCH = S // CH
    w_in_v = Dm["w_in"].rearrange("(kc p) n -> p kc n", p=128)
    xT_v = Dm["xT_ctx"].rearrange("(kc p) t -> p kc t", p=128)
    Wk = ph.sb("Wk", [128, 32, 512], BF16)
    Wv = ph.sb("Wv", [128, 32, 512], BF16)
    Wki = ph.sb("Wki", [128, 32, 128], BF16)
    semw = ph.p.new_sem("wA")
    tw = None
    for kg in range(4):
        ks = slice(kg * 8, kg * 8 + 8)
        ph.dma("gpsimd", Wk[:, ks, :], w_in_v[:, ks, 4096:4608], semw)
        ph.dma("gpsimd", Wv[:, ks, :], w_in_v[:, ks, 4608:5120], semw)
        ph.dma("gpsimd", Wki[:, ks, 0:64], w_in_v[:, ks, 5632:5696], semw)
        tw = ph.dma("gpsimd", Wki[:, ks, 64:128], w_in_v[:, ks, 5632:5696], semw)
    xring = Ring(ph, "xc", 2, [128, 32, CH], BF16)
    pieces = []
    for c in range(NCH):
        pieces.append([((lambda t, kg=kg: t[:, kg * 8:kg * 8 + 8, :]), xT_v[:, kg * 8:kg * 8 + 8, c * CH:(c + 1) * CH])
                       for kg in range(4)])
    xs = Stream(xring, pieces)
    posi = [ph.sb("posi", [128, CH], I32) for _ in range(2)]
    sem_pos = [ph.p.new_sem("pos") for _ in range(2)]
    tmp = (ph.sb("posf", [128, CH], F32), ph.sb("arg", [128, CH], F32), ph.sb("ki", [128, CH], I32),
           ph.sb("kf", [128, CH], F32), ph.sb("r", [128, CH], F32))
    tabs = [tuple(ph.sb("tab", [128, CH], F32) for _ in range(4)) for _ in range(2)]
    psk = [ph.ps("psk") for _ in range(4)]
    psv = [ph.ps("psv") for _ in range(2)]
    pski = ph.ps("pski")
    pssw = ph.ps("pssw")
    u = ph.sb("u", [128, CH], BF16)
    t1 = ph.sb("t1", [128, CH], F32)
    t2 = ph.sb("t2", [128, CH], F32)
    kout = [ph.sb("kout", [128, 4, CH], BF16) for _ in range(2)]
    vout = [ph.sb("vout", [128, 2, 512], BF16) for _ in range(2)]
    kiout = [ph.sb("kiout", [128, CH], BF16) for _ in range(2)]
    sem_o = [ph.p.new_sem("oA") for _ in range(2)]
    out_tok = [None, None]
    tab_free = [None, None]
    psk_free = [None] * 4
    psv_free = [None] * 2
    pski_free = None
    s_v_v = Dm["s_v"].rearrange("(n p) c -> p n c", p=128)
    for c in range(NCH):
        par = c % 2
        s, xt, xtok = xs.get(c)
        tp = ph.dma("sync", posi[par][:], Dm["pos_ctx"][0:1, c * CH:(c + 1) * CH].to_broadcast([128, CH]),
                    sem_pos[par], waits=[tab_free[par]])
        ttab = rope_tables(ph, K, posi[par], CH, tabs[par], tmp, [tp, tab_free[par]])
        cosH, sinH, cosI, sinI = tabs[par]
        last_rope = None
        for g in range(4):
            tk = None
            for kc in range(32):
                tk = ph.pe(lambda e, g=g, kc=kc, xt=xt: e.matmul(
                    psk[g][:, :CH], Wk[:, kc, g * 128:(g + 1) * 128], xt[:, kc, :], start=(kc == 0), stop=(kc == 31)),
                    [tw, xtok, psk_free[g]] if kc == 0 else (), sig=(kc == 31))
            f, a = rope_fm(ph, psk[g][:, :CH], CH, u, pssw, t1, t2, cosH[:, :], sinH[:, :], K.permH[:],
                           kout[par][:, g, :], [tk, ttab, out_tok[par], last_rope])
            psk_free[g] = a
            last_rope = f
        tk = None
        for kc in range(32):
            tk = ph.pe(lambda e, kc=kc, xt=xt: e.matmul(
                pski[:, :CH], Wki[:, kc, :], xt[:, kc, :], start=(kc == 0), stop=(kc == 31)),
                [pski_free] if kc == 0 else (), sig=(kc == 31))
        f, a = rope_fm(ph, pski[:, :CH], CH, u, pssw, t1, t2, cosI[:, :], sinI[:, :], K.permI[:],
                       kiout[par][:, :], [tk, last_rope])
        pski_free = a
        last_rope = f
        tab_free[par] = f
        tv_ev = []
        for tt in range(2):
            tk = None
            for kc in range(32):
                tk = ph.pe(lambda e, tt=tt, kc=kc, xt=xt: e.matmul(
                    psv[tt][:, :], xt[:, kc, tt * 128:(tt + 1) * 128], Wv[:, kc, :], start=(kc == 0), stop=(kc == 31)),
                    [psv_free[tt]] if kc == 0 else (), sig=(kc == 31))
            ev = ph.act(lambda e, tt=tt, par=par: e.activation(out=vout[par][:, tt, :], in_=psv[tt][:, :], func=AF.Copy),
                        [tk, out_tok[par]])
            psv_free[tt] = ev
            tv_ev.append(ev)
            if tt == 1:
                xs.done(c, tk)
        ph.dma("sync", Dm["s_kT"][:, :, c * CH:(c + 1) * CH], kout[par][:, :, :], sem_o[par], waits=[last_rope])
        ph.dma("sync", Dm["s_ki2"][:, c * CH:(c + 1) * CH], kiout[par][:, :], sem_o[par], waits=[last_rope])
        out_tok[par] = ph.dma("sync", s_v_v[:, 2 * c:2 * c + 2, :], vout[par][:, :, :], sem_o[par], waits=tv_ev)


def phase_B(ph, K, Dm):
    TO = T + 128
    w_in_v = Dm["w_in"].rearrange("(kc p) n -> p kc n", p=128)
    xT_v = Dm["xT_own"].rearrange("(kc p) t -> p kc t", p=128)
    xo = ph.sb("xo", [128, 32, TO], BF16)
    semx = ph.p.new_sem("xo")
    tx = None
    for kg in range(8):
        tx = ph.dma("gpsimd", xo[:, kg * 4:kg * 4 + 4, :], xT_v[:, kg * 4:kg * 4 + 4, :], semx)
    posi = [ph.sb("posi", [128, 512], I32) for _ in range(2)]
    tmp = (ph.sb("posf", [128, 512], F32), ph.sb("arg", [128, 512], F32), ph.sb("ki", [128, 512], I32),
           ph.sb("kf", [128, 512], F32), ph.sb("r", [128, 512], F32))
    tabs = tuple(ph.sb("tab", [128, T], F32) for _ in range(4))
    ttab = None
    for hf in range(2):
        tp = ph.dma("sync", posi[hf][:], Dm["pos_own"][0:1, hf * 512:hf * 512 + 512].to_broadcast([128, 512]),
                    ph.p.new_sem("posB"))
        ttab = rope_tables(ph, K, posi[hf], 512, tuple(tb[:, hf * 512:hf * 512 + 512] for tb in tabs), tmp, [tp, ttab])
    cosH, sinH, cosI, sinI = tabs
    vp = ph.sb("vp", [128, 9, 2048], BF16)
    ring = Ring(ph, "wB", 4, [128, 8, 512], BF16)
    blocks = [("vp", nb, nb * 512, 512) for nb in range(4)] + [("vph", nb, nb * 512, 512) for nb in range(4)] + \
             [("q", nb, 2048 + nb * 512, 512) for nb in range(4)] + [("qi", 0, 5120, 512), ("wi", 0, 5696, 8)]
    pieces = []
    for (kind, nb, c0, nc_) in blocks:
        for kg in range(4):
            pieces.append([((lambda t, nc_=nc_: t[:, :, :nc_]), w_in_v[:, kg * 8:kg * 8 + 8, c0:c0 + nc_])])
    ws = Stream(ring, pieces)
    psb = [ph.ps("psB") for _ in range(8)]
    bank_free = [None] * 8
    u = ph.sb("u", [128, 512], BF16)
    t1 = ph.sb("t1", [128, 512], F32)
    t2 = ph.sb("t2", [128, 512], F32)
    qst = [ph.sb("qst", [128, 8, 512], BF16) for _ in range(2)]
    qist = [ph.sb("qist", [128, T], BF16) for _ in range(2)]
    sem_q = [ph.p.new_sem("qo") for _ in range(2)]
    q_free = [None, None]
    nq = 0
    last_rope = None
    vp_done = []
    for bi, (kind, nb, c0, nc_) in enumerate(blocks):
        last_tok = [None] * 8
        for kg in range(4):
            pi = bi * 4 + kg
            s, wt, wtok = ws.get(pi)
            tk = None
            for k8 in range(8):
                kc = kg * 8 + k8
                first = (kc == 0)
                lastk = (kc == 31)
                if kind in ("vp", "wi"):
                    for t in range(8):
                        tk = ph.pe(lambda e, t=t, kc=kc, k8=k8, wt=wt, nc_=nc_: e.matmul(
                            psb[t][:, :nc_], xo[:, kc, 128 + t * 128:256 + t * 128], wt[:, k8, :nc_],
                            start=(kc == 0), stop=(kc == 31)),
                            [wtok, tx, bank_free[t]] if first else ([wtok] if k8 == 0 and t == 0 else ()),
                            sig=(lastk or (k8 == 7 and t == 7)))
                        if lastk:
                            last_tok[t] = tk
                elif kind == "vph":
                    tk = ph.pe(lambda e, kc=kc, k8=k8, wt=wt: e.matmul(
                        psb[0][:, :], xo[:, kc, 0:128], wt[:, k8, :], start=(kc == 0), stop=(kc == 31)),
                        [wtok, tx, bank_free[0]] if first else ([wtok] if k8 == 0 else ()), sig=(lastk or k8 == 7))
                    if lastk:
                        last_tok[0] = tk
                else:
                    for ch in range(4):
                        for hf in range(2):
                            b = ch * 2 + hf
                            tk = ph.pe(lambda e, ch=ch, hf=hf, b=b, kc=kc, k8=k8, wt=wt: e.matmul(
                                psb[b][:, :], wt[:, k8, ch * 128:(ch + 1) * 128], xo[:, kc, 128 + hf * 512:640 + hf * 512],
                                start=(kc == 0), stop=(kc == 31)),
                                [wtok, tx, bank_free[b]] if first else ([wtok] if k8 == 0 and b == 0 else ()),
                                sig=(lastk or (k8 == 7 and b == 7)))
                            if lastk:
                                last_tok[b] = tk
            ws.done(pi, tk)
        if kind == "vp":
            for t in range(8):
                dst = vp[:, 1 + t, c0:c0 + 512]
                if t % 2 == 0:
                    ev = ph.act(lambda e, t=t, dst=dst: e.activation(out=dst, in_=psb[t][:, :], func=AF.Copy), [last_tok[t]])
                else:
                    ev = ph.dve(lambda e, t=t, dst=dst: e.tensor_copy(out=dst, in_=psb[t][:, :]), [last_tok[t]])
                bank_free[t] = ev
                vp_done.append(ev)
        elif kind == "vph":
            ev = ph.act(lambda e, c0=c0: e.activation(out=vp[:, 0, c0:c0 + 512], in_=psb[0][:, :], func=AF.Copy), [last_tok[0]])
            bank_free[0] = ev
            vp_done.append(ev)
        elif kind == "wi":
            sc = (8 ** -0.5) * (64 ** -0.5)
            for t in range(8):
                b_ = ph.dve(lambda e, t=t: e.tensor_scalar(out=K.wsgn[:, t, :], in0=psb[t][:, 0:8], scalar1=0.0, scalar2=2.0,
                                                          op0=ALU.is_gt, op1=ALU.mult), [last_tok[t]])
                c_ = ph.dve(lambda e, t=t: e.tensor_scalar(out=K.wsgn[:, t, :], in0=K.wsgn[:, t, :], scalar1=-1.0, scalar2=None,
                                                          op0=ALU.add), [b_])
                c_ = ph.dve(lambda e, t=t: e.scalar_tensor_tensor(out=K.wabs[:, t, :], in0=psb[t][:, 0:8], scalar=sc,
                                                                 in1=K.wsgn[:, t, :], op0=ALU.mult, op1=ALU.mult), [c_])
                bank_free[t] = c_
        else:
            par = nq % 2
            nq += 1
            for ch in range(4):
                for hf in range(2):
                    b = ch * 2 + hf
                    if kind == "q":
                        out_ap = qst[par][:, hf * 4:hf * 4 + 4, ch * 128:(ch + 1) * 128]
                        f, a = rope_fm(ph, psb[b][:, :], 512, u, psb[b], t1, t2, cosH[:, hf * 512:hf * 512 + 512],
                                       sinH[:, hf * 512:hf * 512 + 512], K.permH[:], out_ap,
                                       [last_tok[b], ttab, q_free[par], last_rope],
                                       view=lambda ap: ap.rearrange("p (a b) -> p a b", a=4))
                    else:
                        out_ap = qist[par][:, hf * 512:hf * 512 + 512]
                        f, a = rope_fm(ph, psb[b][:, :], 512, u, psb[b], t1, t2, cosI[:, hf * 512:hf * 512 + 512],
                                       sinI[:, hf * 512:hf * 512 + 512], K.permI[:], out_ap,
                                       [last_tok[b], ttab, q_free[par], last_rope])
                    bank_free[b] = f
                    last_rope = f
                if kind == "qi" :
                    if True:
                        q_free[par] = ph.dma("sync", Dm["s_qi2"][:, ch, :], qist[par][:, :], sem_q[par], waits=[last_rope])
                        par = nq % 2
                        nq += 1
            if kind == "q":
                q_free[par] = ph.dma("sync", Dm["s_qT"][:, :, nb * 512:(nb + 1) * 512], qst[par][:, :, :], sem_q[par],
                                     waits=[last_rope])
    ph.dma("sync", Dm["s_vp"], vp[:], semx, waits=vp_done)


def phase_B2(ph, K, Dm):
    semx = ph.p.new_sem("b2")
    band = ph.sb("band", [128, 4, 4, 128], BF16)
    pw = ph.sb("pw", [128, 4, 4, 512], BF16)
    pscale = ph.sb("pscale", [128, 16], F32)
    vp = ph.sb("vp", [128, 9, 2048], BF16)
    ph.dma("sync", band[:], Dm["band"], semx)
    ph.dma("sync", pscale[:], Dm["pool_scale_c"], semx)
    ph.dma("sync", vp[:], Dm["s_vp"], semx)
    tpw = None
    for g in range(4):
        tpw = ph.dma("gpsimd", pw[:, g, :, :], Dm["pool_w"][g].rearrange("(cc p) d -> p cc d", p=128), semx)
    vp_done = [tpw]
    psb = [ph.ps("psB2") for _ in range(4)]
    bank_free = [None] * 4
    pl = [ph.sb("pl", [128, 512], BF16) for _ in range(4)]
    atst = [ph.sb("atst", [128, 4, 512], BF16) for _ in range(2)]
    sem_at = [ph.p.new_sem("at") for _ in range(2)]
    at_free = [None, None]
    pl_free = [None] * 4
    it = 0
    for g in range(4):
        for hf in range(2):
            par = it % 2
            it += 1
            pl_tok = []
            for cc in range(4):
                bank = cc % 2
                tk = None
                for i in range(4):
                    tile = hf * 4 + i
                    kA, kB = (2, 3) if tile == 0 else (0, 1)
                    col = g * 512 + cc * 128
                    ph.pe(lambda e, bank=bank, i=i, tile=tile, col=col, kA=kA, g=g: e.matmul(
                        psb[bank][:, i * 128:(i + 1) * 128], vp[:, 1 + tile, col:col + 128], band[:, g, kA, :],
                        start=True, stop=False), (vp_done + [bank_free[bank], tpw]) if i == 0 else (), sig=False)
                    tk = ph.pe(lambda e, bank=bank, i=i, tile=tile, col=col, kB=kB, g=g: e.matmul(
                        psb[bank][:, i * 128:(i + 1) * 128], vp[:, tile, col:col + 128], band[:, g, kB, :],
                        start=False, stop=True), (), sig=(i == 3))
                ev = ph.act(lambda e, bank=bank, cc=cc: e.activation(out=pl[cc][:, :], in_=psb[bank][:, :], func=AF.Copy),
                            [tk, pl_free[cc]])
                bank_free[bank] = ev
                pl_tok.append(ev)
            last_m = None
            for dc in range(4):
                bank = 2 + dc % 2
                tk = None
                for cc in range(4):
                    tk = ph.pe(lambda e, bank=bank, cc=cc, dc=dc, g=g: e.matmul(
                        psb[bank][:, :], pw[:, g, cc, dc * 128:(dc + 1) * 128], pl[cc][:, :], start=(cc == 0), stop=(cc == 3)),
                        (pl_tok + [bank_free[bank]]) if cc == 0 else (), sig=(cc == 3))
                ev = ph.act(lambda e, bank=bank, dc=dc, g=g, par=par: e.activation(
                    out=atst[par][:, dc, :], in_=psb[bank][:, :], func=AF.Identity, scale=pscale[:, g * 4 + dc:g * 4 + dc + 1]),
                    [tk, at_free[par]])
                bank_free[bank] = ev
                last_m = tk
            for cc in range(4):
                pl_free[cc] = last_m
            at_free[par] = ph.dma("sync", Dm["s_AT"][:, g * 4:g * 4 + 4, hf * 512:hf * 512 + 512], atst[par][:, :, :],
                                  sem_at[par], waits=[ev])


PHASES = []
NCORES = int(os.environ.get("K_NCORES", "4"))
NQ = 8 // NCORES
MOE_FUSED = os.environ.get("K_MOE", "1") == "1"


def build(stop=99, dbg=()):
    nc = bass.Bass("TRN2", target_bir_lowering=False)
    Dm = {}

    def din(name, shape, dt=F32):
        Dm[name] = nc.dram_tensor(name, list(shape), dt, kind="ExternalInput").ap()

    def dscr(name, shape, dt):
        kind = "ExternalOutput" if name in dbg else "Internal"
        Dm[name] = nc.dram_tensor(name, list(shape), dt, kind=kind).ap()

    din("xT_ctx", [D, S]); din("memT", [D, 256]); din("pos_ctx", [1, S], I32)
    for j in range(NQ):
        din("xT_own_%d" % j, [D, T + 128]); din("x_own_%d" % j, [T, D]); din("pos_own_%d" % j, [1, T], I32)
        din("cbias_%d" % j, [T, S]); din("band_%d" % j, [128, 4, 4, 128], BF16)
    din("ident_bf", [128, 128], BF16); din("ident_f", [128, 128]); din("permH", [128, 128], BF16)
    din("permI", [128, 128], BF16); din("invs", [128, 4]); din("ones_bf", [128, 128], BF16)
    din("tri", [128, 128], BF16); din("iota64", [128, 64])
    din("w_in", [D, DIN]); din("pool_w", [4, 512, 512]); din("pool_scale_c", [128, 16]); din("w_o", [D, D])
    for nm in ["ln1_g", "ln1_b", "ln2_g", "ln2_b", "ln3_g", "ln3_b"]:
        din(nm, [1, D])
    din("w_mq", [D, D]); din("w_mk", [D, D]); din("w_mv", [D, D]); din("w_mo", [D, D])
    din("w_r", [D, 72]); din("b_r", [1, 72])
    if MOE_FUSED:
        din("w_gate", [NEXP, D, 512]); din("w_up", [NEXP, D, 512]); din("w_down", [NEXP, 512, D])
    dscr("s_kT", [128, 4, S], BF16); dscr("s_v", [S, 512], BF16); dscr("s_ki2", [128, S], BF16)
    dscr("s_qT", [128, 8, 2048], BF16); dscr("s_qi2", [128, 4, T], BF16); dscr("s_AT", [128, 32, T], BF16); dscr("s_vp", [128, 9, 2048], BF16)
    dscr("s_r", [T, D], F32); dscr("s_x1", [T, D], F32); dscr("s_x1T", [128, 32, T], BF16)
    dscr("s_qmT", [128, 32, T], BF16); dscr("s_kmT", [128, 32, 256], BF16); dscr("s_vm", [128, 2, D], BF16); dscr("s_oT", [128, 32, T], BF16); dscr("s_x2T", [128, 32, T], BF16); dscr("s_x2", [T, D], F32); dscr("s_x2b", [T, D], BF16)
    dscr("s_xe", [NEXP * CAP, D], BF16); dscr("s_ye", [NEXP * CAP, D], F32)
    dscr("s_dbg", [128, 512], F32); dscr("s_dbgi", [128, 16], I32)
    out_full = nc.dram_tensor("out", [NQ * T, D], F32, kind="ExternalOutput").ap()

    with ExitStack() as es:
        prog = Prog(nc, es)
        K = Consts()

        def psb(name, shape, dt):
            return es.enter_context(nc.sbuf_tensor(name, list(shape), dt))
        K.ident_bf = psb("ident_bf_s", [128, 128], BF16); K.ident_f = psb("ident_f_s", [128, 128], F32)
        K.permH = psb("permH_s", [128, 128], BF16); K.permI = psb("permI_s", [128, 128], BF16)
        K.invs = psb("invs_s", [128, 4], F32); K.ones_bf = psb("ones_s", [128, 128], BF16)
        K.tri = psb("tri_s", [128, 128], BF16); K.iota64 = psb("iota_s", [128, 64], F32)
        K.bias_r = psb("bias_r_s", [128, 72], F32)
        K.wabs = psb("wabs_s", [128, 8, 8], F32); K.wsgn = psb("wsgn_s", [128, 8, 8], F32)
        K.Aall = psb("Aall_s", [128, 8, 64], BF16)
        K.A12 = psb("A12_s", [128, 8, 2, 64], F32)
        K.gates = psb("gates_s", [128, 8, 2], F32)
        K.dest = psb("dest_s", [128, 8, 2], I32)
        run_phase(prog, lambda ph: phase_consts(ph, K, Dm))
        run_phase(prog, lambda ph: phase_A(ph, K, Dm))
        plist = [phase_B, phase_B2] + PHASES
        for j in range(NQ):
            for nm in ("xT_own", "x_own", "pos_own", "cbias", "band"):
                Dm[nm] = Dm["%s_%d" % (nm, j)]
            Dm["out"] = out_full[j * T:(j + 1) * T, :]
            for i, fn in enumerate(plist):
                if i > stop:
                    break
                run_phase(prog, lambda ph, fn=fn: fn(ph, K, Dm))
        for nm in ("xT_own", "x_own", "pos_own", "cbias", "band", "out"):
            Dm.pop(nm, None)
        if dbg:
            def dbgf(ph):
                sem = ph.p.new_sem("dbg")
                ph.dma("sync", Dm["s_dbg"][:, 0:64], K.wabs[:].rearrange("p a b -> p (a b)"), sem)
                ph.dma("sync", Dm["s_dbg"][:, 64:128], K.wsgn[:].rearrange("p a b -> p (a b)"), sem)
                ph.dma("sync", Dm["s_dbg"][:, 128:144], K.gates[:].rearrange("p a b -> p (a b)"), sem)
                ph.dma("sync", Dm["s_dbgi"][:, :], K.dest[:].rearrange("p a b -> p (a b)"), sem)
            run_phase(prog, dbgf)
    nc._in_names = [k for k in Dm if not k.startswith("s_") and k != "out"]
    return nc


def _bf(a):
    return np.ascontiguousarray(a).astype(ml_dtypes.bfloat16)


def host_consts():
    c = {}
    c["ident_bf"] = _bf(np.eye(128, dtype=np.float32))
    c["ident_f"] = np.eye(128, dtype=np.float32)
    m = np.arange(128)
    pH = np.zeros((128, 128), np.float32); pH[(m + 64) % 128, m] = 1.0
    pI = np.zeros((128, 128), np.float32); pI[64 * (m // 64) + ((m % 64) + 32) % 64, m] = 1.0
    c["permH"] = _bf(pH); c["permI"] = _bf(pI)
    invH = (1.0 / (np.float32(10000.0) ** (np.arange(0, 128, 2, dtype=np.float32) / np.float32(128)))).astype(np.float32)
    invI = (1.0 / (np.float32(10000.0) ** (np.arange(0, 64, 2, dtype=np.float32) / np.float32(64)))).astype(np.float32)
    invs = np.zeros((128, 4), np.float32)
    invs[:, 0] = invH[m % 64]; invs[:, 1] = np.where(m < 64, -1.0, 1.0)
    invs[:, 2] = invI[m % 32]; invs[:, 3] = np.where((m % 64) < 32, -1.0, 1.0)
    c["invs"] = invs
    c["ones_bf"] = _bf(np.ones((128, 128), np.float32))
    c["tri"] = _bf((m[:, None] < m[None, :]).astype(np.float32))
    c["iota64"] = np.tile(np.arange(64, dtype=np.float32)[None, :], (128, 1))
    return c


def host_band(first_of_seq):
    band = np.zeros((128, 4, 4, 128), np.float32)
    tp = np.arange(128)[:, None]; t = np.arange(128)[None, :]
    for g, w in enumerate((2, 4, 8, 16)):
        inwin = (tp <= t) & (tp >= t - w + 1)
        PA = inwin * (1.0 / w) - (tp == t)
        PB = ((tp - 128) >= (t - w + 1)) * (1.0 / w)
        cnt = np.minimum(t + 1, w).astype(np.float32)
        PA0 = inwin / cnt - (tp == t)
        band[:, g, 0] = PA; band[:, g, 1] = PB
        band[:, g, 2] = PA0 if first_of_seq else PA
        band[:, g, 3] = 0.0 if first_of_seq else PB
    return _bf(band)


_NC_CACHE = {}


def kernel(**inp):
    stop = int(os.environ.get("K_STOP", "99"))
    dbg = tuple(x for x in os.environ.get("K_DBG", "").split(",") if x)
    key = (stop, dbg)
    if key not in _NC_CACHE:
        _NC_CACHE[key] = build(stop, dbg)
    nc = _NC_CACHE[key]
    x = np.asarray(inp["x"], np.float32); mem = np.asarray(inp["mem"], np.float32)
    pos = np.asarray(inp["positions"]).astype(np.int32)
    shared = host_consts()
    g = lambda k: np.ascontiguousarray(np.asarray(inp[k], np.float32)[0])
    shared["w_in"] = g("w_in"); shared["pool_w"] = g("pool_w")
    shared["pool_scale_c"] = np.ascontiguousarray(g("pool_scale").reshape(16, 128).T)
    shared["w_o"] = g("w_o")
    for nm in ["ln1_g", "ln1_b", "ln2_g", "ln2_b", "ln3_g", "ln3_b"]:
        shared[nm] = g(nm).reshape(1, D)
    for nm in ["w_mq", "w_mk", "w_mv", "w_mo", "w_gate", "w_up", "w_down"]:
        shared[nm] = g(nm)
    shared["w_r"] = np.ascontiguousarray(np.concatenate([g("w_group_router"), g("w_expert_router")], axis=1))
    shared["b_r"] = np.concatenate([g("b_group_router"), g("b_expert_router")]).reshape(1, 72)
    xT = [np.ascontiguousarray(x[b].T) for b in range(2)]
    memT = [np.ascontiguousarray(mem[b].T) for b in range(2)]
    bands = {True: host_band(True), False: host_band(False)}
    in_maps = []
    kidx = np.arange(S)[None, :]
    cpb = NCORES // 2 if NCORES >= 2 else 1
    for c in range(NCORES):
        b = c // cpb
        m = dict(shared)
        m["xT_ctx"] = xT[b]
        m["memT"] = memT[b]
        m["pos_ctx"] = np.ascontiguousarray(pos[b].reshape(1, S))
        for jl in range(NQ):
            j = (c % cpb) * NQ + jl
            own = np.zeros((D, T + 128), np.float32)
            own[:, 128:] = xT[b][:, j * T:(j + 1) * T]
            if j > 0:
                own[:, :128] = xT[b][:, j * T - 128:j * T]
            m["xT_own_%d" % jl] = own
            m["x_own_%d" % jl] = np.ascontiguousarray(x[b, j * T:(j + 1) * T])
            m["pos_own_%d" % jl] = np.ascontiguousarray(pos[b, j * T:(j + 1) * T].reshape(1, T))
            qidx = (j * T + np.arange(T))[:, None]
            m["cbias_%d" % jl] = np.where(kidx <= qidx, np.float32(0.0), np.float32(NEG_BIG)).astype(np.float32)
            m["band_%d" % jl] = bands[j == 0]
        in_maps.append({k: m[k] for k in nc._in_names})
    res = run_bass_kernel_spmd(nc, in_maps, core_ids=list(range(NCORES)))
    kernel.last = res
    out = np.zeros((2, S, D), np.float32)
    for c in range(NCORES):
        b = c // cpb
        j0 = (c % cpb) * NQ
        out[b, j0 * T:(j0 + NQ) * T] = np.asarray(res.results[c]["out"])
    return out

def phase_C(ph, K, Dm):
    sem_l = ph.p.new_sem("lc")
    kT = ph.sb("kT", [128, 4, S], BF16)
    va = ph.sb("va", [128, 32, 4, 130], BF16)
    ki2 = ph.sb("ki2", [128, S], BF16)
    qT = ph.sb("qT", [128, 8, 2048], BF16)
    qi2 = ph.sb("qi2", [128, 4, T], BF16)
    for g in range(4):
        ph.dma("sync", kT[:, g, :], Dm["s_kT"][:, g, :], sem_l)
        ph.dma("sync", va[:, :, g, 0:128], Dm["s_v"][:, g * 128:(g + 1) * 128].rearrange("(n p) d -> p n d", p=128), sem_l)
    ph.dma("sync", ki2[:], Dm["s_ki2"], sem_l)
    ph.dma("sync", qT[:], Dm["s_qT"], sem_l)
    tl = ph.dma("sync", qi2[:], Dm["s_qi2"], sem_l)
    tones = ph.dve(lambda e: e.memset(va[:, :, :, 128:130], 1.0))
    sc = ph.sb("sc", [128, S], F32)
    work = ph.sb("work", [128, S], F32)
    rl = [ph.sb("rl", [128, 512], F32) for _ in range(2)]
    mask = ph.sb("mask", [128, S], BF16)
    mT = ph.sb("mT", [128, 32, 128], BF16)
    pT = [ph.sb("pT", [128, 512], BF16) for _ in range(3)]
    osb = ph.sb("osb", [128, 2048], BF16)
    atT = ph.sb("atT", [128, 16, 128], BF16)
    m8 = ph.sb("m8", [128, 8], F32)
    rec = ph.sb("rec", [128, 4], F32)
    psS = [ph.ps("psS") for _ in range(2)]
    psT = ph.ps("psT", (128, 1024), BF16)
    psO = [ph.ps("psO") for _ in range(4)]
    sem_b = ph.p.new_sem("cb")
    sem_at = ph.p.new_sem("cat")
    psS_free = [None, None]
    psT_free = None
    psO_free = [None] * 4
    rl_free = [None, None]
    pT_free = [None] * 3
    sc_free = None
    mT_free = None
    osb_free = None
    atT_free = None
    nS = 0
    nP = 0
    scale = 128 ** -0.5
    for i in range(NT):
        tb = ph.dma("sync", sc[:], Dm["cbias"][i * 128:(i + 1) * 128, :], sem_b, waits=[sc_free])
        nR = 0
        last_acc = None
        for kb in range(8):
            acc = tb
            for h in range(8):
                par = nS % 2
                nS += 1
                lo = (h % 2) * 64
                tk = ph.pe(lambda e, par=par, lo=lo, h=h, kb=kb, i=i: e.matmul(
                    psS[par][:, :], qi2[lo:lo + 64, h // 2, i * 128:(i + 1) * 128], ki2[lo:lo + 64, kb * 512:(kb + 1) * 512],
                    start=True, stop=True), [tl, psS_free[par]])
                rp = nR % 2
                nR += 1
                a = ph.act(lambda e, par=par, rp=rp, h=h, i=i: e.activation(
                    out=rl[rp][:, :], in_=psS[par][:, :], func=AF.Relu, scale=K.wabs[:, i, h:h + 1]), [tk, rl_free[rp]])
                psS_free[par] = a
                acc = ph.dve(lambda e, rp=rp, h=h, i=i, kb=kb: e.scalar_tensor_tensor(
                    out=sc[:, kb * 512:(kb + 1) * 512], in0=rl[rp][:, :], scalar=K.wsgn[:, i, h:h + 1],
                    in1=sc[:, kb * 512:(kb + 1) * 512], op0=ALU.mult, op1=ALU.add), [a, acc])
                rl_free[rp] = acc
            last_acc = acc
        t = ph.dve(lambda e: e.max(out=m8[:, :], in_=sc[:, :]), [last_acc, mT_free])
        t = ph.dve(lambda e: e.match_replace(out=work[:, :], in_to_replace=m8[:, :], in_values=sc[:, :], imm_value=NEG_MID), [t])
        for it in range(31):
            t = ph.dve(lambda e: e.max(out=m8[:, :], in_=work[:, :]), [t])
            if it < 30:
                t = ph.dve(lambda e: e.match_replace(out=work[:, :], in_to_replace=m8[:, :], in_values=work[:, :],
                                                     imm_value=NEG_MID), [t])
        tm = ph.dve(lambda e: e.tensor_scalar(out=mask[:, :], in0=sc[:, :], scalar1=m8[:, 7:8], scalar2=None, op0=ALU.is_ge), [t])
        sc_free = tm
        evs = []
        for k4 in range(8):
            tk = None
            for q in range(4):
                tk = ph.pe(lambda e, k4=k4, q=q: e.transpose(psT[:, q * 128:(q + 1) * 128], mask[:, (k4 * 4 + q) * 128:(k4 * 4 + q + 1) * 128],
                                                          K.ident_bf[:]), [tm, psT_free] if q == 0 else (), sig=(q == 3))
            ev = ph.act(lambda e, k4=k4: e.activation(out=mT[:, k4 * 4:k4 * 4 + 4, :],
                                                     in_=psT[:, 0:512].rearrange("p (a b) -> p a b", a=4), func=AF.Copy),
                        [tk, mT_free])
            psT_free = ev
            evs.append(ev)
        tmT = evs[-1]
        last_pool = None
        for g in range(4):
            for kt in range(32):
                par = nS % 2
                nS += 1
                tk = ph.pe(lambda e, par=par, g=g, kt=kt, i=i: e.matmul(
                    psS[par][:, :], kT[:, g, kt * 128:(kt + 1) * 128], qT[:, i, g * 512:(g + 1) * 512], start=True, stop=True),
                    [tl, psS_free[par]])
                p3 = nP % 3
                nP += 1
                a = ph.act(lambda e, par=par, p3=p3: e.activation(out=pT[p3][:, :], in_=psS[par][:, :], func=AF.Exp, scale=scale),
                           [tk, pT_free[p3]])
                psS_free[par] = a
                m = ph.pool(lambda e, p3=p3, kt=kt: e.tensor_tensor(
                    out=pT[p3][:, :].rearrange("p (a b) -> p a b", a=4), in0=pT[p3][:, :].rearrange("p (a b) -> p a b", a=4),
                    in1=mT[:, kt, :].unsqueeze(1).to_broadcast([128, 4, 128]), op=ALU.mult), [a, tmT])
                last_pool = m
                for hh in range(4):
                    tk = ph.pe(lambda e, hh=hh, p3=p3, kt=kt, g=g: e.matmul(
                        psO[hh][:, 0:130], pT[p3][:, hh * 128:(hh + 1) * 128], va[:, kt, g, :], start=(kt == 0), stop=(kt == 31)),
                        [m, tones, psO_free[hh]] if (hh == 0 or kt == 0) else (), sig=(hh == 3))
                pT_free[p3] = tk
            tr = ph.dve(lambda e: e.tensor_copy(out=rec[:, :], in_=rec[:, :]), [tk], sig=False) if False else None
            for hh in range(4):
                r1 = ph.dve(lambda e, hh=hh: e.reciprocal(out=rec[:, hh:hh + 1], in_=psO[hh][:, 128:129]), [tk, osb_free])
                r2 = ph.dve(lambda e, hh=hh, g=g: e.tensor_scalar(
                    out=osb[:, (4 * g + hh) * 128:(4 * g + hh + 1) * 128], in0=psO[hh][:, 0:128], scalar1=rec[:, hh:hh + 1],
                    scalar2=None, op0=ALU.mult), [r1])
                psO_free[hh] = r2
            last_o = r2
        mT_free = last_pool
        for hq in range(4):
            tk = None
            for q in range(4):
                h = hq * 4 + q
                tk = ph.pe(lambda e, q=q, h=h: e.transpose(psT[:, q * 128:(q + 1) * 128], osb[:, h * 128:(h + 1) * 128], K.ident_bf[:]),
                           [last_o, psT_free] if q == 0 else (), sig=(q == 3))
            ev = ph.act(lambda e, hq=hq: e.activation(out=atT[:, hq * 4:hq * 4 + 4, :],
                                                     in_=psT[:, 0:512].rearrange("p (a b) -> p a b", a=4), func=AF.Copy),
                        [tk, atT_free])
            psT_free = ev
        osb_free = tk
        atT_free = ph.dma("sync", Dm["s_AT"][:, 16:32, i * 128:(i + 1) * 128], atT[:, :, :], sem_at, waits=[ev])


def gemm_resid(ph, K, Dm, at_name, w_name, xres_name):
    sem_l = ph.p.new_sem("gl")
    AT = ph.sb("AT", [128, 32, T], BF16)
    tA = None
    for kg in range(4):
        tA = ph.dma("sync", AT[:, kg * 8:kg * 8 + 8, :], Dm[at_name][:, kg * 8:kg * 8 + 8, :], sem_l)
    w_v = Dm[w_name].rearrange("(kc p) n -> p kc n", p=128)
    x_v = Dm[xres_name].rearrange("(t p) c -> p t c", p=128)
    r_v = Dm["s_r"].rearrange("(t p) c -> p t c", p=128)
    ring = Ring(ph, "wG", 4, [128, 8, 512], BF16)
    pieces = [[((lambda t: t[:, :, :]), w_v[:, kg * 8:kg * 8 + 8, nb * 512:(nb + 1) * 512])] for nb in range(8) for kg in range(4)]
    ws = Stream(ring, pieces)
    xring = Ring(ph, "xb", 2, [128, 8, 512], F32, queue="sync")
    xs = Stream(xring, [[((lambda t: t[:, :, :]), x_v[:, :, nb * 512:(nb + 1) * 512])] for nb in range(8)])
    rb = [ph.sb("rb", [128, 8, 512], F32) for _ in range(2)]
    sem_r = [ph.p.new_sem("ro") for _ in range(2)]
    rb_free = [None, None]
    psb = [ph.ps("psG") for _ in range(8)]
    bank_free = [None] * 8
    for nb in range(8):
        last_tok = [None] * 8
        for kg in range(4):
            pi = nb * 4 + kg
            s, wt, wtok = ws.get(pi)
            tk = None
            for k8 in range(8):
                kc = kg * 8 + k8
                for t in range(8):
                    tk = ph.pe(lambda e, t=t, kc=kc, k8=k8, wt=wt: e.matmul(
                        psb[t][:, :], AT[:, kc, t * 128:(t + 1) * 128], wt[:, k8, :], start=(kc == 0), stop=(kc == 31)),
                        [wtok, tA, bank_free[t]] if kc == 0 else ([wtok] if k8 == 0 and t == 0 else ()),
                        sig=(kc == 31 or (k8 == 7 and t == 7)))
                    if kc == 31:
                        last_tok[t] = tk
            ws.done(pi, tk)
        par = nb % 2
        sx, xt, xtok = xs.get(nb)
        ev = None
        for t in range(8):
            ev = ph.dve(lambda e, t=t, xt=xt, par=par: e.scalar_tensor_tensor(
                out=rb[par][:, t, :], in0=xt[:, t, :], scalar=ALPHA, in1=psb[t][:, :], op0=ALU.mult, op1=ALU.add),
                [last_tok[t], xtok, rb_free[par]])
            bank_free[t] = ev
        xs.done(nb, ev)
        rb_free[par] = ph.dma("sync", r_v[:, :, nb * 512:(nb + 1) * 512], rb[par][:, :, :], sem_r[par], waits=[ev])


def ln_phase(ph, K, Dm, g_name, b_name, dst_x, dst_T=None, dst_b16=None):
    sem_l = ph.p.new_sem("ll")
    gb = ph.sb("gb", [128, D], F32)
    bb = ph.sb("bb", [128, D], F32)
    ph.dma("sync", gb[:], Dm[g_name].to_broadcast([128, D]), sem_l)
    tgb = ph.dma("sync", bb[:], Dm[b_name].to_broadcast([128, D]), sem_l)
    rring = Ring(ph, "rt", 2, [128, D], F32, queue="sync")
    rs = Stream(rring, [[((lambda t: t[:, :]), Dm["s_r"][t * 128:(t + 1) * 128, :])] for t in range(NT)])
    st = ph.sb("st", [128, 8, 6], F32)
    mv = ph.sb("mv", [128, 2], F32)
    sd = ph.sb("sd", [128, 4], F32)
    xn = ph.sb("xn", [128, D], F32)
    xg = ph.sb("xg", [128, D], F32)
    yt = [ph.sb("yt", [128, D], F32) for _ in range(2)]
    sem_y = [ph.p.new_sem("yo") for _ in range(2)]
    y_free = [[], []]
    sem_T2 = ph.p.new_sem("yT2")
    yT = ph.sb("yT", [128, 32, 128], BF16) if dst_T is not None else None
    yb = ph.sb("ybf", [128, D], BF16) if dst_b16 is not None else None
    sem_T = ph.p.new_sem("yT")
    yT_free = None
    yb_free = None
    psF = [ph.ps("psF") for _ in range(2)]
    psF_free = [None, None]
    xn_free = None
    xg_free = None
    for t in range(NT):
        s, rt, rtok = rs.get(t)
        par = t % 2
        d = None
        for c in range(8):
            d = ph.dve(lambda e, c=c, rt=rt: e.bn_stats(out=st[:, c, :], in_=rt[:, c * 512:(c + 1) * 512]), [rtok, d])
        d = ph.dve(lambda e: e.bn_aggr(out=mv[:, :], in_=st[:, :, :].rearrange("p a b -> p (a b)")), [d])
        d = ph.dve(lambda e: e.tensor_scalar(out=sd[:, 0:1], in0=mv[:, 1:2], scalar1=LN_EPS, scalar2=None, op0=ALU.add), [d])
        a = ph.act(lambda e: e.activation(out=sd[:, 1:2], in_=sd[:, 0:1], func=AF.Sqrt), [d])
        d = ph.dve(lambda e: e.reciprocal(out=sd[:, 2:3], in_=sd[:, 1:2]), [a])
        d = ph.dve(lambda e: e.tensor_scalar(out=sd[:, 3:4], in0=mv[:, 0:1], scalar1=sd[:, 2:3], scalar2=-1.0,
                                             op0=ALU.mult, op1=ALU.mult), [d])
        a = ph.act(lambda e, rt=rt: e.activation(out=xn[:, :], in_=rt[:, :], func=AF.Identity, scale=sd[:, 2:3], bias=sd[:, 3:4]),
                   [d, xn_free])
        rs.done(t, a)
        p = ph.pool(lambda e: e.tensor_tensor(out=xg[:, :], in0=xn[:, :], in1=gb[:, :], op=ALU.mult), [a, tgb, xg_free])
        xn_free = p
        y = ph.dve(lambda e, par=par: e.tensor_tensor(out=yt[par][:, :], in0=xg[:, :], in1=bb[:, :], op=ALU.add),
                   [p, tgb] + y_free[par])
        xg_free = y
        users = [ph.dma("sync", Dm[dst_x][t * 128:(t + 1) * 128, :], yt[par][:, :], sem_y[par], waits=[y])]
        if dst_b16 is not None:
            cb = ph.act(lambda e, par=par: e.activation(out=yb[:, :], in_=yt[par][:, :], func=AF.Copy), [y, yb_free])
            yb_free = ph.dma("sync", Dm[dst_b16][t * 128:(t + 1) * 128, :], yb[:, :], sem_T, waits=[cb])
            users.append(cb)
        if dst_T is not None:
            ev = None
            for c4 in range(8):
                bp = c4 % 2
                tk = None
                for q in range(4):
                    c = c4 * 4 + q
                    tk = ph.pe(lambda e, bp=bp, q=q, c=c, par=par: e.transpose(
                        psF[bp][:, q * 128:(q + 1) * 128], yt[par][:, c * 128:(c + 1) * 128], K.ident_f[:]),
                        [y, psF_free[bp]] if q == 0 else (), sig=(q == 3))
                if c4 % 2 == 0:
                    ev = ph.act(lambda e, bp=bp, c4=c4: e.activation(
                        out=yT[:, c4 * 4:c4 * 4 + 4, :], in_=psF[bp][:, :].rearrange("p (a b) -> p a b", a=4), func=AF.Copy),
                        [tk, yT_free])
                else:
                    ev = ph.dve(lambda e, bp=bp, c4=c4: e.tensor_copy(
                        out=yT[:, c4 * 4:c4 * 4 + 4, :], in_=psF[bp][:, :].rearrange("p (a b) -> p a b", a=4)), [tk, yT_free, ev])
                psF_free[bp] = ev
                last_tr = tk
            users.append(last_tr)
            yT_free = ph.dma("sync", Dm[dst_T][:, :, t * 128:(t + 1) * 128], yT[:, :, :], sem_T2,
                             waits=[ev, psF_free[0], psF_free[1]])
        y_free[par] = users


def phase_D1(ph, K, Dm):
    gemm_resid(ph, K, Dm, "s_AT", "w_o", "x_own")


def phase_D2(ph, K, Dm):
    ln_phase(ph, K, Dm, "ln1_g", "ln1_b", "s_x1", dst_T="s_x1T")


def phase_E1(ph, K, Dm):
    sem_l = ph.p.new_sem("e1")
    mt_sb = ph.sb("memT", [128, 32, 256], BF16)
    tm = None
    mem_v = Dm["memT"].rearrange("(kc p) t -> p kc t", p=128)
    for kg in range(4):
        tm = ph.dma("gpsimd", mt_sb[:, kg * 8:kg * 8 + 8, :], mem_v[:, kg * 8:kg * 8 + 8, :], sem_l)
    kmT = ph.sb("kmT", [128, 32, 256], BF16)
    vm = ph.sb("vm", [128, 2, D], BF16)
    ring = Ring(ph, "wE1", 4, [128, 8, 512], BF16)
    pieces = []
    for wn in ("w_mk", "w_mv"):
        w_v = Dm[wn].rearrange("(kc p) n -> p kc n", p=128)
        for nb in range(8):
            for kg in range(4):
                pieces.append([((lambda t: t[:, :, :]), w_v[:, kg * 8:kg * 8 + 8, nb * 512:(nb + 1) * 512])])
    ws = Stream(ring, pieces)
    psb = [ph.ps("psE1") for _ in range(8)]
    bank_free = [None] * 8
    evs = []
    for wi_, wn in enumerate(("w_mk", "w_mv")):
        for nb in range(8):
            off = (nb % 2) * 4 if wi_ == 0 else (nb % 4) * 2
            nbk = 4 if wi_ == 0 else 2
            last_tok = [None] * 8
            for kg in range(4):
                pi = (wi_ * 8 + nb) * 4 + kg
                s, wt, wtok = ws.get(pi)
                tk = None
                for k8 in range(8):
                    kc = kg * 8 + k8
                    for b in range(nbk):
                        if wi_ == 0:
                            fn = (lambda e, b=b, kc=kc, k8=k8, wt=wt, off=off: e.matmul(
                                psb[off + b][:, 0:256], wt[:, k8, b * 128:(b + 1) * 128], mt_sb[:, kc, :],
                                start=(kc == 0), stop=(kc == 31)))
                        else:
                            fn = (lambda e, b=b, kc=kc, k8=k8, wt=wt, off=off: e.matmul(
                                psb[off + b][:, :], mt_sb[:, kc, b * 128:(b + 1) * 128], wt[:, k8, :],
                                start=(kc == 0), stop=(kc == 31)))
                        tk = ph.pe(fn, [wtok, tm, bank_free[off + b]] if kc == 0 else ([wtok] if k8 == 0 and b == 0 else ()),
                                   sig=(kc == 31 or (k8 == 7 and b == nbk - 1)))
                        if kc == 31:
                            last_tok[b] = tk
                ws.done(pi, tk)
            for b in range(nbk):
                if wi_ == 0:
                    dst = kmT[:, nb * 4 + b, :]
                    src = psb[off + b][:, 0:256]
                else:
                    dst = vm[:, b, nb * 512:(nb + 1) * 512]
                    src = psb[off + b][:, :]
                if b % 2 == 0:
                    ev = ph.act(lambda e, dst=dst, src=src: e.activation(out=dst, in_=src, func=AF.Copy), [last_tok[b]])
                else:
                    ev = ph.dve(lambda e, dst=dst, src=src: e.tensor_copy(out=dst, in_=src), [last_tok[b]])
                bank_free[off + b] = ev
                evs.append(ev)
    ph.dma("sync", Dm["s_kmT"], kmT[:], sem_l, waits=evs)
    ph.dma("sync", Dm["s_vm"], vm[:], sem_l, waits=evs)


def phase_E2(ph, K, Dm):
    sem_l = ph.p.new_sem("e2")
    xT = ph.sb("x1T", [128, 32, T], BF16)
    tx = None
    for kg in range(4):
        tx = ph.dma("sync", xT[:, kg * 8:kg * 8 + 8, :], Dm["s_x1T"][:, kg * 8:kg * 8 + 8, :], sem_l)
    w_v = Dm["w_mq"].rearrange("(kc p) n -> p kc n", p=128)
    ring = Ring(ph, "wE2", 4, [128, 8, 512], BF16)
    ws = Stream(ring, [[((lambda t: t[:, :, :]), w_v[:, kg * 8:kg * 8 + 8, nb * 512:(nb + 1) * 512])]
                       for nb in range(8) for kg in range(4)])
    psb = [ph.ps("psE2") for _ in range(8)]
    bank_free = [None] * 8
    qst = [ph.sb("qmst", [128, 4, T], BF16) for _ in range(2)]
    sem_q = [ph.p.new_sem("qmo") for _ in range(2)]
    q_free = [None, None]
    for nb in range(8):
        last_tok = [None] * 8
        for kg in range(4):
            pi = nb * 4 + kg
            s, wt, wtok = ws.get(pi)
            tk = None
            for k8 in range(8):
                kc = kg * 8 + k8
                for b in range(8):
                    ch, hf = b // 2, b % 2
                    tk = ph.pe(lambda e, b=b, ch=ch, hf=hf, kc=kc, k8=k8, wt=wt: e.matmul(
                        psb[b][:, :], wt[:, k8, ch * 128:(ch + 1) * 128], xT[:, kc, hf * 512:(hf + 1) * 512],
                        start=(kc == 0), stop=(kc == 31)),
                        [wtok, tx, bank_free[b]] if kc == 0 else ([wtok] if k8 == 0 and b == 0 else ()),
                        sig=(kc == 31 or (k8 == 7 and b == 7)))
                    if kc == 31:
                        last_tok[b] = tk
            ws.done(pi, tk)
        par = nb % 2
        evs = []
        for b in range(8):
            ch, hf = b // 2, b % 2
            dst = qst[par][:, ch, hf * 512:(hf + 1) * 512]
            if b % 2 == 0:
                ev = ph.act(lambda e, b=b, dst=dst: e.activation(out=dst, in_=psb[b][:, :], func=AF.Copy), [last_tok[b], q_free[par]])
            else:
                ev = ph.dve(lambda e, b=b, dst=dst: e.tensor_copy(out=dst, in_=psb[b][:, :]), [last_tok[b], q_free[par]])
            bank_free[b] = ev
            evs.append(ev)
        q_free[par] = ph.dma("sync", Dm["s_qmT"][:, nb * 4:nb * 4 + 4, :], qst[par][:, :, :], sem_q[par], waits=evs)


def phase_E3(ph, K, Dm):
    sem_l = ph.p.new_sem("e3")
    qmT = ph.sb("qmT", [128, 32, T], BF16)
    kmT = ph.sb("kmT", [128, 32, 256], BF16)
    vm = ph.sb("vm", [128, 2, D], BF16)
    for kg in range(4):
        ph.dma("sync", qmT[:, kg * 8:kg * 8 + 8, :], Dm["s_qmT"][:, kg * 8:kg * 8 + 8, :], sem_l)
    ph.dma("sync", kmT[:], Dm["s_kmT"], sem_l)
    tl = ph.dma("sync", vm[:], Dm["s_vm"], sem_l)
    pT = [[ph.sb("pTm", [128, 512], BF16) for _ in range(2)] for _ in range(2)]
    rs = ph.sb("rs", [128, 512], F32)
    ost = [ph.sb("ost", [128, 8, 512], BF16) for _ in range(2)]
    sem_o = [ph.p.new_sem("oo") for _ in range(2)]
    o_free = [None, None]
    psS = [ph.ps("psS3") for _ in range(2)]
    psZ = ph.ps("psZ")
    psO = [ph.ps("psO3") for _ in range(2)]
    psS_free = [None, None]
    psZ_free = None
    psO_free = [None, None]
    pT_free = [None, None]
    rs_free = None
    it = 0
    scale = 1024 ** -0.5
    for hm in range(4):
        for hf in range(2):
            par = it % 2
            it += 1
            exps = []
            for mt in range(2):
                tk = None
                for kc in range(8):
                    tk = ph.pe(lambda e, mt=mt, kc=kc, hm=hm, hf=hf: e.matmul(
                        psS[mt][:, :], kmT[:, hm * 8 + kc, mt * 128:(mt + 1) * 128], qmT[:, hm * 8 + kc, hf * 512:(hf + 1) * 512],
                        start=(kc == 0), stop=(kc == 7)), [tl, psS_free[mt]] if kc == 0 else (), sig=(kc == 7))
                a = ph.act(lambda e, mt=mt, par=par: e.activation(out=pT[par][mt][:, :], in_=psS[mt][:, :], func=AF.Exp, scale=scale),
                           [tk, pT_free[par]])
                psS_free[mt] = a
                exps.append(a)
            tk = None
            for mt in range(2):
                tk = ph.pe(lambda e, mt=mt, par=par: e.matmul(psZ[:, :], K.ones_bf[:], pT[par][mt][:, :], start=(mt == 0), stop=(mt == 1)),
                           (exps + [psZ_free]) if mt == 0 else (), sig=(mt == 1))
            r = ph.dve(lambda e: e.reciprocal(out=rs[:, :], in_=psZ[:, :]), [tk, rs_free])
            psZ_free = r
            ev = None
            for dc in range(8):
                bp = dc % 2
                tk = None
                for mt in range(2):
                    col = hm * 1024 + dc * 128
                    tk = ph.pe(lambda e, mt=mt, bp=bp, col=col, par=par: e.matmul(
                        psO[bp][:, :], vm[:, mt, col:col + 128], pT[par][mt][:, :], start=(mt == 0), stop=(mt == 1)),
                        (exps + [psO_free[bp]]) if mt == 0 else (), sig=(mt == 1))
                ev = ph.dve(lambda e, bp=bp, dc=dc, par=par: e.tensor_tensor(out=ost[par][:, dc, :], in0=psO[bp][:, :], in1=rs[:, :], op=ALU.mult),
                            [tk, r, o_free[par]])
                psO_free[bp] = ev
                last_pv = tk
            pT_free[par] = last_pv
            rs_free = ev
            o_free[par] = ph.dma("sync", Dm["s_oT"][:, hm * 8:hm * 8 + 8, hf * 512:(hf + 1) * 512], ost[par][:, :, :], sem_o[par],
                                 waits=[ev])


def phase_E4(ph, K, Dm):
    gemm_resid(ph, K, Dm, "s_oT", "w_mo", "s_x1")


def phase_E5(ph, K, Dm):
    ln_phase(ph, K, Dm, "ln2_g", "ln2_b", "s_x2", dst_T="s_x2T", dst_b16="s_x2b")


AXX = mybir.AxisListType.X


def phase_F1(ph, K, Dm):
    sem_l = ph.p.new_sem("f1")
    xT = ph.sb("x2T", [128, 32, T], BF16)
    tx = None
    for kg in range(4):
        tx = ph.dma("sync", xT[:, kg * 8:kg * 8 + 8, :], Dm["s_x2T"][:, kg * 8:kg * 8 + 8, :], sem_l)
    wr = ph.sb("wr", [128, 32, 72], BF16)
    twr = ph.dma("gpsimd", wr[:], Dm["w_r"].rearrange("(kc p) n -> p kc n", p=128), ph.p.new_sem("wr"))
    psb = [ph.ps("psF1") for _ in range(8)]
    lg = ph.sb("lg", [128, 72], F32)
    gm = ph.sb("gm", [128, 8], F32)
    ohg = ph.sb("ohg", [128, 8], F32)
    eg = ph.sb("eg", [128, 8], F32)
    sm = ph.sb("sm", [128, 8], F32)
    tmp3 = ph.sb("tmp3", [128, 8, 8], F32)
    esel = ph.sb("esel", [128, 8], F32)
    em = ph.sb("em", [128, 8], F32)
    oh1 = ph.sb("oh1", [128, 8], F32)
    oh2 = ph.sb("oh2", [128, 8], F32)
    d = None
    for t in range(NT):
        tk = None
        for kc in range(32):
            tk = ph.pe(lambda e, t=t, kc=kc: e.matmul(psb[t][:, 0:72], xT[:, kc, t * 128:(t + 1) * 128], wr[:, kc, :],
                                                      start=(kc == 0), stop=(kc == 31)), [tx, twr] if kc == 0 else (), sig=(kc == 31))
        d = ph.dve(lambda e, t=t: e.tensor_tensor(out=lg[:, :], in0=psb[t][:, 0:72], in1=K.bias_r[:, :], op=ALU.add), [tk, d])
        d = ph.dve(lambda e: e.max(out=gm[:, :], in_=lg[:, 0:8]), [d])
        d = ph.dve(lambda e: e.tensor_scalar(out=ohg[:, :], in0=lg[:, 0:8], scalar1=gm[:, 0:1], scalar2=None, op0=ALU.is_equal), [d])
        d = ph.dve(lambda e: e.tensor_scalar(out=sm[:, 0:1], in0=gm[:, 0:1], scalar1=-1.0, scalar2=None, op0=ALU.mult), [d])
        a = ph.act(lambda e: e.activation(out=eg[:, :], in_=lg[:, 0:8], func=AF.Exp, bias=sm[:, 0:1]), [d])
        d = ph.dve(lambda e: e.tensor_reduce(out=sm[:, 1:2], in_=eg[:, :], axis=AXX, op=ALU.add), [a])
        d = ph.dve(lambda e: e.reciprocal(out=sm[:, 2:3], in_=sm[:, 1:2]), [d])
        d = ph.dve(lambda e: e.tensor_tensor(out=tmp3[:, :, :], in0=lg[:, 8:72].rearrange("p (g j) -> p g j", g=8),
                                             in1=ohg[:, :].unsqueeze(2).to_broadcast([128, 8, 8]), op=ALU.mult), [d])
        d = ph.dve(lambda e: e.tensor_reduce(out=esel[:, :], in_=tmp3[:, :, :].rearrange("p g j -> p j g"), axis=AXX, op=ALU.add), [d])
        d = ph.dve(lambda e: e.max(out=em[:, :], in_=esel[:, :]), [d])
        d = ph.dve(lambda e: e.tensor_scalar(out=oh1[:, :], in0=esel[:, :], scalar1=em[:, 0:1], scalar2=None, op0=ALU.is_equal), [d])
        d = ph.dve(lambda e: e.tensor_scalar(out=oh2[:, :], in0=esel[:, :], scalar1=em[:, 1:2], scalar2=None, op0=ALU.is_equal), [d])
        d = ph.dve(lambda e: e.tensor_tensor(out=sm[:, 3:4], in0=em[:, 1:2], in1=em[:, 0:1], op=ALU.subtract), [d])
        a = ph.act(lambda e: e.activation(out=sm[:, 4:5], in_=sm[:, 3:4], func=AF.Exp), [d])
        d = ph.dve(lambda e: e.tensor_scalar(out=sm[:, 5:6], in0=sm[:, 4:5], scalar1=1.0, scalar2=None, op0=ALU.add), [a])
        d = ph.dve(lambda e: e.reciprocal(out=sm[:, 6:7], in_=sm[:, 5:6]), [d])
        d = ph.dve(lambda e, t=t: e.tensor_tensor(out=K.gates[:, t, 0:1], in0=sm[:, 6:7], in1=sm[:, 2:3], op=ALU.mult), [d])
        d = ph.dve(lambda e, t=t: e.tensor_tensor(out=K.gates[:, t, 1:2], in0=K.gates[:, t, 0:1], in1=sm[:, 4:5], op=ALU.mult), [d])
        for a_i, oh in enumerate((oh1, oh2)):
            d = ph.dve(lambda e, t=t, a_i=a_i, oh=oh: e.tensor_tensor(
                out=K.A12[:, t, a_i, :].rearrange("p (g j) -> p g j", g=8), in0=ohg[:, :].unsqueeze(2).to_broadcast([128, 8, 8]),
                in1=oh[:, :].unsqueeze(1).to_broadcast([128, 8, 8]), op=ALU.mult), [d])
        d = ph.dve(lambda e, t=t: e.tensor_tensor(out=K.Aall[:, t, :], in0=K.A12[:, t, 0, :], in1=K.A12[:, t, 1, :], op=ALU.add), [d])
    cnt = ph.sb("cnt", [128, 64], F32)
    tmpc = ph.sb("tmpc", [128, 64], F32)
    pe_ = ph.sb("pe_", [128, 4], F32)
    psC = psb[0]
    zt = ph.sb("zt", [128, D], BF16)
    tz = ph.pool(lambda e: e.memset(zt[:, :], 0.0))
    sem_z = ph.p.new_sem("zf")
    xe_v = Dm["s_xe"].rearrange("(e p) d -> p e d", p=128)
    tzf = None
    for e8 in range(8):
        tzf = ph.dma("sync", xe_v[:, e8 * 8:e8 * 8 + 8, :], zt[:, :].unsqueeze(1).to_broadcast([128, 8, D]), sem_z, waits=[tz])
    x2b = [ph.sb("x2b", [128, D], BF16) for _ in range(2)]
    sem_x = [ph.p.new_sem("x2b") for _ in range(2)]
    sem_s = [ph.p.new_sem("sct") for _ in range(2)]
    x_free = [None, None]
    for t in range(NT):
        tk = None
        for tp in range(t + 1):
            lhs = K.tri if tp == t else K.ones_bf
            tk = ph.pe(lambda e, tp=tp, lhs=lhs, t=t: e.matmul(psC[:, 0:64], lhs[:], K.Aall[:, tp, :], start=(tp == 0), stop=(tp == t)),
                       [d] if tp == 0 else (), sig=(tp == t))
        d = ph.dve(lambda e: e.tensor_copy(out=cnt[:, :], in_=psC[:, 0:64]), [tk, d])
        for a_i in range(2):
            d = ph.dve(lambda e, t=t, a_i=a_i: e.tensor_tensor(out=tmpc[:, :], in0=K.A12[:, t, a_i, :], in1=cnt[:, :], op=ALU.mult), [d])
            d = ph.dve(lambda e, a_i=a_i: e.tensor_reduce(out=pe_[:, 2 * a_i:2 * a_i + 1], in_=tmpc[:, :], axis=AXX, op=ALU.add), [d])
            d = ph.dve(lambda e, t=t, a_i=a_i: e.tensor_tensor(out=tmpc[:, :], in0=K.A12[:, t, a_i, :], in1=K.iota64[:, :], op=ALU.mult), [d])
            d = ph.dve(lambda e, a_i=a_i: e.tensor_reduce(out=pe_[:, 2 * a_i + 1:2 * a_i + 2], in_=tmpc[:, :], axis=AXX, op=ALU.add), [d])
            d = ph.dve(lambda e, a_i=a_i: e.scalar_tensor_tensor(out=pe_[:, 2 * a_i:2 * a_i + 1], in0=pe_[:, 2 * a_i + 1:2 * a_i + 2],
                                                                scalar=float(CAP), in1=pe_[:, 2 * a_i:2 * a_i + 1],
                                                                op0=ALU.mult, op1=ALU.add), [d])
            d = ph.dve(lambda e, a_i=a_i: e.tensor_scalar(out=pe_[:, 2 * a_i:2 * a_i + 1], in0=pe_[:, 2 * a_i:2 * a_i + 1], scalar1=0.0,
                                                         scalar2=float(NEXP * CAP - 1), op0=ALU.max, op1=ALU.min), [d])
            d = ph.dve(lambda e, t=t, a_i=a_i: e.tensor_copy(out=K.dest[:, t, a_i:a_i + 1], in_=pe_[:, 2 * a_i:2 * a_i + 1]), [d])
        par = t % 2
        tl = ph.dma("sync", x2b[par][:, :], Dm["s_x2b"][t * 128:(t + 1) * 128, :], sem_x[par], waits=[x_free[par]])
        for a_i in range(2):
            x_free[par] = ph.idma(out=Dm["s_xe"], in_=x2b[par][:, :], sem=sem_s[par], waits=[tl, d, tzf],
                                  out_off=K.dest[:, t, a_i:a_i + 1])


def phase_F2(ph, K, Dm):
    xring = Ring(ph, "xe", 2, [128, D], BF16, queue="sync")
    xs = Stream(xring, [[((lambda t: t[:, :]), Dm["s_xe"][e * CAP:(e + 1) * CAP, :])] for e in range(NEXP)])
    ring = Ring(ph, "wF", 8, [128, 4, 512], BF16)
    pieces = []
    for e in range(NEXP):
        wg = Dm["w_gate"][e].rearrange("(kc p) f -> p kc f", p=128)
        wu = Dm["w_up"][e].rearrange("(kc p) f -> p kc f", p=128)
        wd = Dm["w_down"][e].rearrange("(fc p) n -> p fc n", p=128)
        for kg in range(8):
            pieces.append([((lambda t: t[:, :, :]), wg[:, kg * 4:kg * 4 + 4, :])])
            pieces.append([((lambda t: t[:, :, :]), wu[:, kg * 4:kg * 4 + 4, :])])
        for nb in range(8):
            pieces.append([((lambda t: t[:, :, :]), wd[:, :, nb * 512:(nb + 1) * 512])])
    ws = Stream(ring, pieces)
    xeT = [ph.sb("xeT", [128, 32, 128], BF16) for _ in range(2)]
    sg = ph.sb("sg", [128, 512], F32)
    hh = ph.sb("hh", [128, 512], BF16)
    hT = ph.sb("hT", [128, 4, 128], BF16)
    ye = [ph.sb("ye", [128, D], F32) for _ in range(2)]
    sem_y = [ph.p.new_sem("yeo") for _ in range(2)]
    ye_free = [None, None]
    psT = [ph.ps("psTe", (128, 1024), BF16) for _ in range(2)]
    psG = ph.ps("psGe")
    psU = ph.ps("psUe")
    psD = [ph.ps("psDe") for _ in range(2)]
    psT_free = [None, None]
    psG_free = None
    psU_free = None
    psD_free = [None, None]
    xeT_free = [None, None]
    sg_free = None
    hh_free = None
    hT_free = None
    nT = 0
    pi = 0
    for e in range(NEXP):
        par = e % 2
        s, xe, xtok = xs.get(e)
        evs = []
        last_tr = None
        for c4 in range(8):
            bp = nT % 2
            nT += 1
            tk = None
            for q in range(4):
                c = c4 * 4 + q
                tk = ph.pe(lambda e_, bp=bp, q=q, c=c, xe=xe: e_.transpose(psT[bp][:, q * 128:(q + 1) * 128], xe[:, c * 128:(c + 1) * 128],
                                                                        K.ident_bf[:]),
                           [xtok, psT_free[bp]] if q == 0 else (), sig=(q == 3))
            src = psT[bp][:, 0:512].rearrange("p (a b) -> p a b", a=4)
            dst = xeT[par][:, c4 * 4:c4 * 4 + 4, :]
            if c4 % 2 == 0:
                ev = ph.act(lambda e_, src=src, dst=dst: e_.activation(out=dst, in_=src, func=AF.Copy), [tk, xeT_free[par]])
            else:
                ev = ph.dve(lambda e_, src=src, dst=dst: e_.tensor_copy(out=dst, in_=src), [tk, xeT_free[par]])
            psT_free[bp] = ev
            evs.append(ev)
            last_tr = tk
        xs.done(e, last_tr)
        tg = tu = None
        for kg in range(8):
            for which in range(2):
                s_, wt, wtok = ws.get(pi)
                ps_ = psG if which == 0 else psU
                fr = psG_free if which == 0 else psU_free
                tk = None
                for k4 in range(4):
                    kc = kg * 4 + k4
                    tk = ph.pe(lambda e_, ps_=ps_, kc=kc, k4=k4, wt=wt, par=par: e_.matmul(
                        ps_[:, :], xeT[par][:, kc, :], wt[:, k4, :], start=(kc == 0), stop=(kc == 31)),
                        ([wtok, fr] + evs) if kc == 0 else ([wtok] if k4 == 0 else ()), sig=(k4 == 3))
                ws.done(pi, tk)
                pi += 1
                if which == 0:
                    tg = tk
                else:
                    tu = tk
        xeT_free[par] = tu
        a = ph.act(lambda e_: e_.activation(out=sg[:, :], in_=psG[:, :], func=AF.Silu), [tg, sg_free])
        psG_free = a
        m = ph.dve(lambda e_: e_.tensor_tensor(out=hh[:, :], in0=sg[:, :], in1=psU[:, :], op=ALU.mult), [a, tu, hh_free])
        psU_free = m
        sg_free = m
        bp = nT % 2
        nT += 1
        tk = None
        for q in range(4):
            tk = ph.pe(lambda e_, bp=bp, q=q: e_.transpose(psT[bp][:, q * 128:(q + 1) * 128], hh[:, q * 128:(q + 1) * 128], K.ident_bf[:]),
                       [m, psT_free[bp]] if q == 0 else (), sig=(q == 3))
        hh_free = tk
        ev = ph.act(lambda e_, bp=bp: e_.activation(out=hT[:, :, :], in_=psT[bp][:, 0:512].rearrange("p (a b) -> p a b", a=4), func=AF.Copy),
                    [tk, hT_free])
        psT_free[bp] = ev
        evd = None
        ev6 = None
        last_dn = None
        for nb in range(8):
            s_, wt, wtok = ws.get(pi)
            bd = nb % 2
            tk = None
            for fc in range(4):
                tk = ph.pe(lambda e_, bd=bd, fc=fc, wt=wt: e_.matmul(psD[bd][:, :], hT[:, fc, :], wt[:, fc, :], start=(fc == 0), stop=(fc == 3)),
                           [wtok, ev, psD_free[bd]] if fc == 0 else (), sig=(fc == 3))
            ws.done(pi, tk)
            pi += 1
            dst = ye[par][:, nb * 512:(nb + 1) * 512]
            if nb % 2 == 0:
                evd = ph.act(lambda e_, bd=bd, dst=dst: e_.activation(out=dst, in_=psD[bd][:, :], func=AF.Copy), [tk, ye_free[par]])
            else:
                evd = ph.dve(lambda e_, bd=bd, dst=dst: e_.tensor_copy(out=dst, in_=psD[bd][:, :]), [tk, ye_free[par]])
            psD_free[bd] = evd
            if nb == 6:
                ev6 = evd
            last_dn = tk
        hT_free = last_dn
        ye_free[par] = ph.dma("sync", Dm["s_ye"][e * CAP:(e + 1) * CAP, :], ye[par][:, :], sem_y[par], waits=[evd, ev6])


def phase_F3(ph, K, Dm):
    x2 = [ph.sb("x2t", [128, D], F32) for _ in range(2)]
    r1 = [ph.sb("r1", [128, D], F32) for _ in range(2)]
    r2 = [ph.sb("r2", [128, D], F32) for _ in range(2)]
    acc = [ph.sb("acc", [128, D], F32) for _ in range(2)]
    sem_x = [ph.p.new_sem("f3x") for _ in range(2)]
    sem_1 = [ph.p.new_sem("f31") for _ in range(2)]
    sem_2 = [ph.p.new_sem("f32") for _ in range(2)]
    sem_o = [ph.p.new_sem("f3o") for _ in range(2)]
    in_free = [None, None]
    acc_free = [None, None]
    for t in range(NT):
        par = t % 2
        tx = ph.dma("sync", x2[par][:, :], Dm["s_x2"][t * 128:(t + 1) * 128, :], sem_x[par], waits=[in_free[par]])
        t1 = ph.idma(out=r1[par][:, :], in_=Dm["s_ye"], sem=sem_1[par], waits=[in_free[par]], in_off=K.dest[:, t, 0:1])
        t2 = ph.idma(out=r2[par][:, :], in_=Dm["s_ye"], sem=sem_2[par], waits=[in_free[par]], in_off=K.dest[:, t, 1:2])
        a = ph.act(lambda e, par=par: e.activation(out=acc[par][:, :], in_=x2[par][:, :], func=AF.Copy, scale=ALPHA), [tx, acc_free[par]])
        d = ph.dve(lambda e, par=par, t=t: e.scalar_tensor_tensor(out=acc[par][:, :], in0=r1[par][:, :], scalar=K.gates[:, t, 0:1],
                                                                in1=acc[par][:, :], op0=ALU.mult, op1=ALU.add), [a, t1])
        d = ph.dve(lambda e, par=par, t=t: e.scalar_tensor_tensor(out=acc[par][:, :], in0=r2[par][:, :], scalar=K.gates[:, t, 1:2],
                                                                in1=acc[par][:, :], op0=ALU.mult, op1=ALU.add), [d, t2])
        in_free[par] = d
        acc_free[par] = ph.dma("sync", Dm["s_r"][t * 128:(t + 1) * 128, :], acc[par][:, :], sem_o[par], waits=[d])


def phase_F4(ph, K, Dm):
    ln_phase(ph, K, Dm, "ln3_g", "ln3_b", "out")


PHASES[:] = [phase_C, phase_D1, phase_D2, phase_E1, phase_E2, phase_E3, phase_E4, phase_E5,
             phase_F1, phase_F2, phase_F3, phase_F4]
```

```python
import os
import math
from contextlib import ExitStack
import numpy as np
import ml_dtypes
import concourse.bass as bass
import concourse.mybir as mybir
from concourse.bass_utils import run_bass_kernel_spmd

F32 = mybir.dt.float32
BF16 = mybir.dt.bfloat16
I32 = mybir.dt.int32
AF = mybir.ActivationFunctionType
ALU = mybir.AluOpType

D = 4096
S = 4096
T = 1024
NT = 8
DIN = 5704
ALPHA = 2.0 ** 0.25
LN_EPS = 1e-5
NEXP = 64
CAP = 128
TWO_PI = 2.0 * math.pi
C1 = 6.28125
C2 = TWO_PI - C1
PI_SAFE = 3.1415925
NEG_BIG = -3.0e30
NEG_MID = -2.0e30
ENGS = ["tensor", "vector", "scalar", "gpsimd", "sync"]


class Sem:
    def __init__(self, h, name):
        self.h = h
        self.name = name
        self.count = 0


class Prog:
    def __init__(self, nc, es):
        self.nc = nc
        self.es = es
        self.nsem = 0
        self.pool = []
        self.pool_idx = 0
        self.eng_sem = {e: self._alloc_sem("pg_" + e) for e in ENGS}
        self.waited = {e: {} for e in ENGS}
        self.uid = 0

    def _alloc_sem(self, name):
        self.nsem += 1
        nm = "%s_%d" % (name, self.nsem)
        return Sem(self.es.enter_context(self.nc.semaphore(nm)), nm)

    def new_sem(self, name="s"):
        if self.pool_idx == len(self.pool):
            self.pool.append(self._alloc_sem("d"))
        sem = self.pool[self.pool_idx]
        self.pool_idx += 1
        return sem

    def name(self, base):
        self.uid += 1
        return "%s_%d" % (base, self.uid)


class Phase:
    def __init__(self, prog):
        self.p = prog
        self.nc = prog.nc
        self.ops = {e: [] for e in ENGS}
        self.dma_toks = {}
        self.es = None

    def sb(self, name, shape, dt):
        return self.es.enter_context(self.nc.sbuf_tensor(self.p.name(name), list(shape), dt))

    def ps(self, name, shape=(128, 512), dt=F32):
        return self.es.enter_context(self.nc.psum_tensor(self.p.name(name), list(shape), dt))

    def op(self, eng, fn, waits=(), sig=True):
        ws = tuple(w for w in waits if w is not None)
        if sig:
            s = self.p.eng_sem[eng]
            s.count += 1
            tok = (s, s.count)
            self.ops[eng].append((ws, fn, (s, 1)))
            return tok
        self.ops[eng].append((ws, fn, None))
        return None

    def pe(self, fn, waits=(), sig=True):
        return self.op("tensor", fn, waits, sig)

    def dve(self, fn, waits=(), sig=True):
        return self.op("vector", fn, waits, sig)

    def act(self, fn, waits=(), sig=True):
        return self.op("scalar", fn, waits, sig)

    def pool(self, fn, waits=(), sig=True):
        return self.op("gpsimd", fn, waits, sig)

    def dma(self, eng, out, in_, sem, waits=()):
        ws = tuple(w for w in waits if w is not None)
        sem.count += 16
        tok = (sem, sem.count)
        self.ops[eng].append((ws, (lambda e, o=out, i=in_: e.dma_start(out=o, in_=i)), (sem, 16)))
        self.dma_toks[sem.name] = tok
        return tok

    def idma(self, out, in_, sem, waits=(), out_off=None, in_off=None):
        ws = tuple(w for w in waits if w is not None)
        sem.count += 16
        tok = (sem, sem.count)

        def fn(e, o=out, i=in_, oo=out_off, io=in_off):
            return e.indirect_dma_start(
                out=o, out_offset=(bass.IndirectOffsetOnAxis(ap=oo, axis=0) if oo is not None else None),
                in_=i, in_offset=(bass.IndirectOffsetOnAxis(ap=io, axis=0) if io is not None else None))
        self.ops["gpsimd"].append((ws, fn, (sem, 16)))
        self.dma_toks[sem.name] = tok
        return tok

    def emit(self, block):
        fin = tuple(self.dma_toks.values())
        if fin:
            self.ops["sync"].append((fin, None, None))
        for eng in ENGS:
            ops = self.ops[eng]
            if not ops:
                continue
            waited = self.p.waited[eng]

            def body(e, ops=ops, waited=waited):
                for ws, fn, inc in ops:
                    for (sem, val) in ws:
                        if waited.get(sem.name, 0) < val:
                            e.wait_ge(sem.h, val)
                            waited[sem.name] = val
                    if fn is None:
                        continue
                    inst = fn(e)
                    if inc is not None:
                        inst.then_inc(inc[0].h, inc[1])
            getattr(block, eng)(body)


def run_phase(prog, fn):
    nc = prog.nc
    with ExitStack() as es:
        ph = Phase(prog)
        prog.pool_idx = 0
        ph.es = es
        block = es.enter_context(nc.Block())
        fn(ph)
        ph.emit(block)


class Ring:
    def __init__(self, ph, name, nslots, shape, dt, queue="gpsimd"):
        self.ph = ph
        self.n = nslots
        self.tiles = [ph.sb(name + str(i), shape, dt) for i in range(nslots)]
        self.sems = [ph.p.new_sem(name) for _ in range(nslots)]
        self.free = [None] * nslots
        self.k = 0
        self.queue = queue

    def load(self, parts):
        s = self.k % self.n
        self.k += 1
        tok = None
        for dst_fn, src in parts:
            tok = self.ph.dma(self.queue, dst_fn(self.tiles[s]), src, self.sems[s], waits=[self.free[s]])
        return s, self.tiles[s], tok

    def release(self, s, tok):
        self.free[s] = tok


class Stream:
    def __init__(self, ring, pieces):
        self.ring = ring
        self.pieces = pieces
        self.loaded = []

    def get(self, i):
        want = min(len(self.pieces), i + self.ring.n)
        while len(self.loaded) < want:
            self.loaded.append(self.ring.load(self.pieces[len(self.loaded)]))
        return self.loaded[i]

    def done(self, i, tok):
        self.ring.release(self.loaded[i][0], tok)


def rope_tables(ph, K, posi, n, tabs, tmp, waits):
    posf, arg, ki, kf, r = tmp
    t = ph.dve(lambda e: e.tensor_copy(out=posf[:, :n], in_=posi[:, :n]), waits)
    last_act = None
    for ti, tab in enumerate(tabs):
        fam = ti // 2
        is_cos = (ti % 2 == 0)
        inv = K.invs[:, 2 * fam:2 * fam + 1]
        sign = K.invs[:, 2 * fam + 1:2 * fam + 2]
        shift = (math.pi / 2.0) if is_cos else 0.0
        t = ph.dve(lambda e, inv=inv, shift=shift: e.tensor_scalar(
            out=arg[:, :n], in0=posf[:, :n], scalar1=inv, scalar2=shift, op0=ALU.mult, op1=ALU.add), [t, last_act])
        t = ph.dve(lambda e: e.tensor_scalar(
            out=ki[:, :n], in0=arg[:, :n], scalar1=1.0 / TWO_PI, scalar2=None, op0=ALU.mult), [t])
        t = ph.dve(lambda e: e.tensor_copy(out=kf[:, :n], in_=ki[:, :n]), [t])
        t = ph.dve(lambda e: e.scalar_tensor_tensor(
            out=r[:, :n], in0=kf[:, :n], scalar=-C1, in1=arg[:, :n], op0=ALU.mult, op1=ALU.add), [t, last_act])
        t = ph.dve(lambda e: e.scalar_tensor_tensor(
            out=r[:, :n], in0=kf[:, :n], scalar=-C2, in1=r[:, :n], op0=ALU.mult, op1=ALU.add), [t])
        t = ph.dve(lambda e: e.tensor_scalar(
            out=r[:, :n], in0=r[:, :n], scalar1=-PI_SAFE, scalar2=PI_SAFE, op0=ALU.max, op1=ALU.min), [t])
        if is_cos:
            last_act = ph.act(lambda e, tab=tab: e.activation(out=tab[:, :n], in_=r[:, :n], func=AF.Sin), [t])
        else:
            last_act = ph.act(lambda e, tab=tab, sign=sign: e.activation(
                out=tab[:, :n], in_=r[:, :n], func=AF.Sin, scale=sign), [t])
        t = last_act
    return last_act


def rope_fm(ph, src_ps, n, u, sw_ps, t1, t2, cosT, sinT, perm, out_ap, waits, view=None):
    if view is None:
        view = lambda ap: ap
    a = ph.act(lambda e: e.activation(out=u[:, :n], in_=src_ps, func=AF.Copy), waits)
    b = ph.pe(lambda e: e.matmul(sw_ps[:, :n], perm, u[:, :n], start=True, stop=True), [a])
    c = ph.dve(lambda e: e.tensor_tensor(out=t1[:, :n], in0=u[:, :n], in1=cosT, op=ALU.mult), [a])
    d = ph.dve(lambda e: e.tensor_tensor(out=t2[:, :n], in0=sw_ps[:, :n], in1=sinT, op=ALU.mult), [b, c])
    f = ph.dve(lambda e: e.tensor_tensor(out=out_ap, in0=view(t1[:, :n]), in1=view(t2[:, :n]), op=ALU.add), [d])
    return f, a


class Consts:
    pass


def phase_consts(ph, K, Dm):
    sem = ph.p.new_sem("c")
    for nm in ["ident_bf", "ident_f", "permH", "permI", "invs", "ones_bf", "tri", "iota64"]:
        ph.dma("sync", getattr(K, nm)[:], Dm[nm], sem)
    K.tok = ph.dma("sync", K.bias_r[:], Dm["b_r"].to_broadcast([128, 72]), sem)


def phase_A(ph, K, Dm):
    CH = 256
    NCH = S // CH
    w_in_v = Dm["w_in"].rearrange("(kc p) n -> p kc n", p=128)
    xT_v = Dm["xT_ctx"].rearrange("(kc p) t -> p kc t", p=128)
    Wk = ph.sb("Wk", [128, 32, 512], BF16)
    Wv = ph.sb("Wv", [128, 32, 512], BF16)
    Wki = ph.sb("Wki", [128, 32, 128], BF16)
    semw = ph.p.new_sem("wA")
    tw = None
    for kg in range(4):
        ks = slice(kg * 8, kg * 8 + 8)
        ph.dma("gpsimd", Wk[:, ks, :], w_in_v[:, ks, 4096:4608], semw)
        ph.dma("gpsimd", Wv[:, ks, :], w_in_v[:, ks, 4608:5120], semw)
        ph.dma("gpsimd", Wki[:, ks, 0:64], w_in_v[:, ks, 5632:5696], semw)
        tw = ph.dma("gpsimd", Wki[:, ks, 64:128], w_in_v[:, ks, 5632:5696], semw)
    xring = Ring(ph, "xc", 2, [128, 32, CH], BF16)
    pieces = []
    for c in range(NCH):
        pieces.append([((lambda t, kg=kg: t[:, kg * 8:kg * 8 + 8, :]), xT_v[:, kg * 8:kg * 8 + 8, c * CH:(c + 1) * CH])
                       for kg in range(4)])
    xs = Stream(xring, pieces)
    posi = [ph.sb("posi", [128, CH], I32) for _ in range(2)]
    sem_pos = [ph.p.new_sem("pos") for _ in range(2)]
    tmp = (ph.sb("posf", [128, CH], F32), ph.sb("arg", [128, CH], F32), ph.sb("ki", [128, CH], I32),
           ph.sb("kf", [128, CH], F32), ph.sb("r", [128, CH], F32))
    tabs = [tuple(ph.sb("tab", [128, CH], F32) for _ in range(4)) for _ in range(2)]
    psk = [ph.ps("psk") for _ in range(4)]
    psv = [ph.ps("psv") for _ in range(2)]
    pski = ph.ps("pski")
    pssw = ph.ps("pssw")
    u = ph.sb("u", [128, CH], BF16)
    t1 = ph.sb("t1", [128, CH], F32)
    t2 = ph.sb("t2", [128, CH], F32)
    kout = [ph.sb("kout", [128, 4, CH], BF16) for _ in range(2)]
    vout = [ph.sb("vout", [128, 2, 512], BF16) for _ in range(2)]
    kiout = [ph.sb("kiout", [128, CH], BF16) for _ in range(2)]
    sem_o = [ph.p.new_sem("oA") for _ in range(2)]
    out_tok = [None, None]
    tab_free = [None, None]
    psk_free = [None] * 4
    psv_free = [None] * 2
    pski_free = None
    s_v_v = Dm["s_v"].rearrange("(n p) c -> p n c", p=128)
    for c in range(NCH):
        par = c % 2
        s, xt, xtok = xs.get(c)
        tp = ph.dma("sync", posi[par][:], Dm["pos_ctx"][0:1, c * CH:(c + 1) * CH].to_broadcast([128, CH]),
                    sem_pos[par], waits=[tab_free[par]])
        ttab = rope_tables(ph, K, posi[par], CH, tabs[par], tmp, [tp, tab_free[par]])
        cosH, sinH, cosI, sinI = tabs[par]
        last_rope = None
        for g in range(4):
            tk = None
            for kc in range(32):
                tk = ph.pe(lambda e, g=g, kc=kc, xt=xt: e.matmul(
                    psk[g][:, :CH], Wk[:, kc, g * 128:(g + 1) * 128], xt[:, kc, :], start=(kc == 0), stop=(kc == 31)),
                    [tw, xtok, psk_free[g]] if kc == 0 else (), sig=(kc == 31))
            f, a = rope_fm(ph, psk[g][:, :CH], CH, u, pssw, t1, t2, cosH[:, :], sinH[:, :], K.permH[:],
                           kout[par][:, g, :], [tk, ttab, out_tok[par], last_rope])
            psk_free[g] = a
            last_rope = f
        tk = None
        for kc in range(32):
            tk = ph.pe(lambda e, kc=kc, xt=xt: e.matmul(
                pski[:, :CH], Wki[:, kc, :], xt[:, kc, :], start=(kc == 0), stop=(kc == 31)),
                [pski_free] if kc == 0 else (), sig=(kc == 31))
        f, a = rope_fm(ph, pski[:, :CH], CH, u, pssw, t1, t2, cosI[:, :], sinI[:, :], K.permI[:],
                       kiout[par][:, :], [tk, last_rope])
        pski_free = a
        last_rope = f
        tab_free[par] = f
        tv_ev = []
        for tt in range(2):
            tk = None
            for kc in range(32):
                tk = ph.pe(lambda e, tt=tt, kc=kc, xt=xt: e.matmul(
                    psv[tt][:, :], xt[:, kc, tt * 128:(tt + 1) * 128], Wv[:, kc, :], start=(kc == 0), stop=(kc == 31)),
                    [psv_free[tt]] if kc == 0 else (), sig=(kc == 31))
            ev = ph.act(lambda e, tt=tt, par=par: e.activation(out=vout[par][:, tt, :], in_=psv[tt][:, :], func=AF.Copy),
                        [tk, out_tok[par]])
            psv_free[tt] = ev
            tv_ev.append(ev)
            if tt == 1:
                xs.done(c, tk)
        ph.dma("sync", Dm["s_kT"][:, :, c * CH:(c + 1) * CH], kout[par][:, :, :], sem_o[par], waits=[last_rope])
        ph.dma("sync", Dm["s_ki2"][:, c * CH:(c + 1) * CH], kiout[par][:, :], sem_o[par], waits=[last_rope])
        out_tok[par] = ph.dma("sync", s_v_v[:, 2 * c:2 * c + 2, :], vout[par][:, :, :], sem_o[par], waits=tv_ev)


def phase_B(ph, K, Dm):
    TO = T + 128
    w_in_v = Dm["w_in"].rearrange("(kc p) n -> p kc n", p=128)
    xT_v = Dm["xT_own"].rearrange("(kc p) t -> p kc t", p=128)
    xo = ph.sb("xo", [128, 32, TO], BF16)
    semx = ph.p.new_sem("xo")
    tx = None
    for kg in range(8):
        tx = ph.dma("gpsimd", xo[:, kg * 4:kg * 4 + 4, :], xT_v[:, kg * 4:kg * 4 + 4, :], semx)
    posi = [ph.sb("posi", [128, 512], I32) for _ in range(2)]
    tmp = (ph.sb("posf", [128, 512], F32), ph.sb("arg", [128, 512], F32), ph.sb("ki", [128, 512], I32),
           ph.sb("kf", [128, 512], F32), ph.sb("r", [128, 512], F32))
    tabs = tuple(ph.sb("tab", [128, T], F32) for _ in range(4))
    ttab = None
    for hf in range(2):
        tp = ph.dma("sync", posi[hf][:], Dm["pos_own"][0:1, hf * 512:hf * 512 + 512].to_broadcast([128, 512]),
                    ph.p.new_sem("posB"))
        ttab = rope_tables(ph, K, posi[hf], 512, tuple(tb[:, hf * 512:hf * 512 + 512] for tb in tabs), tmp, [tp, ttab])
    cosH, sinH, cosI, sinI = tabs
    vp = ph.sb("vp", [128, 9, 2048], BF16)
    ring = Ring(ph, "wB", 4, [128, 8, 512], BF16)
    blocks = [("vp", nb, nb * 512, 512) for nb in range(4)] + [("vph", nb, nb * 512, 512) for nb in range(4)] + \
             [("q", nb, 2048 + nb * 512, 512) for nb in range(4)] + [("qi", 0, 5120, 512), ("wi", 0, 5696, 8)]
    pieces = []
    for (kind, nb, c0, nc_) in blocks:
        for kg in range(4):
            pieces.append([((lambda t, nc_=nc_: t[:, :, :nc_]), w_in_v[:, kg * 8:kg * 8 + 8, c0:c0 + nc_])])
    ws = Stream(ring, pieces)
    psb = [ph.ps("psB") for _ in range(8)]
    bank_free = [None] * 8
    u = ph.sb("u", [128, 512], BF16)
    t1 = ph.sb("t1", [128, 512], F32)
    t2 = ph.sb("t2", [128, 512], F32)
    qst = [ph.sb("qst", [128, 8, 512], BF16) for _ in range(2)]
    qist = [ph.sb("qist", [128, T], BF16) for _ in range(2)]
    sem_q = [ph.p.new_sem("qo") for _ in range(2)]
    q_free = [None, None]
    nq = 0
    last_rope = None
    vp_done = []
    for bi, (kind, nb, c0, nc_) in enumerate(blocks):
        last_tok = [None] * 8
        for kg in range(4):
            pi = bi * 4 + kg
            s, wt, wtok = ws.get(pi)
            tk = None
            for k8 in range(8):
                kc = kg * 8 + k8
                first = (kc == 0)
                lastk = (kc == 31)
                if kind in ("vp", "wi"):
                    for t in range(8):
                        tk = ph.pe(lambda e, t=t, kc=kc, k8=k8, wt=wt, nc_=nc_: e.matmul(
                            psb[t][:, :nc_], xo[:, kc, 128 + t * 128:256 + t * 128], wt[:, k8, :nc_],
                            start=(kc == 0), stop=(kc == 31)),
                            [wtok, tx, bank_free[t]] if first else ([wtok] if k8 == 0 and t == 0 else ()),
                            sig=(lastk or (k8 == 7 and t == 7)))
                        if lastk:
                            last_tok[t] = tk
                elif kind == "vph":
                    tk = ph.pe(lambda e, kc=kc, k8=k8, wt=wt: e.matmul(
                        psb[0][:, :], xo[:, kc, 0:128], wt[:, k8, :], start=(kc == 0), stop=(kc == 31)),
                        [wtok, tx, bank_free[0]] if first else ([wtok] if k8 == 0 else ()), sig=(lastk or k8 == 7))
                    if lastk:
                        last_tok[0] = tk
                else:
                    for ch in range(4):
                        for hf in range(2):
                            b = ch * 2 + hf
                            tk = ph.pe(lambda e, ch=ch, hf=hf, b=b, kc=kc, k8=k8, wt=wt: e.matmul(
                                psb[b][:, :], wt[:, k8, ch * 128:(ch + 1) * 128], xo[:, kc, 128 + hf * 512:640 + hf * 512],
                                start=(kc == 0), stop=(kc == 31)),
                                [wtok, tx, bank_free[b]] if first else ([wtok] if k8 == 0 and b == 0 else ()),
                                sig=(lastk or (k8 == 7 and b == 7)))
                            if lastk:
                                last_tok[b] = tk
            ws.done(pi, tk)
        if kind == "vp":
            for t in range(8):
                dst = vp[:, 1 + t, c0:c0 + 512]
                if t % 2 == 0:
                    ev = ph.act(lambda e, t=t, dst=dst: e.activation(out=dst, in_=psb[t][:, :], func=AF.Copy), [last_tok[t]])
                else:
                    ev = ph.dve(lambda e, t=t, dst=dst: e.tensor_copy(out=dst, in_=psb[t][:, :]), [last_tok[t]])
                bank_free[t] = ev
                vp_done.append(ev)
        elif kind == "vph":
            ev = ph.act(lambda e, c0=c0: e.activation(out=vp[:, 0, c0:c0 + 512], in_=psb[0][:, :], func=AF.Copy), [last_tok[0]])
            bank_free[0] = ev
            vp_done.append(ev)
        elif kind == "wi":
            sc = (8 ** -0.5) * (64 ** -0.5)
            for t in range(8):
                b_ = ph.dve(lambda e, t=t: e.tensor_scalar(out=K.wsgn[:, t, :], in0=psb[t][:, 0:8], scalar1=0.0, scalar2=2.0,
                                                          op0=ALU.is_gt, op1=ALU.mult), [last_tok[t]])
                c_ = ph.dve(lambda e, t=t: e.tensor_scalar(out=K.wsgn[:, t, :], in0=K.wsgn[:, t, :], scalar1=-1.0, scalar2=None,
                                                          op0=ALU.add), [b_])
                c_ = ph.dve(lambda e, t=t: e.scalar_tensor_tensor(out=K.wabs[:, t, :], in0=psb[t][:, 0:8], scalar=sc,
                                                                 in1=K.wsgn[:, t, :], op0=ALU.mult, op1=ALU.mult), [c_])
                bank_free[t] = c_
        else:
            par = nq % 2
            nq += 1
            for ch in range(4):
                for hf in range(2):
                    b = ch * 2 + hf
                    if kind == "q":
                        out_ap = qst[par][:, hf * 4:hf * 4 + 4, ch * 128:(ch + 1) * 128]
                        f, a = rope_fm(ph, psb[b][:, :], 512, u, psb[b], t1, t2, cosH[:, hf * 512:hf * 512 + 512],
                                       sinH[:, hf * 512:hf * 512 + 512], K.permH[:], out_ap,
                                       [last_tok[b], ttab, q_free[par], last_rope],
                                       view=lambda ap: ap.rearrange("p (a b) -> p a b", a=4))
                    else:
                        out_ap = qist[par][:, hf * 512:hf * 512 + 512]
                        f, a = rope_fm(ph, psb[b][:, :], 512, u, psb[b], t1, t2, cosI[:, hf * 512:hf * 512 + 512],
                                       sinI[:, hf * 512:hf * 512 + 512], K.permI[:], out_ap,
                                       [last_tok[b], ttab, q_free[par], last_rope])
                    bank_free[b] = f
                    last_rope = f
                if kind == "qi" :
                    if True:
                        q_free[par] = ph.dma("sync", Dm["s_qi2"][:, ch, :], qist[par][:, :], sem_q[par], waits=[last_rope])
                        par = nq % 2
                        nq += 1
            if kind == "q":
                q_free[par] = ph.dma("sync", Dm["s_qT"][:, :, nb * 512:(nb + 1) * 512], qst[par][:, :, :], sem_q[par],
                                     waits=[last_rope])
    ph.dma("sync", Dm["s_vp"], vp[:], semx, waits=vp_done)


def phase_B2(ph, K, Dm):
    semx = ph.p.new_sem("b2")
    band = ph.sb("band", [128, 4, 4, 128], BF16)
    pw = ph.sb("pw", [128, 4, 4, 512], BF16)
    pscale = ph.sb("pscale", [128, 16], F32)
    vp = ph.sb("vp", [128, 9, 2048], BF16)
    ph.dma("sync", band[:], Dm["band"], semx)
    ph.dma("sync", pscale[:], Dm["pool_scale_c"], semx)
    ph.dma("sync", vp[:], Dm["s_vp"], semx)
    tpw = None
    for g in range(4):
        tpw = ph.dma("gpsimd", pw[:, g, :, :], Dm["pool_w"][g].rearrange("(cc p) d -> p cc d", p=128), semx)
    vp_done = [tpw]
    psb = [ph.ps("psB2") for _ in range(4)]
    bank_free = [None] * 4
    pl = [ph.sb("pl", [128, 512], BF16) for _ in range(4)]
    atst = [ph.sb("atst", [128, 4, 512], BF16) for _ in range(2)]
    sem_at = [ph.p.new_sem("at") for _ in range(2)]
    at_free = [None, None]
    pl_free = [None] * 4
    it = 0
    for g in range(4):
        for hf in range(2):
            par = it % 2
            it += 1
            pl_tok = []
            for cc in range(4):
                bank = cc % 2
                tk = None
                for i in range(4):
                    tile = hf * 4 + i
                    kA, kB = (2, 3) if tile == 0 else (0, 1)
                    col = g * 512 + cc * 128
                    ph.pe(lambda e, bank=bank, i=i, tile=tile, col=col, kA=kA, g=g: e.matmul(
                        psb[bank][:, i * 128:(i + 1) * 128], vp[:, 1 + tile, col:col + 128], band[:, g, kA, :],
                        start=True, stop=False), (vp_done + [bank_free[bank], tpw]) if i == 0 else (), sig=False)
                    tk = ph.pe(lambda e, bank=bank, i=i, tile=tile, col=col, kB=kB, g=g: e.matmul(
                        psb[bank][:, i * 128:(i + 1) * 128], vp[:, tile, col:col + 128], band[:, g, kB, :],
                        start=False, stop=True), (), sig=(i == 3))
                ev = ph.act(lambda e, bank=bank, cc=cc: e.activation(out=pl[cc][:, :], in_=psb[bank][:, :], func=AF.Copy),
                            [tk, pl_free[cc]])
                bank_free[bank] = ev
                pl_tok.append(ev)
            last_m = None
            for dc in range(4):
                bank = 2 + dc % 2
                tk = None
                for cc in range(4):
                    tk = ph.pe(lambda e, bank=bank, cc=cc, dc=dc, g=g: e.matmul(
                        psb[bank][:, :], pw[:, g, cc, dc * 128:(dc + 1) * 128], pl[cc][:, :], start=(cc == 0), stop=(cc == 3)),
                        (pl_tok + [bank_free[bank]]) if cc == 0 else (), sig=(cc == 3))
                ev = ph.act(lambda e, bank=bank, dc=dc, g=g, par=par: e.activation(
                    out=atst[par][:, dc, :], in_=psb[bank][:, :], func=AF.Identity, scale=pscale[:, g * 4 + dc:g * 4 + dc + 1]),
                    [tk, at_free[par]])
                bank_free[bank] = ev
                last_m = tk
            for cc in range(4):
                pl_free[cc] = last_m
            at_free[par] = ph.dma("sync", Dm["s_AT"][:, g * 4:g * 4 + 4, hf * 512:hf * 512 + 512], atst[par][:, :, :],
                                  sem_at[par], waits=[ev])


PHASES = []
NCORES = int(os.environ.get("K_NCORES", "8"))
NQ = 8 // NCORES
MOE_FUSED = os.environ.get("K_MOE", "1") == "1"


def build(stop=99, dbg=()):
    nc = bass.Bass("TRN2", target_bir_lowering=False)
    Dm = {}

    def din(name, shape, dt=F32):
        Dm[name] = nc.dram_tensor(name, list(shape), dt, kind="ExternalInput").ap()

    def dscr(name, shape, dt):
        kind = "ExternalOutput" if name in dbg else "Internal"
        Dm[name] = nc.dram_tensor(name, list(shape), dt, kind=kind).ap()

    din("xT_ctx", [D, S]); din("memT", [D, 256]); din("pos_ctx", [1, S], I32)
    for j in range(NQ):
        din("xT_own_%d" % j, [D, T + 128]); din("x_own_%d" % j, [T, D]); din("pos_own_%d" % j, [1, T], I32)
        din("cbias_%d" % j, [T, S]); din("band_%d" % j, [128, 4, 4, 128], BF16)
    din("ident_bf", [128, 128], BF16); din("ident_f", [128, 128]); din("permH", [128, 128], BF16)
    din("permI", [128, 128], BF16); din("invs", [128, 4]); din("ones_bf", [128, 128], BF16)
    din("tri", [128, 128], BF16); din("iota64", [128, 64])
    din("w_in", [D, DIN]); din("pool_w", [4, 512, 512]); din("pool_scale_c", [128, 16]); din("w_o", [D, D])
    for nm in ["ln1_g", "ln1_b", "ln2_g", "ln2_b", "ln3_g", "ln3_b"]:
        din(nm, [1, D])
    din("w_mq", [D, D]); din("w_mk", [D, D]); din("w_mv", [D, D]); din("w_mo", [D, D])
    din("w_r", [D, 72]); din("b_r", [1, 72])
    if MOE_FUSED:
        din("w_gate", [NEXP, D, 512]); din("w_up", [NEXP, D, 512]); din("w_down", [NEXP, 512, D])
    dscr("s_kT", [128, 4, S], BF16); dscr("s_v", [S, 512], BF16); dscr("s_ki2", [128, S], BF16)
    dscr("s_qT", [128, 8, 2048], BF16); dscr("s_qi2", [128, 4, T], BF16); dscr("s_AT", [128, 32, T], BF16); dscr("s_vp", [128, 9, 2048], BF16)
    dscr("s_r", [T, D], F32); dscr("s_x1", [T, D], F32); dscr("s_x1T", [128, 32, T], BF16)
    dscr("s_qmT", [128, 32, T], BF16); dscr("s_kmT", [128, 32, 256], BF16); dscr("s_vm", [128, 2, D], BF16); dscr("s_oT", [128, 32, T], BF16); dscr("s_x2T", [128, 32, T], BF16); dscr("s_x2", [T, D], F32); dscr("s_x2b", [T, D], BF16)
    dscr("s_xe", [NEXP * CAP, D], BF16); dscr("s_ye", [NEXP * CAP, D], F32)
    dscr("s_dbg", [128, 512], F32); dscr("s_dbgi", [128, 16], I32)
    out_full = nc.dram_tensor("out", [NQ * T, D], F32, kind="ExternalOutput").ap()

    with ExitStack() as es:
        prog = Prog(nc, es)
        K = Consts()

        def psb(name, shape, dt):
            return es.enter_context(nc.sbuf_tensor(name, list(shape), dt))
        K.ident_bf = psb("ident_bf_s", [128, 128], BF16); K.ident_f = psb("ident_f_s", [128, 128], F32)
        K.permH = psb("permH_s", [128, 128], BF16); K.permI = psb("permI_s", [128, 128], BF16)
        K.invs = psb("invs_s", [128, 4], F32); K.ones_bf = psb("ones_s", [128, 128], BF16)
        K.tri = psb("tri_s", [128, 128], BF16); K.iota64 = psb("iota_s", [128, 64], F32)
        K.bias_r = psb("bias_r_s", [128, 72], F32)
        K.wabs = psb("wabs_s", [128, 8, 8], F32); K.wsgn = psb("wsgn_s", [128, 8, 8], F32)
        K.Aall = psb("Aall_s", [128, 8, 64], BF16)
        K.A12 = psb("A12_s", [128, 8, 2, 64], F32)
        K.gates = psb("gates_s", [128, 8, 2], F32)
        K.dest = psb("dest_s", [128, 8, 2], I32)
        run_phase(prog, lambda ph: phase_consts(ph, K, Dm))
        run_phase(prog, lambda ph: phase_A(ph, K, Dm))
        plist = [phase_B, phase_B2] + PHASES
        for j in range(NQ):
            for nm in ("xT_own", "x_own", "pos_own", "cbias", "band"):
                Dm[nm] = Dm["%s_%d" % (nm, j)]
            Dm["out"] = out_full[j * T:(j + 1) * T, :]
            for i, fn in enumerate(plist):
                if i > stop:
                    break
                run_phase(prog, lambda ph, fn=fn: fn(ph, K, Dm))
        for nm in ("xT_own", "x_own", "pos_own", "cbias", "band", "out"):
            Dm.pop(nm, None)
        if dbg:
            def dbgf(ph):
                sem = ph.p.new_sem("dbg")
                ph.dma("sync", Dm["s_dbg"][:, 0:64], K.wabs[:].rearrange("p a b -> p (a b)"), sem)
                ph.dma("sync", Dm["s_dbg"][:, 64:128], K.wsgn[:].rearrange("p a b -> p (a b)"), sem)
                ph.dma("sync", Dm["s_dbg"][:, 128:144], K.gates[:].rearrange("p a b -> p (a b)"), sem)
                ph.dma("sync", Dm["s_dbgi"][:, :], K.dest[:].rearrange("p a b -> p (a b)"), sem)
            run_phase(prog, dbgf)
    nc._in_names = [k for k in Dm if not k.startswith("s_") and k != "out"]
    return nc


def _bf(a):
    return np.ascontiguousarray(a).astype(ml_dtypes.bfloat16)


def host_consts():
    c = {}
    c["ident_bf"] = _bf(np.eye(128, dtype=np.float32))
    c["ident_f"] = np.eye(128, dtype=np.float32)
    m = np.arange(128)
    pH = np.zeros((128, 128), np.float32); pH[(m + 64) % 128, m] = 1.0
    pI = np.zeros((128, 128), np.float32); pI[64 * (m // 64) + ((m % 64) + 32) % 64, m] = 1.0
    c["permH"] = _bf(pH); c["permI"] = _bf(pI)
    invH = (1.0 / (np.float32(10000.0) ** (np.arange(0, 128, 2, dtype=np.float32) / np.float32(128)))).astype(np.float32)
    invI = (1.0 / (np.float32(10000.0) ** (np.arange(0, 64, 2, dtype=np.float32) / np.float32(64)))).astype(np.float32)
    invs = np.zeros((128, 4), np.float32)
    invs[:, 0] = invH[m % 64]; invs[:, 1] = np.where(m < 64, -1.0, 1.0)
    invs[:, 2] = invI[m % 32]; invs[:, 3] = np.where((m % 64) < 32, -1.0, 1.0)
    c["invs"] = invs
    c["ones_bf"] = _bf(np.ones((128, 128), np.float32))
    c["tri"] = _bf((m[:, None] < m[None, :]).astype(np.float32))
    c["iota64"] = np.tile(np.arange(64, dtype=np.float32)[None, :], (128, 1))
    return c


def host_band(first_of_seq):
    band = np.zeros((128, 4, 4, 128), np.float32)
    tp = np.arange(128)[:, None]; t = np.arange(128)[None, :]
    for g, w in enumerate((2, 4, 8, 16)):
        inwin = (tp <= t) & (tp >= t - w + 1)
        PA = inwin * (1.0 / w) - (tp == t)
        PB = ((tp - 128) >= (t - w + 1)) * (1.0 / w)
        cnt = np.minimum(t + 1, w).astype(np.float32)
        PA0 = inwin / cnt - (tp == t)
        band[:, g, 0] = PA; band[:, g, 1] = PB
        band[:, g, 2] = PA0 if first_of_seq else PA
        band[:, g, 3] = 0.0 if first_of_seq else PB
    return _bf(band)


_NC_CACHE = {}


def kernel(**inp):
    stop = int(os.environ.get("K_STOP", "99"))
    dbg = tuple(x for x in os.environ.get("K_DBG", "").split(",") if x)
    key = (stop, dbg)
    if key not in _NC_CACHE:
        _NC_CACHE[key] = build(stop, dbg)
    nc = _NC_CACHE[key]
    x = np.asarray(inp["x"], np.float32); mem = np.asarray(inp["mem"], np.float32)
    pos = np.asarray(inp["positions"]).astype(np.int32)
    shared = host_consts()
    g = lambda k: np.ascontiguousarray(np.asarray(inp[k], np.float32)[0])
    shared["w_in"] = g("w_in"); shared["pool_w"] = g("pool_w")
    shared["pool_scale_c"] = np.ascontiguousarray(g("pool_scale").reshape(16, 128).T)
    shared["w_o"] = g("w_o")
    for nm in ["ln1_g", "ln1_b", "ln2_g", "ln2_b", "ln3_g", "ln3_b"]:
        shared[nm] = g(nm).reshape(1, D)
    for nm in ["w_mq", "w_mk", "w_mv", "w_mo", "w_gate", "w_up", "w_down"]:
        shared[nm] = g(nm)
    shared["w_r"] = np.ascontiguousarray(np.concatenate([g("w_group_router"), g("w_expert_router")], axis=1))
    shared["b_r"] = np.concatenate([g("b_group_router"), g("b_expert_router")]).reshape(1, 72)
    xT = [np.ascontiguousarray(x[b].T) for b in range(2)]
    memT = [np.ascontiguousarray(mem[b].T) for b in range(2)]
    bands = {True: host_band(True), False: host_band(False)}
    in_maps = []
    kidx = np.arange(S)[None, :]
    cpb = NCORES // 2 if NCORES >= 2 else 1
    for c in range(NCORES):
        b = c // cpb
        m = dict(shared)
        m["xT_ctx"] = xT[b]
        m["memT"] = memT[b]
        m["pos_ctx"] = np.ascontiguousarray(pos[b].reshape(1, S))
        for jl in range(NQ):
            j = (c % cpb) * NQ + jl
            own = np.zeros((D, T + 128), np.float32)
            own[:, 128:] = xT[b][:, j * T:(j + 1) * T]
            if j > 0:
                own[:, :128] = xT[b][:, j * T - 128:j * T]
            m["xT_own_%d" % jl] = own
            m["x_own_%d" % jl] = np.ascontiguousarray(x[b, j * T:(j + 1) * T])
            m["pos_own_%d" % jl] = np.ascontiguousarray(pos[b, j * T:(j + 1) * T].reshape(1, T))
            qidx = (j * T + np.arange(T))[:, None]
            m["cbias_%d" % jl] = np.where(kidx <= qidx, np.float32(0.0), np.float32(NEG_BIG)).astype(np.float32)
            m["band_%d" % jl] = bands[j == 0]
        in_maps.append({k: m[k] for k in nc._in_names})
    res = run_bass_kernel_spmd(nc, in_maps, core_ids=list(range(NCORES)))
    kernel.last = res
    out = np.zeros((2, S, D), np.float32)
    for c in range(NCORES):
        b = c // cpb
        j0 = (c % cpb) * NQ
        out[b, j0 * T:(j0 + NQ) * T] = np.asarray(res.results[c]["out"])
    return out

def phase_C(ph, K, Dm):
    sem_l = ph.p.new_sem("lc")
    kT = ph.sb("kT", [128, 4, S], BF16)
    va = ph.sb("va", [128, 32, 4, 130], BF16)
    ki2 = ph.sb("ki2", [128, S], BF16)
    qT = ph.sb("qT", [128, 8, 2048], BF16)
    qi2 = ph.sb("qi2", [128, 4, T], BF16)
    for g in range(4):
        ph.dma("sync", kT[:, g, :], Dm["s_kT"][:, g, :], sem_l)
        ph.dma("sync", va[:, :, g, 0:128], Dm["s_v"][:, g * 128:(g + 1) * 128].rearrange("(n p) d -> p n d", p=128), sem_l)
    ph.dma("sync", ki2[:], Dm["s_ki2"], sem_l)
    ph.dma("sync", qT[:], Dm["s_qT"], sem_l)
    tl = ph.dma("sync", qi2[:], Dm["s_qi2"], sem_l)
    tones = ph.dve(lambda e: e.memset(va[:, :, :, 128:130], 1.0))
    sc = ph.sb("sc", [128, S], F32)
    work = ph.sb("work", [128, S], F32)
    rl = [ph.sb("rl", [128, 512], F32) for _ in range(2)]
    mask = ph.sb("mask", [128, S], BF16)
    mT = ph.sb("mT", [128, 32, 128], BF16)
    pT = [ph.sb("pT", [128, 512], BF16) for _ in range(3)]
    osb = ph.sb("osb", [128, 2048], BF16)
    atT = ph.sb("atT", [128, 16, 128], BF16)
    m8 = ph.sb("m8", [128, 8], F32)
    rec = ph.sb("rec", [128, 4], F32)
    psS = [ph.ps("psS") for _ in range(2)]
    psT = ph.ps("psT", (128, 1024), BF16)
    psO = [ph.ps("psO") for _ in range(4)]
    sem_b = ph.p.new_sem("cb")
    sem_at = ph.p.new_sem("cat")
    psS_free = [None, None]
    psT_free = None
    psO_free = [None] * 4
    rl_free = [None, None]
    pT_free = [None] * 3
    sc_free = None
    mT_free = None
    osb_free = None
    atT_free = None
    nS = 0
    nP = 0
    scale = 128 ** -0.5
    for i in range(NT):
        tb = ph.dma("sync", sc[:], Dm["cbias"][i * 128:(i + 1) * 128, :], sem_b, waits=[sc_free])
        nR = 0
        last_acc = None
        for kb in range(8):
            acc = tb
            for h in range(8):
                par = nS % 2
                nS += 1
                lo = (h % 2) * 64
                tk = ph.pe(lambda e, par=par, lo=lo, h=h, kb=kb, i=i: e.matmul(
                    psS[par][:, :], qi2[lo:lo + 64, h // 2, i * 128:(i + 1) * 128], ki2[lo:lo + 64, kb * 512:(kb + 1) * 512],
                    start=True, stop=True), [tl, psS_free[par]])
                rp = nR % 2
                nR += 1
                a = ph.act(lambda e, par=par, rp=rp, h=h, i=i: e.activation(
                    out=rl[rp][:, :], in_=psS[par][:, :], func=AF.Relu, scale=K.wabs[:, i, h:h + 1]), [tk, rl_free[rp]])
                psS_free[par] = a
                acc = ph.dve(lambda e, rp=rp, h=h, i=i, kb=kb: e.scalar_tensor_tensor(
                    out=sc[:, kb * 512:(kb + 1) * 512], in0=rl[rp][:, :], scalar=K.wsgn[:, i, h:h + 1],
                    in1=sc[:, kb * 512:(kb + 1) * 512], op0=ALU.mult, op1=ALU.add), [a, acc])
                rl_free[rp] = acc
            last_acc = acc
        t = ph.dve(lambda e: e.max(out=m8[:, :], in_=sc[:, :]), [last_acc, mT_free])
        t = ph.dve(lambda e: e.match_replace(out=work[:, :], in_to_replace=m8[:, :], in_values=sc[:, :], imm_value=NEG_MID), [t])
        for it in range(31):
            t = ph.dve(lambda e: e.max(out=m8[:, :], in_=work[:, :]), [t])
            if it < 30:
                t = ph.dve(lambda e: e.match_replace(out=work[:, :], in_to_replace=m8[:, :], in_values=work[:, :],
                                                     imm_value=NEG_MID), [t])
        tm = ph.dve(lambda e: e.tensor_scalar(out=mask[:, :], in0=sc[:, :], scalar1=m8[:, 7:8], scalar2=None, op0=ALU.is_ge), [t])
        sc_free = tm
        evs = []
        for k4 in range(8):
            tk = None
            for q in range(4):
                tk = ph.pe(lambda e, k4=k4, q=q: e.transpose(psT[:, q * 128:(q + 1) * 128], mask[:, (k4 * 4 + q) * 128:(k4 * 4 + q + 1) * 128],
                                                          K.ident_bf[:]), [tm, psT_free] if q == 0 else (), sig=(q == 3))
            ev = ph.act(lambda e, k4=k4: e.activation(out=mT[:, k4 * 4:k4 * 4 + 4, :],
                                                     in_=psT[:, 0:512].rearrange("p (a b) -> p a b", a=4), func=AF.Copy),
                        [tk, mT_free])
            psT_free = ev
            evs.append(ev)
        tmT = evs[-1]
        last_pool = None
        for g in range(4):
            for kt in range(32):
                par = nS % 2
                nS += 1
                tk = ph.pe(lambda e, par=par, g=g, kt=kt, i=i: e.matmul(
                    psS[par][:, :], kT[:, g, kt * 128:(kt + 1) * 128], qT[:, i, g * 512:(g + 1) * 512], start=True, stop=True),
                    [tl, psS_free[par]])
                p3 = nP % 3
                nP += 1
                a = ph.act(lambda e, par=par, p3=p3: e.activation(out=pT[p3][:, :], in_=psS[par][:, :], func=AF.Exp, scale=scale),
                           [tk, pT_free[p3]])
                psS_free[par] = a
                m = ph.pool(lambda e, p3=p3, kt=kt: e.tensor_tensor(
                    out=pT[p3][:, :].rearrange("p (a b) -> p a b", a=4), in0=pT[p3][:, :].rearrange("p (a b) -> p a b", a=4),
                    in1=mT[:, kt, :].unsqueeze(1).to_broadcast([128, 4, 128]), op=ALU.mult), [a, tmT])
                last_pool = m
                for hh in range(4):
                    tk = ph.pe(lambda e, hh=hh, p3=p3, kt=kt, g=g: e.matmul(
                        psO[hh][:, 0:130], pT[p3][:, hh * 128:(hh + 1) * 128], va[:, kt, g, :], start=(kt == 0), stop=(kt == 31)),
                        [m, tones, psO_free[hh]] if (hh == 0 or kt == 0) else (), sig=(hh == 3))
                pT_free[p3] = tk
            tr = ph.dve(lambda e: e.tensor_copy(out=rec[:, :], in_=rec[:, :]), [tk], sig=False) if False else None
            for hh in range(4):
                r1 = ph.dve(lambda e, hh=hh: e.reciprocal(out=rec[:, hh:hh + 1], in_=psO[hh][:, 128:129]), [tk, osb_free])
                r2 = ph.dve(lambda e, hh=hh, g=g: e.tensor_scalar(
                    out=osb[:, (4 * g + hh) * 128:(4 * g + hh + 1) * 128], in0=psO[hh][:, 0:128], scalar1=rec[:, hh:hh + 1],
                    scalar2=None, op0=ALU.mult), [r1])
                psO_free[hh] = r2
            last_o = r2
        mT_free = last_pool
        for hq in range(4):
            tk = None
            for q in range(4):
                h = hq * 4 + q
                tk = ph.pe(lambda e, q=q, h=h: e.transpose(psT[:, q * 128:(q + 1) * 128], osb[:, h * 128:(h + 1) * 128], K.ident_bf[:]),
                           [last_o, psT_free] if q == 0 else (), sig=(q == 3))
            ev = ph.act(lambda e, hq=hq: e.activation(out=atT[:, hq * 4:hq * 4 + 4, :],
                                                     in_=psT[:, 0:512].rearrange("p (a b) -> p a b", a=4), func=AF.Copy),
                        [tk, atT_free])
            psT_free = ev
        osb_free = tk
        atT_free = ph.dma("sync", Dm["s_AT"][:, 16:32, i * 128:(i + 1) * 128], atT[:, :, :], sem_at, waits=[ev])


def gemm_resid(ph, K, Dm, at_name, w_name, xres_name):
    sem_l = ph.p.new_sem("gl")
    AT = ph.sb("AT", [128, 32, T], BF16)
    tA = None
    for kg in range(4):
        tA = ph.dma("sync", AT[:, kg * 8:kg * 8 + 8, :], Dm[at_name][:, kg * 8:kg * 8 + 8, :], sem_l)
    w_v = Dm[w_name].rearrange("(kc p) n -> p kc n", p=128)
    x_v = Dm[xres_name].rearrange("(t p) c -> p t c", p=128)
    r_v = Dm["s_r"].rearrange("(t p) c -> p t c", p=128)
    ring = Ring(ph, "wG", 4, [128, 8, 512], BF16)
    pieces = [[((lambda t: t[:, :, :]), w_v[:, kg * 8:kg * 8 + 8, nb * 512:(nb + 1) * 512])] for nb in range(8) for kg in range(4)]
    ws = Stream(ring, pieces)
    xring = Ring(ph, "xb", 2, [128, 8, 512], F32, queue="sync")
    xs = Stream(xring, [[((lambda t: t[:, :, :]), x_v[:, :, nb * 512:(nb + 1) * 512])] for nb in range(8)])
    rb = [ph.sb("rb", [128, 8, 512], F32) for _ in range(2)]
    sem_r = [ph.p.new_sem("ro") for _ in range(2)]
    rb_free = [None, None]
    psb = [ph.ps("psG") for _ in range(8)]
    bank_free = [None] * 8
    for nb in range(8):
        last_tok = [None] * 8
        for kg in range(4):
            pi = nb * 4 + kg
            s, wt, wtok = ws.get(pi)
            tk = None
            for k8 in range(8):
                kc = kg * 8 + k8
                for t in range(8):
                    tk = ph.pe(lambda e, t=t, kc=kc, k8=k8, wt=wt: e.matmul(
                        psb[t][:, :], AT[:, kc, t * 128:(t + 1) * 128], wt[:, k8, :], start=(kc == 0), stop=(kc == 31)),
                        [wtok, tA, bank_free[t]] if kc == 0 else ([wtok] if k8 == 0 and t == 0 else ()),
                        sig=(kc == 31 or (k8 == 7 and t == 7)))
                    if kc == 31:
                        last_tok[t] = tk
            ws.done(pi, tk)
        par = nb % 2
        sx, xt, xtok = xs.get(nb)
        ev = None
        for t in range(8):
            ev = ph.dve(lambda e, t=t, xt=xt, par=par: e.scalar_tensor_tensor(
                out=rb[par][:, t, :], in0=xt[:, t, :], scalar=ALPHA, in1=psb[t][:, :], op0=ALU.mult, op1=ALU.add),
                [last_tok[t], xtok, rb_free[par]])
            bank_free[t] = ev
        xs.done(nb, ev)
        rb_free[par] = ph.dma("sync", r_v[:, :, nb * 512:(nb + 1) * 512], rb[par][:, :, :], sem_r[par], waits=[ev])


def ln_phase(ph, K, Dm, g_name, b_name, dst_x, dst_T=None, dst_b16=None):
    sem_l = ph.p.new_sem("ll")
    gb = ph.sb("gb", [128, D], F32)
    bb = ph.sb("bb", [128, D], F32)
    ph.dma("sync", gb[:], Dm[g_name].to_broadcast([128, D]), sem_l)
    tgb = ph.dma("sync", bb[:], Dm[b_name].to_broadcast([128, D]), sem_l)
    rring = Ring(ph, "rt", 2, [128, D], F32, queue="sync")
    rs = Stream(rring, [[((lambda t: t[:, :]), Dm["s_r"][t * 128:(t + 1) * 128, :])] for t in range(NT)])
    st = ph.sb("st", [128, 8, 6], F32)
    mv = ph.sb("mv", [128, 2], F32)
    sd = ph.sb("sd", [128, 4], F32)
    xn = ph.sb("xn", [128, D], F32)
    xg = ph.sb("xg", [128, D], F32)
    yt = [ph.sb("yt", [128, D], F32) for _ in range(2)]
    sem_y = [ph.p.new_sem("yo") for _ in range(2)]
    y_free = [[], []]
    sem_T2 = ph.p.new_sem("yT2")
    yT = ph.sb("yT", [128, 32, 128], BF16) if dst_T is not None else None
    yb = ph.sb("ybf", [128, D], BF16) if dst_b16 is not None else None
    sem_T = ph.p.new_sem("yT")
    yT_free = None
    yb_free = None
    psF = [ph.ps("psF") for _ in range(2)]
    psF_free = [None, None]
    xn_free = None
    xg_free = None
    for t in range(NT):
        s, rt, rtok = rs.get(t)
        par = t % 2
        d = None
        for c in range(8):
            d = ph.dve(lambda e, c=c, rt=rt: e.bn_stats(out=st[:, c, :], in_=rt[:, c * 512:(c + 1) * 512]), [rtok, d])
        d = ph.dve(lambda e: e.bn_aggr(out=mv[:, :], in_=st[:, :, :].rearrange("p a b -> p (a b)")), [d])
        d = ph.dve(lambda e: e.tensor_scalar(out=sd[:, 0:1], in0=mv[:, 1:2], scalar1=LN_EPS, scalar2=None, op0=ALU.add), [d])
        a = ph.act(lambda e: e.activation(out=sd[:, 1:2], in_=sd[:, 0:1], func=AF.Sqrt), [d])
        d = ph.dve(lambda e: e.reciprocal(out=sd[:, 2:3], in_=sd[:, 1:2]), [a])
        d = ph.dve(lambda e: e.tensor_scalar(out=sd[:, 3:4], in0=mv[:, 0:1], scalar1=sd[:, 2:3], scalar2=-1.0,
                                             op0=ALU.mult, op1=ALU.mult), [d])
        a = ph.act(lambda e, rt=rt: e.activation(out=xn[:, :], in_=rt[:, :], func=AF.Identity, scale=sd[:, 2:3], bias=sd[:, 3:4]),
                   [d, xn_free])
        rs.done(t, a)
        p = ph.pool(lambda e: e.tensor_tensor(out=xg[:, :], in0=xn[:, :], in1=gb[:, :], op=ALU.mult), [a, tgb, xg_free])
        xn_free = p
        y = ph.dve(lambda e, par=par: e.tensor_tensor(out=yt[par][:, :], in0=xg[:, :], in1=bb[:, :], op=ALU.add),
                   [p, tgb] + y_free[par])
        xg_free = y
        users = [ph.dma("sync", Dm[dst_x][t * 128:(t + 1) * 128, :], yt[par][:, :], sem_y[par], waits=[y])]
        if dst_b16 is not None:
            cb = ph.act(lambda e, par=par: e.activation(out=yb[:, :], in_=yt[par][:, :], func=AF.Copy), [y, yb_free])
            yb_free = ph.dma("sync", Dm[dst_b16][t * 128:(t + 1) * 128, :], yb[:, :], sem_T, waits=[cb])
            users.append(cb)
        if dst_T is not None:
            ev = None
            for c4 in range(8):
                bp = c4 % 2
                tk = None
                for q in range(4):
                    c = c4 * 4 + q
                    tk = ph.pe(lambda e, bp=bp, q=q, c=c, par=par: e.transpose(
                        psF[bp][:, q * 128:(q + 1) * 128], yt[par][:, c * 128:(c + 1) * 128], K.ident_f[:]),
                        [y, psF_free[bp]] if q == 0 else (), sig=(q == 3))
                if c4 % 2 == 0:
                    ev = ph.act(lambda e, bp=bp, c4=c4: e.activation(
                        out=yT[:, c4 * 4:c4 * 4 + 4, :], in_=psF[bp][:, :].rearrange("p (a b) -> p a b", a=4), func=AF.Copy),
                        [tk, yT_free])
                else:
                    ev = ph.dve(lambda e, bp=bp, c4=c4: e.tensor_copy(
                        out=yT[:, c4 * 4:c4 * 4 + 4, :], in_=psF[bp][:, :].rearrange("p (a b) -> p a b", a=4)), [tk, yT_free, ev])
                psF_free[bp] = ev
                last_tr = tk
            users.append(last_tr)
            yT_free = ph.dma("sync", Dm[dst_T][:, :, t * 128:(t + 1) * 128], yT[:, :, :], sem_T2,
                             waits=[ev, psF_free[0], psF_free[1]])
        y_free[par] = users


def phase_D1(ph, K, Dm):
    gemm_resid(ph, K, Dm, "s_AT", "w_o", "x_own")


def phase_D2(ph, K, Dm):
    ln_phase(ph, K, Dm, "ln1_g", "ln1_b", "s_x1", dst_T="s_x1T")


def phase_E1(ph, K, Dm):
    sem_l = ph.p.new_sem("e1")
    mt_sb = ph.sb("memT", [128, 32, 256], BF16)
    tm = None
    mem_v = Dm["memT"].rearrange("(kc p) t -> p kc t", p=128)
    for kg in range(4):
        tm = ph.dma("gpsimd", mt_sb[:, kg * 8:kg * 8 + 8, :], mem_v[:, kg * 8:kg * 8 + 8, :], sem_l)
    kmT = ph.sb("kmT", [128, 32, 256], BF16)
    vm = ph.sb("vm", [128, 2, D], BF16)
    ring = Ring(ph, "wE1", 4, [128, 8, 512], BF16)
    pieces = []
    for wn in ("w_mk", "w_mv"):
        w_v = Dm[wn].rearrange("(kc p) n -> p kc n", p=128)
        for nb in range(8):
            for kg in range(4):
                pieces.append([((lambda t: t[:, :, :]), w_v[:, kg * 8:kg * 8 + 8, nb * 512:(nb + 1) * 512])])
    ws = Stream(ring, pieces)
    psb = [ph.ps("psE1") for _ in range(8)]
    bank_free = [None] * 8
    evs = []
    for wi_, wn in enumerate(("w_mk", "w_mv")):
        for nb in range(8):
            off = (nb % 2) * 4 if wi_ == 0 else (nb % 4) * 2
            nbk = 4 if wi_ == 0 else 2
            last_tok = [None] * 8
            for kg in range(4):
                pi = (wi_ * 8 + nb) * 4 + kg
                s, wt, wtok = ws.get(pi)
                tk = None
                for k8 in range(8):
                    kc = kg * 8 + k8
                    for b in range(nbk):
                        if wi_ == 0:
                            fn = (lambda e, b=b, kc=kc, k8=k8, wt=wt, off=off: e.matmul(
                                psb[off + b][:, 0:256], wt[:, k8, b * 128:(b + 1) * 128], mt_sb[:, kc, :],
                                start=(kc == 0), stop=(kc == 31)))
                        else:
                            fn = (lambda e, b=b, kc=kc, k8=k8, wt=wt, off=off: e.matmul(
                                psb[off + b][:, :], mt_sb[:, kc, b * 128:(b + 1) * 128], wt[:, k8, :],
                                start=(kc == 0), stop=(kc == 31)))
                        tk = ph.pe(fn, [wtok, tm, bank_free[off + b]] if kc == 0 else ([wtok] if k8 == 0 and b == 0 else ()),
                                   sig=(kc == 31 or (k8 == 7 and b == nbk - 1)))
                        if kc == 31:
                            last_tok[b] = tk
                ws.done(pi, tk)
            for b in range(nbk):
                if wi_ == 0:
                    dst = kmT[:, nb * 4 + b, :]
                    src = psb[off + b][:, 0:256]
                else:
                    dst = vm[:, b, nb * 512:(nb + 1) * 512]
                    src = psb[off + b][:, :]
                if b % 2 == 0:
                    ev = ph.act(lambda e, dst=dst, src=src: e.activation(out=dst, in_=src, func=AF.Copy), [last_tok[b]])
                else:
                    ev = ph.dve(lambda e, dst=dst, src=src: e.tensor_copy(out=dst, in_=src), [last_tok[b]])
                bank_free[off + b] = ev
                evs.append(ev)
    ph.dma("sync", Dm["s_kmT"], kmT[:], sem_l, waits=evs)
    ph.dma("sync", Dm["s_vm"], vm[:], sem_l, waits=evs)


def phase_E2(ph, K, Dm):
    sem_l = ph.p.new_sem("e2")
    xT = ph.sb("x1T", [128, 32, T], BF16)
    tx = None
    for kg in range(4):
        tx = ph.dma("sync", xT[:, kg * 8:kg * 8 + 8, :], Dm["s_x1T"][:, kg * 8:kg * 8 + 8, :], sem_l)
    w_v = Dm["w_mq"].rearrange("(kc p) n -> p kc n", p=128)
    ring = Ring(ph, "wE2", 4, [128, 8, 512], BF16)
    ws = Stream(ring, [[((lambda t: t[:, :, :]), w_v[:, kg * 8:kg * 8 + 8, nb * 512:(nb + 1) * 512])]
                       for nb in range(8) for kg in range(4)])
    psb = [ph.ps("psE2") for _ in range(8)]
    bank_free = [None] * 8
    qst = [ph.sb("qmst", [128, 4, T], BF16) for _ in range(2)]
    sem_q = [ph.p.new_sem("qmo") for _ in range(2)]
    q_free = [None, None]
    for nb in range(8):
        last_tok = [None] * 8
        for kg in range(4):
            pi = nb * 4 + kg
            s, wt, wtok = ws.get(pi)
            tk = None
            for k8 in range(8):
                kc = kg * 8 + k8
                for b in range(8):
                    ch, hf = b // 2, b % 2
                    tk = ph.pe(lambda e, b=b, ch=ch, hf=hf, kc=kc, k8=k8, wt=wt: e.matmul(
                        psb[b][:, :], wt[:, k8, ch * 128:(ch + 1) * 128], xT[:, kc, hf * 512:(hf + 1) * 512],
                        start=(kc == 0), stop=(kc == 31)),
                        [wtok, tx, bank_free[b]] if kc == 0 else ([wtok] if k8 == 0 and b == 0 else ()),
                        sig=(kc == 31 or (k8 == 7 and b == 7)))
                    if kc == 31:
                        last_tok[b] = tk
            ws.done(pi, tk)
        par = nb % 2
        evs = []
        for b in range(8):
            ch, hf = b // 2, b % 2
            dst = qst[par][:, ch, hf * 512:(hf + 1) * 512]
            if b % 2 == 0:
                ev = ph.act(lambda e, b=b, dst=dst: e.activation(out=dst, in_=psb[b][:, :], func=AF.Copy), [last_tok[b], q_free[par]])
            else:
                ev = ph.dve(lambda e, b=b, dst=dst: e.tensor_copy(out=dst, in_=psb[b][:, :]), [last_tok[b], q_free[par]])
            bank_free[b] = ev
            evs.append(ev)
        q_free[par] = ph.dma("sync", Dm["s_qmT"][:, nb * 4:nb * 4 + 4, :], qst[par][:, :, :], sem_q[par], waits=evs)


def phase_E3(ph, K, Dm):
    sem_l = ph.p.new_sem("e3")
    qmT = ph.sb("qmT", [128, 32, T], BF16)
    kmT = ph.sb("kmT", [128, 32, 256], BF16)
    vm = ph.sb("vm", [128, 2, D], BF16)
    for kg in range(4):
        ph.dma("sync", qmT[:, kg * 8:kg * 8 + 8, :], Dm["s_qmT"][:, kg * 8:kg * 8 + 8, :], sem_l)
    ph.dma("sync", kmT[:], Dm["s_kmT"], sem_l)
    tl = ph.dma("sync", vm[:], Dm["s_vm"], sem_l)
    pT = [[ph.sb("pTm", [128, 512], BF16) for _ in range(2)] for _ in range(2)]
    rs = ph.sb("rs", [128, 512], F32)
    ost = [ph.sb("ost", [128, 8, 512], BF16) for _ in range(2)]
    sem_o = [ph.p.new_sem("oo") for _ in range(2)]
    o_free = [None, None]
    psS = [ph.ps("psS3") for _ in range(2)]
    psZ = ph.ps("psZ")
    psO = [ph.ps("psO3") for _ in range(2)]
    psS_free = [None, None]
    psZ_free = None
    psO_free = [None, None]
    pT_free = [None, None]
    rs_free = None
    it = 0
    scale = 1024 ** -0.5
    for hm in range(4):
        for hf in range(2):
            par = it % 2
            it += 1
            exps = []
            for mt in range(2):
                tk = None
                for kc in range(8):
                    tk = ph.pe(lambda e, mt=mt, kc=kc, hm=hm, hf=hf: e.matmul(
                        psS[mt][:, :], kmT[:, hm * 8 + kc, mt * 128:(mt + 1) * 128], qmT[:, hm * 8 + kc, hf * 512:(hf + 1) * 512],
                        start=(kc == 0), stop=(kc == 7)), [tl, psS_free[mt]] if kc == 0 else (), sig=(kc == 7))
                a = ph.act(lambda e, mt=mt, par=par: e.activation(out=pT[par][mt][:, :], in_=psS[mt][:, :], func=AF.Exp, scale=scale),
                           [tk, pT_free[par]])
                psS_free[mt] = a
                exps.append(a)
            tk = None
            for mt in range(2):
                tk = ph.pe(lambda e, mt=mt, par=par: e.matmul(psZ[:, :], K.ones_bf[:], pT[par][mt][:, :], start=(mt == 0), stop=(mt == 1)),
                           (exps + [psZ_free]) if mt == 0 else (), sig=(mt == 1))
            r = ph.dve(lambda e: e.reciprocal(out=rs[:, :], in_=psZ[:, :]), [tk, rs_free])
            psZ_free = r
            ev = None
            for dc in range(8):
                bp = dc % 2
                tk = None
                for mt in range(2):
                    col = hm * 1024 + dc * 128
                    tk = ph.pe(lambda e, mt=mt, bp=bp, col=col, par=par: e.matmul(
                        psO[bp][:, :], vm[:, mt, col:col + 128], pT[par][mt][:, :], start=(mt == 0), stop=(mt == 1)),
                        (exps + [psO_free[bp]]) if mt == 0 else (), sig=(mt == 1))
                ev = ph.dve(lambda e, bp=bp, dc=dc, par=par: e.tensor_tensor(out=ost[par][:, dc, :], in0=psO[bp][:, :], in1=rs[:, :], op=ALU.mult),
                            [tk, r, o_free[par]])
                psO_free[bp] = ev
                last_pv = tk
            pT_free[par] = last_pv
            rs_free = ev
            o_free[par] = ph.dma("sync", Dm["s_oT"][:, hm * 8:hm * 8 + 8, hf * 512:(hf + 1) * 512], ost[par][:, :, :], sem_o[par],
                                 waits=[ev])


def phase_E4(ph, K, Dm):
    gemm_resid(ph, K, Dm, "s_oT", "w_mo", "s_x1")


def phase_E5(ph, K, Dm):
    ln_phase(ph, K, Dm, "ln2_g", "ln2_b", "s_x2", dst_T="s_x2T", dst_b16="s_x2b")


AXX = mybir.AxisListType.X


def phase_F1(ph, K, Dm):
    sem_l = ph.p.new_sem("f1")
    xT = ph.sb("x2T", [128, 32, T], BF16)
    tx = None
    for kg in range(4):
        tx = ph.dma("sync", xT[:, kg * 8:kg * 8 + 8, :], Dm["s_x2T"][:, kg * 8:kg * 8 + 8, :], sem_l)
    wr = ph.sb("wr", [128, 32, 72], BF16)
    twr = ph.dma("gpsimd", wr[:], Dm["w_r"].rearrange("(kc p) n -> p kc n", p=128), ph.p.new_sem("wr"))
    psb = [ph.ps("psF1") for _ in range(8)]
    lg = ph.sb("lg", [128, 72], F32)
    gm = ph.sb("gm", [128, 8], F32)
    ohg = ph.sb("ohg", [128, 8], F32)
    eg = ph.sb("eg", [128, 8], F32)
    sm = ph.sb("sm", [128, 8], F32)
    tmp3 = ph.sb("tmp3", [128, 8, 8], F32)
    esel = ph.sb("esel", [128, 8], F32)
    em = ph.sb("em", [128, 8], F32)
    oh1 = ph.sb("oh1", [128, 8], F32)
    oh2 = ph.sb("oh2", [128, 8], F32)
    d = None
    for t in range(NT):
        tk = None
        for kc in range(32):
            tk = ph.pe(lambda e, t=t, kc=kc: e.matmul(psb[t][:, 0:72], xT[:, kc, t * 128:(t + 1) * 128], wr[:, kc, :],
                                                      start=(kc == 0), stop=(kc == 31)), [tx, twr] if kc == 0 else (), sig=(kc == 31))
        d = ph.dve(lambda e, t=t: e.tensor_tensor(out=lg[:, :], in0=psb[t][:, 0:72], in1=K.bias_r[:, :], op=ALU.add), [tk, d])
        d = ph.dve(lambda e: e.max(out=gm[:, :], in_=lg[:, 0:8]), [d])
        d = ph.dve(lambda e: e.tensor_scalar(out=ohg[:, :], in0=lg[:, 0:8], scalar1=gm[:, 0:1], scalar2=None, op0=ALU.is_equal), [d])
        d = ph.dve(lambda e: e.tensor_scalar(out=sm[:, 0:1], in0=gm[:, 0:1], scalar1=-1.0, scalar2=None, op0=ALU.mult), [d])
        a = ph.act(lambda e: e.activation(out=eg[:, :], in_=lg[:, 0:8], func=AF.Exp, bias=sm[:, 0:1]), [d])
        d = ph.dve(lambda e: e.tensor_reduce(out=sm[:, 1:2], in_=eg[:, :], axis=AXX, op=ALU.add), [a])
        d = ph.dve(lambda e: e.reciprocal(out=sm[:, 2:3], in_=sm[:, 1:2]), [d])
        d = ph.dve(lambda e: e.tensor_tensor(out=tmp3[:, :, :], in0=lg[:, 8:72].rearrange("p (g j) -> p g j", g=8),
                                             in1=ohg[:, :].unsqueeze(2).to_broadcast([128, 8, 8]), op=ALU.mult), [d])
        d = ph.dve(lambda e: e.tensor_reduce(out=esel[:, :], in_=tmp3[:, :, :].rearrange("p g j -> p j g"), axis=AXX, op=ALU.add), [d])
        d = ph.dve(lambda e: e.max(out=em[:, :], in_=esel[:, :]), [d])
        d = ph.dve(lambda e: e.tensor_scalar(out=oh1[:, :], in0=esel[:, :], scalar1=em[:, 0:1], scalar2=None, op0=ALU.is_equal), [d])
        d = ph.dve(lambda e: e.tensor_scalar(out=oh2[:, :], in0=esel[:, :], scalar1=em[:, 1:2], scalar2=None, op0=ALU.is_equal), [d])
        d = ph.dve(lambda e: e.tensor_tensor(out=sm[:, 3:4], in0=em[:, 1:2], in1=em[:, 0:1], op=ALU.subtract), [d])
        a = ph.act(lambda e: e.activation(out=sm[:, 4:5], in_=sm[:, 3:4], func=AF.Exp), [d])
        d = ph.dve(lambda e: e.tensor_scalar(out=sm[:, 5:6], in0=sm[:, 4:5], scalar1=1.0, scalar2=None, op0=ALU.add), [a])
        d = ph.dve(lambda e: e.reciprocal(out=sm[:, 6:7], in_=sm[:, 5:6]), [d])
        d = ph.dve(lambda e, t=t: e.tensor_tensor(out=K.gates[:, t, 0:1], in0=sm[:, 6:7], in1=sm[:, 2:3], op=ALU.mult), [d])
        d = ph.dve(lambda e, t=t: e.tensor_tensor(out=K.gates[:, t, 1:2], in0=K.gates[:, t, 0:1], in1=sm[:, 4:5], op=ALU.mult), [d])
        for a_i, oh in enumerate((oh1, oh2)):
            d = ph.dve(lambda e, t=t, a_i=a_i, oh=oh: e.tensor_tensor(
                out=K.A12[:, t, a_i, :].rearrange("p (g j) -> p g j", g=8), in0=ohg[:, :].unsqueeze(2).to_broadcast([128, 8, 8]),
                in1=oh[:, :].unsqueeze(1).to_broadcast([128, 8, 8]), op=ALU.mult), [d])
        d = ph.dve(lambda e, t=t: e.tensor_tensor(out=K.Aall[:, t, :], in0=K.A12[:, t, 0, :], in1=K.A12[:, t, 1, :], op=ALU.add), [d])
    cnt = ph.sb("cnt", [128, 64], F32)
    tmpc = ph.sb("tmpc", [128, 64], F32)
    pe_ = ph.sb("pe_", [128, 4], F32)
    psC = psb[0]
    zt = ph.sb("zt", [128, D], BF16)
    tz = ph.pool(lambda e: e.memset(zt[:, :], 0.0))
    sem_z = ph.p.new_sem("zf")
    xe_v = Dm["s_xe"].rearrange("(e p) d -> p e d", p=128)
    tzf = None
    for e8 in range(8):
        tzf = ph.dma("sync", xe_v[:, e8 * 8:e8 * 8 + 8, :], zt[:, :].unsqueeze(1).to_broadcast([128, 8, D]), sem_z, waits=[tz])
    x2b = [ph.sb("x2b", [128, D], BF16) for _ in range(2)]
    sem_x = [ph.p.new_sem("x2b") for _ in range(2)]
    sem_s = [ph.p.new_sem("sct") for _ in range(2)]
    x_free = [None, None]
    for t in range(NT):
        tk = None
        for tp in range(t + 1):
            lhs = K.tri if tp == t else K.ones_bf
            tk = ph.pe(lambda e, tp=tp, lhs=lhs, t=t: e.matmul(psC[:, 0:64], lhs[:], K.Aall[:, tp, :], start=(tp == 0), stop=(tp == t)),
                       [d] if tp == 0 else (), sig=(tp == t))
        d = ph.dve(lambda e: e.tensor_copy(out=cnt[:, :], in_=psC[:, 0:64]), [tk, d])
        for a_i in range(2):
            d = ph.dve(lambda e, t=t, a_i=a_i: e.tensor_tensor(out=tmpc[:, :], in0=K.A12[:, t, a_i, :], in1=cnt[:, :], op=ALU.mult), [d])
            d = ph.dve(lambda e, a_i=a_i: e.tensor_reduce(out=pe_[:, 2 * a_i:2 * a_i + 1], in_=tmpc[:, :], axis=AXX, op=ALU.add), [d])
            d = ph.dve(lambda e, t=t, a_i=a_i: e.tensor_tensor(out=tmpc[:, :], in0=K.A12[:, t, a_i, :], in1=K.iota64[:, :], op=ALU.mult), [d])
            d = ph.dve(lambda e, a_i=a_i: e.tensor_reduce(out=pe_[:, 2 * a_i + 1:2 * a_i + 2], in_=tmpc[:, :], axis=AXX, op=ALU.add), [d])
            d = ph.dve(lambda e, a_i=a_i: e.scalar_tensor_tensor(out=pe_[:, 2 * a_i:2 * a_i + 1], in0=pe_[:, 2 * a_i + 1:2 * a_i + 2],
                                                                scalar=float(CAP), in1=pe_[:, 2 * a_i:2 * a_i + 1],
                                                                op0=ALU.mult, op1=ALU.add), [d])
            d = ph.dve(lambda e, a_i=a_i: e.tensor_scalar(out=pe_[:, 2 * a_i:2 * a_i + 1], in0=pe_[:, 2 * a_i:2 * a_i + 1], scalar1=0.0,
                                                         scalar2=float(NEXP * CAP - 1), op0=ALU.max, op1=ALU.min), [d])
            d = ph.dve(lambda e, t=t, a_i=a_i: e.tensor_copy(out=K.dest[:, t, a_i:a_i + 1], in_=pe_[:, 2 * a_i:2 * a_i + 1]), [d])
        par = t % 2
        tl = ph.dma("sync", x2b[par][:, :], Dm["s_x2b"][t * 128:(t + 1) * 128, :], sem_x[par], waits=[x_free[par]])
        for a_i in range(2):
            x_free[par] = ph.idma(out=Dm["s_xe"], in_=x2b[par][:, :], sem=sem_s[par], waits=[tl, d, tzf],
                                  out_off=K.dest[:, t, a_i:a_i + 1])


def phase_F2(ph, K, Dm):
    xring = Ring(ph, "xe", 2, [128, D], BF16, queue="sync")
    xs = Stream(xring, [[((lambda t: t[:, :]), Dm["s_xe"][e * CAP:(e + 1) * CAP, :])] for e in range(NEXP)])
    ring = Ring(ph, "wF", 8, [128, 4, 512], BF16)
    pieces = []
    for e in range(NEXP):
        wg = Dm["w_gate"][e].rearrange("(kc p) f -> p kc f", p=128)
        wu = Dm["w_up"][e].rearrange("(kc p) f -> p kc f", p=128)
        wd = Dm["w_down"][e].rearrange("(fc p) n -> p fc n", p=128)
        for kg in range(8):
            pieces.append([((lambda t: t[:, :, :]), wg[:, kg * 4:kg * 4 + 4, :])])
            pieces.append([((lambda t: t[:, :, :]), wu[:, kg * 4:kg * 4 + 4, :])])
        for nb in range(8):
            pieces.append([((lambda t: t[:, :, :]), wd[:, :, nb * 512:(nb + 1) * 512])])
    ws = Stream(ring, pieces)
    xeT = [ph.sb("xeT", [128, 32, 128], BF16) for _ in range(2)]
    sg = ph.sb("sg", [128, 512], F32)
    hh = ph.sb("hh", [128, 512], BF16)
    hT = ph.sb("hT", [128, 4, 128], BF16)
    ye = [ph.sb("ye", [128, D], F32) for _ in range(2)]
    sem_y = [ph.p.new_sem("yeo") for _ in range(2)]
    ye_free = [None, None]
    psT = [ph.ps("psTe", (128, 1024), BF16) for _ in range(2)]
    psG = ph.ps("psGe")
    psU = ph.ps("psUe")
    psD = [ph.ps("psDe") for _ in range(2)]
    psT_free = [None, None]
    psG_free = None
    psU_free = None
    psD_free = [None, None]
    xeT_free = [None, None]
    sg_free = None
    hh_free = None
    hT_free = None
    nT = 0
    pi = 0
    for e in range(NEXP):
        par = e % 2
        s, xe, xtok = xs.get(e)
        evs = []
        last_tr = None
        for c4 in range(8):
            bp = nT % 2
            nT += 1
            tk = None
            for q in range(4):
                c = c4 * 4 + q
                tk = ph.pe(lambda e_, bp=bp, q=q, c=c, xe=xe: e_.transpose(psT[bp][:, q * 128:(q + 1) * 128], xe[:, c * 128:(c + 1) * 128],
                                                                        K.ident_bf[:]),
                           [xtok, psT_free[bp]] if q == 0 else (), sig=(q == 3))
            src = psT[bp][:, 0:512].rearrange("p (a b) -> p a b", a=4)
            dst = xeT[par][:, c4 * 4:c4 * 4 + 4, :]
            if c4 % 2 == 0:
                ev = ph.act(lambda e_, src=src, dst=dst: e_.activation(out=dst, in_=src, func=AF.Copy), [tk, xeT_free[par]])
            else:
                ev = ph.dve(lambda e_, src=src, dst=dst: e_.tensor_copy(out=dst, in_=src), [tk, xeT_free[par]])
            psT_free[bp] = ev
            evs.append(ev)
            last_tr = tk
        xs.done(e, last_tr)
        tg = tu = None
        for kg in range(8):
            for which in range(2):
                s_, wt, wtok = ws.get(pi)
                ps_ = psG if which == 0 else psU
                fr = psG_free if which == 0 else psU_free
                tk = None
                for k4 in range(4):
                    kc = kg * 4 + k4
                    tk = ph.pe(lambda e_, ps_=ps_, kc=kc, k4=k4, wt=wt, par=par: e_.matmul(
                        ps_[:, :], xeT[par][:, kc, :], wt[:, k4, :], start=(kc == 0), stop=(kc == 31)),
                        ([wtok, fr] + evs) if kc == 0 else ([wtok] if k4 == 0 else ()), sig=(k4 == 3))
                ws.done(pi, tk)
                pi += 1
                if which == 0:
                    tg = tk
                else:
                    tu = tk
        xeT_free[par] = tu
        a = ph.act(lambda e_: e_.activation(out=sg[:, :], in_=psG[:, :], func=AF.Silu), [tg, sg_free])
        psG_free = a
        m = ph.dve(lambda e_: e_.tensor_tensor(out=hh[:, :], in0=sg[:, :], in1=psU[:, :], op=ALU.mult), [a, tu, hh_free])
        psU_free = m
        sg_free = m
        bp = nT % 2
        nT += 1
        tk = None
        for q in range(4):
            tk = ph.pe(lambda e_, bp=bp, q=q: e_.transpose(psT[bp][:, q * 128:(q + 1) * 128], hh[:, q * 128:(q + 1) * 128], K.ident_bf[:]),
                       [m, psT_free[bp]] if q == 0 else (), sig=(q == 3))
        hh_free = tk
        ev = ph.act(lambda e_, bp=bp: e_.activation(out=hT[:, :, :], in_=psT[bp][:, 0:512].rearrange("p (a b) -> p a b", a=4), func=AF.Copy),
                    [tk, hT_free])
        psT_free[bp] = ev
        evd = None
        ev6 = None
        last_dn = None
        for nb in range(8):
            s_, wt, wtok = ws.get(pi)
            bd = nb % 2
            tk = None
            for fc in range(4):
                tk = ph.pe(lambda e_, bd=bd, fc=fc, wt=wt: e_.matmul(psD[bd][:, :], hT[:, fc, :], wt[:, fc, :], start=(fc == 0), stop=(fc == 3)),
                           [wtok, ev, psD_free[bd]] if fc == 0 else (), sig=(fc == 3))
            ws.done(pi, tk)
            pi += 1
            dst = ye[par][:, nb * 512:(nb + 1) * 512]
            if nb % 2 == 0:
                evd = ph.act(lambda e_, bd=bd, dst=dst: e_.activation(out=dst, in_=psD[bd][:, :], func=AF.Copy), [tk, ye_free[par]])
            else:
                evd = ph.dve(lambda e_, bd=bd, dst=dst: e_.tensor_copy(out=dst, in_=psD[bd][:, :]), [tk, ye_free[par]])
            psD_free[bd] = evd
            if nb == 6:
                ev6 = evd
            last_dn = tk
        hT_free = last_dn
        ye_free[par] = ph.dma("sync", Dm["s_ye"][e * CAP:(e + 1) * CAP, :], ye[par][:, :], sem_y[par], waits=[evd, ev6])


def phase_F3(ph, K, Dm):
    x2 = [ph.sb("x2t", [128, D], F32) for _ in range(2)]
    r1 = [ph.sb("r1", [128, D], F32) for _ in range(2)]
    r2 = [ph.sb("r2", [128, D], F32) for _ in range(2)]
    acc = [ph.sb("acc", [128, D], F32) for _ in range(2)]
    sem_x = [ph.p.new_sem("f3x") for _ in range(2)]
    sem_1 = [ph.p.new_sem("f31") for _ in range(2)]
    sem_2 = [ph.p.new_sem("f32") for _ in range(2)]
    sem_o = [ph.p.new_sem("f3o") for _ in range(2)]
    in_free = [None, None]
    acc_free = [None, None]
    for t in range(NT):
        par = t % 2
        tx = ph.dma("sync", x2[par][:, :], Dm["s_x2"][t * 128:(t + 1) * 128, :], sem_x[par], waits=[in_free[par]])
        t1 = ph.idma(out=r1[par][:, :], in_=Dm["s_ye"], sem=sem_1[par], waits=[in_free[par]], in_off=K.dest[:, t, 0:1])
        t2 = ph.idma(out=r2[par][:, :], in_=Dm["s_ye"], sem=sem_2[par], waits=[in_free[par]], in_off=K.dest[:, t, 1:2])
        a = ph.act(lambda e, par=par: e.activation(out=acc[par][:, :], in_=x2[par][:, :], func=AF.Copy, scale=ALPHA), [tx, acc_free[par]])
        d = ph.dve(lambda e, par=par, t=t: e.scalar_tensor_tensor(out=acc[par][:, :], in0=r1[par][:, :], scalar=K.gates[:, t, 0:1],
                                                                in1=acc[par][:, :], op0=ALU.mult, op1=ALU.add), [a, t1])
        d = ph.dve(lambda e, par=par, t=t: e.scalar_tensor_tensor(out=acc[par][:, :], in0=r2[par][:, :], scalar=K.gates[:, t, 1:2],
                                                                in1=acc[par][:, :], op0=ALU.mult, op1=ALU.add), [d, t2])
        in_free[par] = d
        acc_free[par] = ph.dma("sync", Dm["s_r"][t * 128:(t + 1) * 128, :], acc[par][:, :], sem_o[par], waits=[d])


def phase_F4(ph, K, Dm):
    ln_phase(ph, K, Dm, "ln3_g", "ln3_b", "out")


PHASES[:] = [phase_C, phase_D1, phase_D2, phase_E1, phase_E2, phase_E3, phase_E4, phase_E5,
             phase_F1, phase_F2, phase_F3, phase_F4]
```
